# Optimizing a Trainium2 kernel written in Bass

```python
import jax, jax.numpy as jnp
from jax import lax
import numpy as np

D_MODEL = 1024
BATCH = 16
SEQ = 4096
DEPTH = 4

MLA_HEADS = 8
MLA_NOPE_DIM = 64
MLA_ROPE_DIM = 32
MLA_V_DIM = 64
MLA_Q_LORA = 256
MLA_KV_LORA = 128
ROPE_THETA = 10000.0
FOX_HEADS = 8
FOX_HEAD_DIM = 64
Q_BLOCK = 128
D_FF = 3584
N_EXPERTS = 8
TOP_K = 2
RMS_EPS = 1e-6

MLA_WIDTH = MLA_HEADS * MLA_V_DIM
FOX_WIDTH = FOX_HEADS * FOX_HEAD_DIM
MLA_QK_DIM = MLA_NOPE_DIM + MLA_ROPE_DIM
IN_WIDTHS = (MLA_Q_LORA, MLA_KV_LORA, MLA_ROPE_DIM, FOX_WIDTH, FOX_WIDTH, FOX_WIDTH, FOX_HEADS, D_MODEL, D_MODEL)
IN_COLS = sum(IN_WIDTHS)
N_DENSE = (DEPTH + 1) // 2
N_MOE = DEPTH // 2

kernel_name = "hybrid_mla_fox_gated_moe_adaln"


def rmsnorm(x, g):
    xf = x.astype(jnp.float32)
    y = xf * lax.rsqrt(jnp.mean(xf * xf, axis=-1, keepdims=True) + RMS_EPS)
    return (y * g.astype(jnp.float32)).astype(x.dtype)


def apply_rope(t, cos, sin):
    half = t.shape[-1] // 2
    t1, t2 = t[..., :half], t[..., half:]
    return jnp.concatenate([t1 * cos - t2 * sin, t1 * sin + t2 * cos], axis=-1)


def adaln(c, w, b):
    mod = jax.nn.silu(c) @ w + b
    shift, scale, gate = jnp.split(mod, 3, axis=-1)
    return shift[:, None, :], scale[:, None, :], gate[:, None, :]


def causal_block_attention(logits_fn, v):
    seq = v.shape[1]
    outs = []
    for q0 in range(0, seq, Q_BLOCK):
        q1 = q0 + Q_BLOCK
        logits = logits_fn(q0, q1)
        causal = (q0 + jnp.arange(Q_BLOCK))[:, None] >= jnp.arange(q1)[None, :]
        p = jax.nn.softmax(jnp.where(causal, logits, -jnp.inf), axis=-1)
        outs.append(jnp.einsum('bhqs,bshd->bqhd', p.astype(v.dtype), v[:, :q1]))
    return jnp.concatenate(outs, axis=1)


def hybrid_mixer(h, cos, sin, w_in, b_forget, q_norm_g, w_uq, kv_norm_g, w_ukv,
                 w_branch_mla, w_branch_fox, w_out):
    bsz, seq, _ = h.shape
    proj = h @ w_in
    offs = np.cumsum(IN_WIDTHS)[:-1].tolist()
    c_q, c_kv, k_r, fq, fk, fv, f_logit, g_a, g_b = jnp.split(proj, offs, axis=-1)

    q = (rmsnorm(c_q, q_norm_g) @ w_uq).reshape(bsz, seq, MLA_HEADS, MLA_QK_DIM)
    q_nope = q[..., :MLA_NOPE_DIM]
    q_rope = apply_rope(q[..., MLA_NOPE_DIM:], cos[:, :, None, :], sin[:, :, None, :])
    kv = (rmsnorm(c_kv, kv_norm_g) @ w_ukv).reshape(bsz, seq, MLA_HEADS, MLA_NOPE_DIM + MLA_V_DIM)
    k_nope, v_mla = kv[..., :MLA_NOPE_DIM], kv[..., MLA_NOPE_DIM:]
    k_rope = apply_rope(k_r, cos, sin)
    mla_scale = MLA_QK_DIM ** -0.5

    def mla_logits(q0, q1):
        s = jnp.einsum('bqhd,bshd->bhqs', q_nope[:, q0:q1], k_nope[:, :q1],
                       preferred_element_type=jnp.float32)
        s = s + jnp.einsum('bqhr,bsr->bhqs', q_rope[:, q0:q1], k_rope[:, :q1],
                           preferred_element_type=jnp.float32)
        return s * mla_scale

    y_mla = causal_block_attention(mla_logits, v_mla).reshape(bsz, seq, MLA_WIDTH)

    fq = fq.reshape(bsz, seq, FOX_HEADS, FOX_HEAD_DIM)
    fk = fk.reshape(bsz, seq, FOX_HEADS, FOX_HEAD_DIM)
    fv = fv.reshape(bsz, seq, FOX_HEADS, FOX_HEAD_DIM)
    log_f = jax.nn.log_sigmoid(f_logit.astype(jnp.float32) + b_forget.astype(jnp.float32))
    cum_log_f = jnp.transpose(jnp.cumsum(log_f, axis=1), (0, 2, 1))
    fox_scale = FOX_HEAD_DIM ** -0.5

    def fox_logits(q0, q1):
        s = jnp.einsum('bqhd,bshd->bhqs', fq[:, q0:q1], fk[:, :q1],
                       preferred_element_type=jnp.float32) * fox_scale
        return s + (cum_log_f[:, :, q0:q1, None] - cum_log_f[:, :, None, :q1])

    y_fox = causal_block_attention(fox_logits, fv).reshape(bsz, seq, FOX_WIDTH)

    merged = jax.nn.sigmoid(g_a) * (y_mla @ w_branch_mla) + jax.nn.sigmoid(g_b) * (y_fox @ w_branch_fox)
    return merged @ w_out


def swiglu(h, w_gate, w_up, w_down):
    return (jax.nn.silu(h @ w_gate) * (h @ w_up)) @ w_down


def moe_swiglu(h, w_router, w_gate, w_up, w_down):
    logits = jnp.einsum('bsd,de->bse', h, w_router, preferred_element_type=jnp.float32)
    top_v, top_i = lax.top_k(logits, TOP_K)
    top_w = jax.nn.softmax(top_v, axis=-1)
    combine = jnp.sum(jax.nn.one_hot(top_i, N_EXPERTS, dtype=jnp.float32) * top_w[..., None],
                      axis=-2).astype(h.dtype)
    y = jnp.zeros_like(h)
    for e in range(N_EXPERTS):
        y = y + combine[..., e:e + 1] * swiglu(h, w_gate[e], w_up[e], w_down[e])
    return y


def setup_inputs(seed: int = 0) -> dict:
    key = jax.random.key(seed)
    ks = iter(jax.random.split(key, 40))

    def nrm(shape, scale):
        return jax.random.normal(next(ks), shape, jnp.float32) * scale

    def gain(shape):
        return 1.0 + nrm(shape, 0.02)

    x = nrm((BATCH, SEQ, D_MODEL), 1.0)
    c = nrm((BATCH, D_MODEL), 1.0)
    offsets = jax.random.randint(next(ks), (BATCH, 1), 0, 2048, dtype=jnp.int32)
    positions = offsets + jnp.arange(SEQ, dtype=jnp.int32)[None, :]
    return {
        "x": x,
        "c": c,
        "positions": positions,
        "ada_w": nrm((DEPTH, 2, D_MODEL, 3 * D_MODEL), 0.5 * D_MODEL ** -0.5),
        "ada_b": nrm((DEPTH, 2, 3 * D_MODEL), 0.02),
        "norm_mix_g": gain((DEPTH, D_MODEL)),
        "norm_ffn_g": gain((DEPTH, D_MODEL)),
        "w_in": nrm((DEPTH, D_MODEL, IN_COLS), D_MODEL ** -0.5),
        "b_forget": jax.random.uniform(next(ks), (DEPTH, FOX_HEADS), jnp.float32, 1.0, 4.0),
        "q_norm_g": gain((DEPTH, MLA_Q_LORA)),
        "w_uq": nrm((DEPTH, MLA_Q_LORA, MLA_HEADS * MLA_QK_DIM), MLA_Q_LORA ** -0.5),
        "kv_norm_g": gain((DEPTH, MLA_KV_LORA)),
        "w_ukv": nrm((DEPTH, MLA_KV_LORA, MLA_HEADS * (MLA_NOPE_DIM + MLA_V_DIM)), MLA_KV_LORA ** -0.5),
        "w_branch_mla": nrm((DEPTH, MLA_WIDTH, D_MODEL), MLA_WIDTH ** -0.5),
        "w_branch_fox": nrm((DEPTH, FOX_WIDTH, D_MODEL), FOX_WIDTH ** -0.5),
        "w_out": nrm((DEPTH, D_MODEL, D_MODEL), D_MODEL ** -0.5),
        "dense_w_gate": nrm((N_DENSE, D_MODEL, D_FF), D_MODEL ** -0.5),
        "dense_w_up": nrm((N_DENSE, D_MODEL, D_FF), D_MODEL ** -0.5),
        "dense_w_down": nrm((N_DENSE, D_FF, D_MODEL), D_FF ** -0.5),
        "moe_w_router": nrm((N_MOE, D_MODEL, N_EXPERTS), D_MODEL ** -0.5),
        "moe_w_gate": nrm((N_MOE, N_EXPERTS, D_MODEL, D_FF), D_MODEL ** -0.5),
        "moe_w_up": nrm((N_MOE, N_EXPERTS, D_MODEL, D_FF), D_MODEL ** -0.5),
        "moe_w_down": nrm((N_MOE, N_EXPERTS, D_FF, D_MODEL), D_FF ** -0.5),
        "final_norm_g": gain((D_MODEL,)),
    }


def reference(x, c, positions, ada_w, ada_b, norm_mix_g, norm_ffn_g, w_in, b_forget,
              q_norm_g, w_uq, kv_norm_g, w_ukv, w_branch_mla, w_branch_fox, w_out,
              dense_w_gate, dense_w_up, dense_w_down,
              moe_w_router, moe_w_gate, moe_w_up, moe_w_down, final_norm_g):
    inv_freq = ROPE_THETA ** (-jnp.arange(0, MLA_ROPE_DIM, 2, dtype=jnp.float32) / MLA_ROPE_DIM)
    ang = positions.astype(jnp.float32)[..., None] * inv_freq
    cos = jnp.cos(ang).astype(x.dtype)
    sin = jnp.sin(ang).astype(x.dtype)

    for l in range(DEPTH):
        shift, scale, gate = adaln(c, ada_w[l, 0], ada_b[l, 0])
        h = rmsnorm(x, norm_mix_g[l]) * (1.0 + scale) + shift
        x = x + gate * hybrid_mixer(h, cos, sin, w_in[l], b_forget[l], q_norm_g[l], w_uq[l],
                                    kv_norm_g[l], w_ukv[l], w_branch_mla[l], w_branch_fox[l], w_out[l])
        shift, scale, gate = adaln(c, ada_w[l, 1], ada_b[l, 1])
        h = rmsnorm(x, norm_ffn_g[l]) * (1.0 + scale) + shift
        if l % 2 == 0:
            i = l // 2
            f = swiglu(h, dense_w_gate[i], dense_w_up[i], dense_w_down[i])
        else:
            i = l // 2
            f = moe_swiglu(h, moe_w_router[i], moe_w_gate[i], moe_w_up[i], moe_w_down[i])
        x = x + gate * f
    return rmsnorm(x, final_norm_g)
```

```python
import contextlib
import numpy as np
import concourse.bass as bass
import concourse.mybir as mybir
from concourse.bass_utils import run_bass_kernel_spmd

F32 = mybir.dt.float32
BF16 = mybir.dt.bfloat16
I32 = mybir.dt.int32
AF = mybir.ActivationFunctionType
ALU = mybir.AluOpType

D = 1024
NH = 8
QL = 256
KVL = 128
ROPE = 32
DFF = 3584
NEXP = 8
EPS = 1e-6
MLA_SCALE = 96 ** -0.5
FOX_SCALE = 64 ** -0.5
NEG = -30000.0
CW1 = 6.28125
CW2 = float(2.0 * np.pi - 6.28125)
PI_LO = 3.1415925

CA_CQ = 0
CA_CKV = 256
CA_KR = 384
CA_FQ = 448
CA_FK = 960
CA_FV = 1472
CA_GA = 1984
CA_GB = 3008
CA_FL = 4032
CA = 4040
CA_PAD = 4096


class Cfg:
    def __init__(self, nseq=2, seq=4096, depth=4, ncores=8, debug=False):
        self.nseq, self.seq, self.depth, self.ncores, self.debug = nseq, seq, depth, ncores, debug
        self.kt = 4
        self.stop = None
        self.T = nseq * seq


class Sched:
    COMPUTE = ("pe", "act", "dve", "pool")

    def __init__(self, nc, n_dma_sems=44, n_sw=12, prefix=""):
        self.nc = nc
        self.prefix = prefix
        self.eng = {"pe": nc.tensor, "act": nc.scalar, "dve": nc.vector, "pool": nc.gpsimd, "sp": nc.sync}
        self.sems = {}
        self.cnt = {}
        for e in self.COMPUTE:
            self.sems[e] = nc.alloc_semaphore(prefix + "c_" + e)
            self.cnt[e] = 0
        self.dma_pool = {"hw": [], "sw": []}
        for kind, n in (("hw", n_dma_sems), ("sw", n_sw)):
            for i in range(n):
                k = "%s%d" % (kind, i)
                self.sems[k] = nc.alloc_semaphore(prefix + k)
                self.cnt[k] = 0
                self.dma_pool[kind].append(k)
        self.dma_map = {}
        self.dma_next = {"hw": 0, "sw": 0}
        self.waited = {}
        self.res = {}
        self.n_wait = 0
        self.n_ins = 0

    def _wait(self, eng, dep):
        semk, val, deng = dep
        if semk == "pe" and eng == "pe":
            return
        key = (eng, semk)
        if self.waited.get(key, 0) >= val:
            return
        self.eng[eng].wait_ge(self.sems[semk], val)
        self.waited[key] = val
        self.n_wait += 1

    def _deps(self, eng, r, w):
        for k in r:
            st = self.res.get(k)
            if st is not None and st[0] is not None:
                self._wait(eng, st[0])
        for k in w:
            st = self.res.get(k)
            if st is not None:
                if st[0] is not None:
                    self._wait(eng, st[0])
                for semk, (val, deng) in st[1].items():
                    self._wait(eng, (semk, val, deng))

    def _record(self, comp, r, w):
        for k in r:
            st = self.res.get(k)
            if st is None:
                st = [None, {}]
                self.res[k] = st
            old = st[1].get(comp[0])
            if old is None or old[0] < comp[1]:
                st[1][comp[0]] = (comp[1], comp[2])
        for k in w:
            self.res[k] = [comp, {}]

    def op(self, eng, fn, r=(), w=()):
        self._deps(eng, r, w)
        ins = fn()
        self.cnt[eng] += 1
        ins.then_inc(self.sems[eng], 1)
        self._record((eng, self.cnt[eng], eng), r, w)
        self.n_ins += 1
        return ins

    def dma(self, eng, out, in_, r=(), w=(), key=None, **kw):
        if key is None:
            key = w[0] if (len(w) and isinstance(w[0], tuple) and w[0][0] != "dram") else r[0]
        r = [k for k in r if k[0] != "dram"]
        w = [k for k in w if k[0] != "dram"]
        kind = "sw" if eng == "pool" else "hw"
        key = (kind, key)
        semk = self.dma_map.get(key)
        if semk is None:
            assert self.dma_next[kind] < len(self.dma_pool[kind]), "out of DMA semaphores"
            semk = self.dma_pool[kind][self.dma_next[kind]]
            self.dma_next[kind] += 1
            self.dma_map[key] = semk
        self._deps(eng, r, w)
        ins = self.eng[eng].dma_start(out=out, in_=in_, **kw)
        self.cnt[semk] += 16
        ins.then_inc(self.sems[semk], 16)
        self._record((semk, self.cnt[semk], "dma"), r, w)
        self.n_ins += 1
        return ins

    def dma_indirect(self, eng, out, out_offset, in_, in_offset, r=(), w=(), key=None, bound=None):
        kind = "sw"
        r = [k for k in r if k[0] != "dram"]
        w = [k for k in w if k[0] != "dram"]
        key = (kind, key)
        semk = self.dma_map.get(key)
        if semk is None:
            assert self.dma_next[kind] < len(self.dma_pool[kind]), "out of DMA semaphores"
            semk = self.dma_pool[kind][self.dma_next[kind]]
            self.dma_next[kind] += 1
            self.dma_map[key] = semk
        self._deps(eng, r, w)
        ins = self.nc.gpsimd.indirect_dma_start(out=out, out_offset=out_offset, in_=in_, in_offset=in_offset,
                                                bounds_check=bound, oob_is_err=False if bound is not None else True)
        self.cnt[semk] += 16
        ins.then_inc(self.sems[semk], 16)
        self._record((semk, self.cnt[semk], "dma"), r, w)
        self.n_ins += 1
        return ins

    def barrier(self, engines=("pe", "act", "dve", "pool", "sp")):
        for e in engines:
            for semk, c in self.cnt.items():
                if c > 0:
                    self._wait(e, (semk, c, "x"))
        self.res = {}
        self.dma_map = {}
        self.dma_next = {"hw": 0, "sw": 0}


    def finish_local_block(self):
        self.barrier()
        self.nc.all_engine_barrier()
        for semk, c in self.cnt.items():
            if c > 0:
                owner = semk if semk in self.COMPUTE else "sp"
                self.eng[owner].sem_clear(self.sems[semk])
        self.nc.all_engine_barrier()
        for k in self.cnt:
            self.cnt[k] = 0
        self.waited = {}
        self.res = {}


class _Stop(Exception):
    pass


def build_program(cfg):
    try:
        return _build_program(cfg)
    except _Stop as e:
        return e.args


def _build_program(cfg):
    nc = bass.Bass("TRN2", target_bir_lowering=False)
    S = Sched(nc)
    S2 = Sched(nc, n_dma_sems=10, n_sw=0, prefix="L_")
    NSEQ, SEQ, L, T = cfg.nseq, cfg.seq, cfg.depth, cfg.T
    NT = SEQ // 512
    n_dense, n_moe = (L + 1) // 2, L // 2
    dbg = cfg.debug

    uid = [0]

    def SB(name, shape, dt):
        uid[0] += 1
        return nc.sbuf_tensor("%s_u%d" % (name, uid[0]), shape, dt)

    def PS(name, shape, dt):
        uid[0] += 1
        return nc.psum_tensor("%s_u%d" % (name, uid[0]), shape, dt)

    def dram(name, shape, dt, kind="Internal"):
        return nc.dram_tensor(name, list(shape), dt, kind=kind).ap()

    scr_kind = "ExternalOutput" if dbg else "Internal"
    x_in = dram("x", [T, D], F32, "ExternalInput")
    cT_in = dram("cT", [D, NSEQ], F32, "ExternalInput")
    pos_in = dram("pos", [NSEQ, SEQ], I32, "ExternalInput")
    ada_w = dram("ada_w", [L, 2, D, 3 * D], F32, "ExternalInput")
    ada_b = dram("ada_b", [L, 2, 3 * D], F32, "ExternalInput")
    g_mix = dram("norm_mix_g", [L, D], F32, "ExternalInput")
    g_ffn = dram("norm_ffn_g", [L, D], F32, "ExternalInput")
    wa_in = dram("wa", [L, D, CA], F32, "ExternalInput")
    bfg_in = dram("b_forget", [L, NH], F32, "ExternalInput")
    gq_in = dram("q_norm_gT", [128, L * 2], F32, "ExternalInput")
    wq_in = dram("wq", [L, QL, NH * 128], F32, "ExternalInput")
    gkv_in = dram("kv_norm_gT", [128, L], F32, "ExternalInput")
    wkv_in = dram("wkv", [L, KVL, 1024], F32, "ExternalInput")
    wbm_in = dram("w_branch_mla", [L, 512, D], F32, "ExternalInput")
    wbf_in = dram("w_branch_fox", [L, 512, D], F32, "ExternalInput")
    wo_in = dram("w_out", [L, D, D], F32, "ExternalInput")
    dwg_in = dram("dense_w_gate", [n_dense, D, DFF], F32, "ExternalInput")
    dwu_in = dram("dense_w_up", [n_dense, D, DFF], F32, "ExternalInput")
    dwd_in = dram("dense_w_down", [n_dense, DFF, D], F32, "ExternalInput")
    n_moe_a = max(n_moe, 1)
    wr_in = dram("moe_w_routerT", [n_moe_a, NEXP, D], F32, "ExternalInput")
    if n_moe > 0:
        mwg_in = dram("moe_w_gate", [n_moe_a, NEXP, D, DFF], F32, "ExternalInput")
        mwu_in = dram("moe_w_up", [n_moe_a, NEXP, D, DFF], F32, "ExternalInput")
        mwd_in = dram("moe_w_down", [n_moe_a, NEXP, DFF, D], F32, "ExternalInput")
    gfin_in = dram("final_norm_g", [D], F32, "ExternalInput")
    cst_in = dram("consts", [128, 8], F32, "ExternalInput")
    out_d = dram("out", [T, D], F32, "ExternalOutput")
    xs = dram("xs", [T, D], F32, scr_kind)
    tabs = dram("tabs", [NSEQ, 128, SEQ], F32, scr_kind)
    modrows = dram("modrows", [L, 2, NSEQ, 3, D], F32, scr_kind)
    kropeT = dram("kropeT", [NSEQ, 32, SEQ], BF16, scr_kind)
    knopeT = dram("knopeT", [NSEQ, 512, SEQ], BF16, scr_kind)
    qmT = dram("qmT", [NSEQ, NH, 96, SEQ], BF16, scr_kind)
    vmla = dram("vmla", [NSEQ, SEQ, 1024], BF16, scr_kind)
    fqT = dram("fqT", [NSEQ, 512, SEQ], BF16, scr_kind)
    fkT = dram("fkT", [NSEQ, 512, SEQ], BF16, scr_kind)
    vfox = dram("vfox", [NSEQ, SEQ, 1024], BF16, scr_kind)
    L3 = dram("L3", [NSEQ, NH, 3, SEQ], BF16, scr_kind)
    nL3 = dram("nL3", [NSEQ, NH, 3, SEQ], BF16, scr_kind)
    sgT = dram("sgT", [NSEQ, 2048, SEQ], BF16, scr_kind)
    yT = dram("yT", [NSEQ, 1024, SEQ], BF16, scr_kind)
    h2T = dram("h2T", [D, T], BF16, scr_kind)
    comb = dram("comb", [T, NEXP], F32, scr_kind)
    CAPT = T // 512
    CAPR = CAPT * 512
    KT = min(CAPT, cfg.kt)
    if n_moe > 0:
        hg = dram("hg", [NEXP * CAPR, D], BF16, "Internal")
        og = dram("og", [NEXP * CAPR, D], F32, "Internal")
        hgT = dram("hgT", [D, NEXP * CAPR], BF16, "Internal")
        gidx = dram("gidx", [T, 2], I32, scr_kind)
        gwd = dram("gwd", [T, 2], F32, scr_kind)
        cnts = dram("cnts", [1, NEXP], I32, scr_kind)
        cnt_reg = nc.alloc_registers("cnt_e", mybir.ALL_ENGINES)

    def dkey(name, *idx):
        return ("dram", name) + tuple(idx)

    def stop_here(tag):
        if getattr(cfg, "stop", None) == tag:
            S.barrier()
            raise _Stop(nc, S)

    cst = nc.alloc_sbuf_tensor("cst", [128, 8], F32)
    ident = nc.alloc_sbuf_tensor("ident", [128, 128], BF16)
    ones_bf = nc.alloc_sbuf_tensor("ones_bf", [128, 128], BF16)
    maskT = nc.alloc_sbuf_tensor("maskT", [128, 128], BF16)
    zero_bf = nc.alloc_sbuf_tensor("zero_bf", [128, 128], BF16)
    ones_f = nc.alloc_sbuf_tensor("ones_f", [128, 512], F32)

    epsc = nc.alloc_sbuf_tensor("epsc", [128, 1], F32)
    S.op("pool", lambda: nc.gpsimd.memset(epsc[:], EPS), w=[("epsc",)])
    cnt_sb = nc.alloc_sbuf_tensor("cnt_sb", [1, NEXP], I32)
    ustr = nc.alloc_sbuf_tensor("ustr", [128, 128], F32)
    onesq = nc.alloc_sbuf_tensor("onesq", [128, 128], F32)
    eoff = nc.alloc_sbuf_tensor("eoff", [128, NEXP], F32)
    S.op("pool", lambda: nc.gpsimd.memset(onesq[:], 1.0), w=[("onesq",)])
    S.op("pool", lambda: nc.gpsimd.affine_select(out=ustr[:], in_=onesq[:], pattern=[[1, 128]],
                                                 compare_op=ALU.is_gt, fill=0.0, base=0, channel_multiplier=-1),
         r=[("onesq",)], w=[("ustr",)])
    for e in range(NEXP):
        S.op("pool", lambda e=e: nc.gpsimd.memset(eoff[:, e:e + 1], float(e * (T // 512) * 512)), w=[("eoff",)])
    S.dma("sp", cst[:], cst_in[:, :], r=[dkey("cst")], w=[("cst",)])
    S.op("pool", lambda: nc.gpsimd.memset(zero_bf[:], 0.0), w=[("zero_bf",)])
    S.op("pool", lambda: nc.gpsimd.memset(ones_bf[:], 1.0), w=[("ones_bf",)])
    S.op("pool", lambda: nc.gpsimd.memset(ones_f[:], 1.0), w=[("ones_f",)])
    S.op("pool", lambda: nc.gpsimd.affine_select(out=ident[:], in_=zero_bf[:], pattern=[[-1, 128]],
                                                 compare_op=ALU.not_equal, fill=1.0, base=0,
                                                 channel_multiplier=1),
         r=[("zero_bf",)], w=[("ident",)])
    S.op("pool", lambda: nc.gpsimd.affine_select(out=maskT[:], in_=zero_bf[:], pattern=[[1, 128]],
                                                 compare_op=ALU.is_ge, fill=NEG, base=0,
                                                 channel_multiplier=-1),
         r=[("zero_bf",)], w=[("maskT",)])
    S.barrier()

    if n_moe > 0:
        ztf = nc.alloc_sbuf_tensor("zfill_f", [128, D], F32)
        S.op("pool", lambda: nc.gpsimd.memset(ztf[:], 0.0), w=[("zfill_f",)])
        hg2 = hg.rearrange("(n p r) d -> n p (r d)", p=128, r=2)
        for i in range(NEXP * CAPR // 256):
            S.dma("sp", hg2[i], ztf[:].bitcast(BF16), r=[("zfill_f",)], w=[dkey("hg")])
        og1 = og.rearrange("(n p) d -> n p d", p=128)
        for i in range(NEXP * CAPR // 128):
            S.dma("act", og1[i], ztf[:], r=[("zfill_f",)], w=[dkey("og")])

    def load_w(stack, name, src2d, K, N, eng="pool", npad=None):
        kc = K // 128
        t = stack.enter_context(SB(name, [128, kc, npad or N], BF16))
        for k in range(kc):
            S.dma(eng, t[:, k, 0:N], src2d[k * 128:(k + 1) * 128, :], r=[dkey(name)], w=[(name,)], key=(name,))
        return t

    def rms_rstd(ss, rstd, n):
        S.op("act", lambda: nc.scalar.activation(out=rstd[0], in_=ss[0], func=AF.Ln, bias=epsc[0:rstd[0].shape[0], 0:1],
                                                 scale=1.0 / n), r=[ss[1], ("epsc",)], w=[rstd[1]])
        S.op("act", lambda: nc.scalar.activation(out=rstd[0], in_=rstd[0], func=AF.Exp, scale=-0.5),
             r=[rstd[1]], w=[rstd[1]])

    with contextlib.ExitStack() as st:
        posi = st.enter_context(SB("posi", [128, SEQ], I32))
        ang = st.enter_context(SB("ang", [128, SEQ], F32))
        tb = st.enter_context(SB("tb", [128, SEQ], F32))
        for b in range(NSEQ):
            S.dma("sp", posi[:], pos_in[b, :].partition_broadcast(128), r=[dkey("pos")], w=[("posi",)])
            S.op("dve", lambda: nc.vector.tensor_copy(out=ang[:], in_=posi[:]), r=[("posi",)], w=[("ang",)])
            S.op("dve", lambda: nc.vector.tensor_scalar(out=ang[:], in0=ang[:], scalar1=cst[:, 0:1], scalar2=None,
                                                        op0=ALU.mult), r=[("ang",), ("cst",)], w=[("ang",)])
            S.op("dve", lambda: nc.vector.tensor_scalar(out=tb[:], in0=ang[:], scalar1=float(1.0 / (2.0 * np.pi)), scalar2=None,
                                                        op0=ALU.mult), r=[("ang",)], w=[("tb",)])
            S.op("dve", lambda: nc.vector.tensor_copy(out=posi[:], in_=tb[:]), r=[("tb",)], w=[("posi",)])
            S.op("dve", lambda: nc.vector.tensor_copy(out=tb[:], in_=posi[:]), r=[("posi",)], w=[("tb",)])
            S.op("dve", lambda: nc.vector.scalar_tensor_tensor(out=ang[:], in0=tb[:], scalar=-CW1, in1=ang[:],
                                                               op0=ALU.mult, op1=ALU.add), r=[("tb",), ("ang",)], w=[("ang",)])
            S.op("dve", lambda: nc.vector.scalar_tensor_tensor(out=ang[:], in0=tb[:], scalar=-CW2, in1=ang[:],
                                                               op0=ALU.mult, op1=ALU.add), r=[("tb",), ("ang",)], w=[("ang",)])
            S.op("dve", lambda: nc.vector.tensor_scalar(out=ang[:], in0=ang[:], scalar1=-PI_LO, scalar2=PI_LO,
                                                        op0=ALU.max, op1=ALU.min), r=[("ang",)], w=[("ang",)])
            S.op("dve", lambda: nc.vector.scalar_tensor_tensor(out=ang[0:64, :], in0=ang[0:64, :], scalar=-1.0,
                                                               in1=ang[0:64, :], op0=ALU.mult, op1=ALU.max),
                 r=[("ang",)], w=[("ang",)])
            S.op("act", lambda: nc.scalar.activation(out=tb[:], in_=ang[:], func=AF.Sin, bias=cst[:, 3:4],
                                                     scale=cst[:, 2:3]), r=[("ang",), ("cst",)], w=[("tb",)])
            S.op("dve", lambda: nc.vector.tensor_scalar(out=tb[:], in0=tb[:], scalar1=cst[:, 4:5], scalar2=None,
                                                        op0=ALU.mult), r=[("tb",), ("cst",)], w=[("tb",)])
            S.dma("sp", tabs[b, :, :], tb[:], r=[("tb",)], w=[dkey("tabs", b)])
        S.barrier()
    stop_here("p0")

    with contextlib.ExitStack() as st:
        cTs = st.enter_context(SB("cTs", [128, 8, NSEQ], F32))
        scT = st.enter_context(SB("scT", [128, 8, NSEQ], F32))
        wch = [st.enter_context(SB("wch%d" % i, [128, 1536], F32)) for i in range(4)]
        mod = st.enter_context(SB("mod", [NSEQ, 3 * D], F32))
        bia = st.enter_context(SB("bia", [NSEQ, 3 * D], F32))
        gbc = st.enter_context(SB("gbc", [NSEQ, D], F32))
        arow = st.enter_context(SB("arow", [NSEQ, D], F32))
        pmod = [st.enter_context(PS("pmod%d" % i, [NSEQ, 512], F32)) for i in range(3)]
        with nc.allow_non_contiguous_dma(reason="tiny transposed conditioning vector"):
            S.dma("sp", cTs[:], cT_in.rearrange("(k p) b -> p k b", p=128), r=[dkey("cT")], w=[("cTs",)])
        S.op("act", lambda: nc.scalar.activation(out=scT[:], in_=cTs[:], func=AF.Silu), r=[("cTs",)], w=[("scT",)])
        wi = 0
        for l in range(L):
            for sub in range(2):
                S.dma("sp", bia[:], ada_b[l, sub, :].partition_broadcast(NSEQ), r=[dkey("ada_b")], w=[("bia",)])
                gsrc = g_mix if sub == 0 else g_ffn
                S.dma("sp", gbc[:], gsrc[l, :].partition_broadcast(NSEQ), r=[dkey("g")], w=[("gbc",)])
                for half in range(2):
                    for kc in range(8):
                        wt = wch[wi % 4]
                        wk = ("wch", wi % 4)
                        wi += 1
                        S.dma("sp", wt[:], ada_w[l, sub, kc * 128:(kc + 1) * 128, half * 1536:(half + 1) * 1536],
                              r=[dkey("ada_w")], w=[wk])
                        for j in range(3):
                            S.op("pe", lambda j=j, wt=wt, kc=kc: nc.tensor.matmul(
                                pmod[j][:], lhsT=scT[:, kc, :], rhs=wt[:, j * 512:(j + 1) * 512],
                                start=(kc == 0), stop=(kc == 7)), r=[wk, ("scT",)], w=[("pmod", j)])
                    for j in range(3):
                        c0 = half * 1536 + j * 512
                        S.op("dve", lambda j=j, c0=c0: nc.vector.tensor_tensor(
                            out=mod[:, c0:c0 + 512], in0=pmod[j][:], in1=bia[:, c0:c0 + 512], op=ALU.add),
                            r=[("pmod", j), ("bia",)], w=[("mod",)])
                S.op("dve", lambda: nc.vector.scalar_tensor_tensor(out=arow[:], in0=mod[:, D:2 * D], scalar=1.0,
                                                                   in1=gbc[:], op0=ALU.add, op1=ALU.mult),
                     r=[("mod",), ("gbc",)], w=[("arow",)])
                S.dma("sp", modrows[l, sub, :, 0, :], arow[:], r=[("arow",)], w=[dkey("modrows", l, sub)])
                S.dma("sp", modrows[l, sub, :, 1, :], mod[:, 0:D], r=[("mod",)], w=[dkey("modrows", l, sub)])
                S.dma("sp", modrows[l, sub, :, 2, :], mod[:, 2 * D:3 * D], r=[("mod",)], w=[dkey("modrows", l, sub)])
        S.barrier()
    stop_here("p1")

    def norm_block(xb, xk, ss, ssk, rstd, rk, junk, jk, tmp, tk, hb, hk, abc, bbc, bck):
        S.op("act", lambda: nc.scalar.activation(out=junk, in_=xb, func=AF.Square, accum_out=ss),
             r=[xk], w=[jk, ssk])
        rms_rstd((ss, ssk), (rstd, rk), D)
        S.op("dve", lambda: nc.vector.scalar_tensor_tensor(out=tmp, in0=xb, scalar=rstd, in1=abc,
                                                           op0=ALU.mult, op1=ALU.mult),
             r=[xk, rk, bck], w=[tk])
        if bbc is None:
            S.op("pool", lambda: nc.gpsimd.tensor_copy(out=hb, in_=tmp), r=[tk], w=[hk])
        else:
            S.op("pool", lambda: nc.gpsimd.tensor_tensor(out=hb, in0=tmp, in1=bbc, op=ALU.add),
                 r=[tk, bck], w=[hk])

    for l in range(L):
        xsrc = x_in if l == 0 else xs
        xsrc_key = "x_in" if l == 0 else "xs"

        with contextlib.ExitStack() as st:
            WA = load_w(st, "WA", wa_in[l], D, CA, npad=CA_PAD)
            WQ = load_w(st, "WQ", wq_in[l], QL, NH * 128)
            WKV = load_w(st, "WKV", wkv_in[l], KVL, 1024)
            gq = st.enter_context(SB("gq", [128, 2 * L], F32))
            gkv = st.enter_context(SB("gkv", [128, L], F32))
            nbf = st.enter_context(SB("nbf", [NH, 1], F32))
            S.dma("sp", gq[:], gq_in[:, :], r=[dkey("gq")], w=[("gq",)])
            S.dma("sp", gkv[:], gkv_in[:, :], r=[dkey("gkv")], w=[("gkv",)])
            with nc.allow_non_contiguous_dma(reason="8-element bias column"):
                S.dma("sp", nbf[:], bfg_in[l, :].rearrange("(h o) -> h o", o=1), r=[dkey("bf")], w=[("nbf",)])
            S.op("dve", lambda: nc.vector.tensor_scalar(out=nbf[:], in0=nbf[:], scalar1=-1.0, scalar2=None,
                                                        op0=ALU.mult), r=[("nbf",)], w=[("nbf",)])
            NXB = 5
            xbuf = [st.enter_context(SB("xb%d" % i, [128, D], F32)) for i in range(NXB)]
            junk = st.enter_context(SB("junk", [128, D], BF16))
            tmpf = [st.enter_context(SB("tmpf%d" % i, [128, D], F32)) for i in range(2)]
            hbuf = [st.enter_context(SB("hb%d" % i, [128, D], BF16)) for i in range(2)]
            ssb = [st.enter_context(SB("ss%d" % i, [128, 1], F32)) for i in range(4)]
            rsb = [st.enter_context(SB("rs%d" % i, [128, 1], F32)) for i in range(4)]
            hT = [st.enter_context(SB("hT%d" % i, [128, 8, 512], BF16)) for i in range(2)]
            abc = st.enter_context(SB("abc", [128, D], F32))
            bbc = st.enter_context(SB("bbc", [128, D], F32))
            tabt = [st.enter_context(SB("tabt%d" % i, [128, 512], F32)) for i in range(2)]
            craw = st.enter_context(SB("craw", [128, 3, 512], F32))
            sq = st.enter_context(SB("sq", [128, 3, 512], BF16))
            rq = st.enter_context(SB("rq", [128, 2, 512], F32))
            cqn = st.enter_context(SB("cqn", [128, 2, 512], BF16))
            ckvn = st.enter_context(SB("ckvn", [128, 512], BF16))
            rt = [st.enter_context(SB("rt%d" % i, [32, 512], F32)) for i in range(4)]
            NST = 8
            stg = [st.enter_context(SB("stg%d" % i, [128, 512], BF16)) for i in range(NST)]
            vst = [st.enter_context(SB("vst%d" % i, [128, NH, 128], BF16)) for i in range(4)]
            fl = [st.enter_context(SB("fl%d" % i, [NH, 512], F32)) for i in range(3)]
            Lt = [st.enter_context(SB("Lt%d" % i, [NH, 512], F32)) for i in range(2)]
            l3 = st.enter_context(SB("l3", [NH, 6, 512], BF16))
            pst = st.enter_context(PS("pst", [128, D], BF16))
            pp = [st.enter_context(PS("pp%d" % i, [128, 512], F32)) for i in range(7)]
            for i in range(4):
                S.op("pool", lambda i=i: nc.gpsimd.memset(vst[i][:, :, 64:128], 1.0), w=[("vst", i)])
            cnt = {"x": 0, "pp": 0, "stg": 0, "vst": 0, "ss": 0, "rt": 0}

            def nxt(name, n):
                v = cnt[name] % n
                cnt[name] += 1
                return v

            def stage1(b, tt, hTi):
                for blk in range(4):
                    r0 = b * SEQ + tt * 512 + blk * 128
                    xi = nxt("x", NXB)
                    S.dma("sp", xbuf[xi][:], xsrc[r0:r0 + 128, :], r=[dkey(xsrc_key)], w=[("xb", xi)])
                    si = nxt("ss", 4)
                    hi = blk % 2
                    norm_block(xbuf[xi][:], ("xb", xi), ssb[si][:], ("ss", si), rsb[si][:], ("rs", si),
                               junk[:], ("junk",), tmpf[hi][:], ("tmpf", hi), hbuf[hi][:], ("hb", hi),
                               abc[:], bbc[:], ("abc",))
                    for kc in range(8):
                        S.op("pe", lambda kc=kc, hi=hi: nc.tensor.transpose(
                            pst[:, kc * 128:(kc + 1) * 128], hbuf[hi][:, kc * 128:(kc + 1) * 128], ident[:]),
                            r=[("hb", hi), ("ident",)], w=[("pst",)])
                    S.op("act", lambda blk=blk: nc.scalar.copy(
                        out=hT[hTi][:, :, blk * 128:(blk + 1) * 128],
                        in_=pst[:].rearrange("p (k t) -> p k t", k=8)),
                        r=[("pst",)], w=[("hT", hTi)])

            def group(c0, M, hTi):
                pi = nxt("pp", 7)
                for kc in range(8):
                    S.op("pe", lambda kc=kc: nc.tensor.matmul(pp[pi][0:M, :], lhsT=WA[:, kc, c0:c0 + M],
                                                              rhs=hT[hTi][:, kc, :], start=(kc == 0), stop=(kc == 7)),
                         r=[("WA",), ("hT", hTi)], w=[("pp", pi)])
                return pi

            def store_stage(si, nrows, dst, dk):
                S.dma("sp", dst, stg[si][0:nrows, :], r=[("stg", si)], w=[dk])

            def stage2(b, tt, hTi, carry):
                t0 = tt * 512
                tsl = slice(t0, t0 + 512)
                ti = tt % 2
                stop_here("A%ds2" % l)
                S.dma("sp", tabt[ti][:], tabs[b, :, tsl], r=[dkey("tabs", b)], w=[("tabt", ti)])
                tab = tabt[ti]
                tabk = ("tabt", ti)
                stop_here("A%dt" % l)
                for j in range(3):
                    pi = group(CA_CQ + j * 128, 128, hTi)
                    stop_here("A%dm" % l)
                    S.op("act", lambda j=j, pi=pi: nc.scalar.copy(out=craw[:, j, :], in_=pp[pi][:]),
                         r=[("pp", pi)], w=[("craw", j)])
                    stop_here("A%da" % l)
                    S.op("dve", lambda j=j: nc.vector.tensor_tensor(out=sq[:, j, :], in0=craw[:, j, :], in1=craw[:, j, :],
                                                                    op=ALU.mult), r=[("craw", j)], w=[("sq", j)])
                    stop_here("A%dd" % l)
                    if j == 1:
                        stop_here("A%dd1" % l)
                stop_here("A%dg" % l)
                pq = nxt("pp", 7)
                S.op("pe", lambda: nc.tensor.matmul(pp[pq][:], lhsT=ones_bf[:], rhs=sq[:, 0, :], start=True, stop=False),
                     r=[("sq", 0), ("ones_bf",)], w=[("pp", pq)])
                S.op("pe", lambda: nc.tensor.matmul(pp[pq][:], lhsT=ones_bf[:], rhs=sq[:, 1, :], start=False, stop=True),
                     r=[("sq", 1), ("ones_bf",)], w=[("pp", pq)])
                rms_rstd((pp[pq][:], ("pp", pq)), (rq[:, 0, :], ("rq", 0)), QL)
                pk = nxt("pp", 7)
                S.op("pe", lambda: nc.tensor.matmul(pp[pk][:], lhsT=ones_bf[:], rhs=sq[:, 2, :], start=True, stop=True),
                     r=[("sq", 2), ("ones_bf",)], w=[("pp", pk)])
                rms_rstd((pp[pk][:], ("pp", pk)), (rq[:, 1, :], ("rq", 1)), KVL)
                for j in range(2):
                    S.op("dve", lambda j=j: nc.vector.scalar_tensor_tensor(
                        out=cqn[:, j, :], in0=craw[:, j, :], scalar=gq[:, 2 * l + j:2 * l + j + 1], in1=rq[:, 0, :],
                        op0=ALU.mult, op1=ALU.mult), r=[("craw", j), ("gq",), ("rq", 0)], w=[("cqn", j)])
                S.op("dve", lambda: nc.vector.scalar_tensor_tensor(
                    out=ckvn[:], in0=craw[:, 2, :], scalar=gkv[:, l:l + 1], in1=rq[:, 1, :],
                    op0=ALU.mult, op1=ALU.mult), r=[("craw", 2), ("gkv",), ("rq", 1)], w=[("ckvn",)])

                def rope_evict(pi, dst, dk, cos_rows, sin_rows, pbase=0):
                    r1, r2 = nxt("rt", 4), nxt("rt", 4)
                    S.op("dve", lambda: nc.vector.tensor_tensor(out=rt[r1][:], in0=pp[pi][pbase:pbase + 32, :],
                                                                in1=tab[cos_rows, :], op=ALU.mult),
                         r=[("pp", pi), tabk], w=[("rt", r1)])
                    S.op("dve", lambda: nc.vector.tensor_tensor(out=rt[r2][:], in0=pp[pi][pbase + 32:pbase + 64, :],
                                                                in1=tab[sin_rows, :], op=ALU.mult),
                         r=[("pp", pi), tabk], w=[("rt", r2)])
                    S.op("pool", lambda: nc.gpsimd.tensor_tensor(out=dst, in0=rt[r1][:], in1=rt[r2][:], op=ALU.add),
                         r=[("rt", r1), ("rt", r2)], w=[dk])

                stop_here("A%d%s" % (l, "x1"))
                pi = group(CA_KR, 64, hTi)
                si = nxt("stg", NST)
                rope_evict(pi, stg[si][0:32, :], ("stg", si), slice(0, 32), slice(64, 96))
                store_stage(si, 32, kropeT[b, :, tsl], dkey("kropeT", b))
                stop_here("A%d%s" % (l, "x2"))
                for h in range(NH):
                    pi = nxt("pp", 7)
                    for kc in range(2):
                        S.op("pe", lambda kc=kc, h=h: nc.tensor.matmul(
                            pp[pi][:], lhsT=WQ[:, kc, h * 128:(h + 1) * 128], rhs=cqn[:, kc, :],
                            start=(kc == 0), stop=(kc == 1)), r=[("WQ",), ("cqn", kc)], w=[("pp", pi)])
                    si = nxt("stg", NST)
                    S.op("act", lambda pi=pi, si=si: nc.scalar.mul(out=stg[si][0:64, :], in_=pp[pi][0:64, :],
                                                                   mul=MLA_SCALE),
                         r=[("pp", pi)], w=[("stg", si)])
                    rope_evict(pi, stg[si][64:96, :], ("stg", si), slice(32, 64), slice(96, 128), 64)
                    store_stage(si, 96, qmT[b, h, :, tsl], dkey("qmT", b, h))
                stop_here("A%d%s" % (l, "x3"))
                for g in range(4):
                    pi = nxt("pp", 7)
                    S.op("pe", lambda g=g: nc.tensor.matmul(pp[pi][:], lhsT=WKV[:, 0, g * 128:(g + 1) * 128],
                                                            rhs=ckvn[:], start=True, stop=True),
                         r=[("WKV",), ("ckvn",)], w=[("pp", pi)])
                    si = nxt("stg", NST)
                    S.op("dve", lambda pi=pi, si=si: nc.vector.tensor_copy(out=stg[si][:], in_=pp[pi][:]),
                         r=[("pp", pi)], w=[("stg", si)])
                    store_stage(si, 128, knopeT[b, g * 128:(g + 1) * 128, tsl], dkey("knopeT", b, g))
                stop_here("A%d%s" % (l, "x4"))
                for blk in range(4):
                    pi = nxt("pp", 7)
                    S.op("pe", lambda blk=blk: nc.tensor.matmul(pp[pi][:], lhsT=ckvn[:, blk * 128:(blk + 1) * 128],
                                                                rhs=WKV[:, 0, 512:1024], start=True, stop=True),
                         r=[("WKV",), ("ckvn",)], w=[("pp", pi)])
                    vi = nxt("vst", 4)
                    S.op("act", lambda pi=pi, vi=vi: nc.scalar.copy(
                        out=vst[vi][:, :, 0:64], in_=pp[pi][:].rearrange("p (h d) -> p h d", h=NH)),
                        r=[("pp", pi)], w=[("vst", vi)])
                    r0 = t0 + blk * 128
                    S.dma("sp", vmla[b, r0:r0 + 128, :], vst[vi][:].rearrange("p h d -> p (h d)"),
                          r=[("vst", vi)], w=[dkey("vmla", b)])
                stop_here("A%d%s" % (l, "x5"))
                for g in range(4):
                    pi = group(CA_FQ + g * 128, 128, hTi)
                    si = nxt("stg", NST)
                    S.op("act", lambda pi=pi, si=si: nc.scalar.mul(out=stg[si][:], in_=pp[pi][:], mul=FOX_SCALE),
                         r=[("pp", pi)], w=[("stg", si)])
                    store_stage(si, 128, fqT[b, g * 128:(g + 1) * 128, tsl], dkey("fqT", b, g))
                for g in range(4):
                    pi = group(CA_FK + g * 128, 128, hTi)
                    si = nxt("stg", NST)
                    S.op("dve", lambda pi=pi, si=si: nc.vector.tensor_copy(out=stg[si][:], in_=pp[pi][:]),
                         r=[("pp", pi)], w=[("stg", si)])
                    store_stage(si, 128, fkT[b, g * 128:(g + 1) * 128, tsl], dkey("fkT", b, g))
                stop_here("A%d%s" % (l, "x6"))
                for blk in range(4):
                    pi = nxt("pp", 7)
                    for kc in range(8):
                        S.op("pe", lambda kc=kc, blk=blk: nc.tensor.matmul(
                            pp[pi][:], lhsT=hT[hTi][:, kc, blk * 128:(blk + 1) * 128],
                            rhs=WA[:, kc, CA_FV:CA_FV + 512], start=(kc == 0), stop=(kc == 7)),
                            r=[("WA",), ("hT", hTi)], w=[("pp", pi)])
                    vi = nxt("vst", 4)
                    S.op("act", lambda pi=pi, vi=vi: nc.scalar.copy(
                        out=vst[vi][:, :, 0:64], in_=pp[pi][:].rearrange("p (h d) -> p h d", h=NH)),
                        r=[("pp", pi)], w=[("vst", vi)])
                    r0 = t0 + blk * 128
                    S.dma("sp", vfox[b, r0:r0 + 128, :], vst[vi][:].rearrange("p h d -> p (h d)"),
                          r=[("vst", vi)], w=[dkey("vfox", b)])
                stop_here("A%d%s" % (l, "x7"))
                pi = group(CA_FL, NH, hTi)
                S.op("act", lambda: nc.scalar.activation(out=fl[0][:], in_=pp[pi][0:NH, :], func=AF.Exp,
                                                         bias=nbf[:, 0:1], scale=-1.0),
                     r=[("pp", pi), ("nbf",)], w=[("fl", 0)])
                S.op("act", lambda: nc.scalar.activation(out=fl[1][:], in_=fl[0][:], func=AF.Ln, bias=1.0, scale=1.0),
                     r=[("fl", 0)], w=[("fl", 1)])
                li = tt % 2
                init = 0.0 if carry is None else carry
                rr = [("fl", 1), ("ones_f",)] + ([("Lt", 1 - li)] if carry is not None else [])
                S.op("dve", lambda: nc.vector.tensor_tensor_scan(out=Lt[li][:], data0=ones_f[0:NH, :], data1=fl[1][:],
                                                                 initial=init, op0=ALU.mult, op1=ALU.subtract),
                     r=rr, w=[("Lt", li)])
                S.op("dve", lambda: nc.vector.tensor_copy(out=l3[:, 0, :], in_=Lt[li][:]), r=[("Lt", li)], w=[("l3",)])
                S.op("dve", lambda: nc.vector.tensor_tensor(out=fl[2][:], in0=Lt[li][:], in1=l3[:, 0, :], op=ALU.subtract),
                     r=[("Lt", li), ("l3",)], w=[("fl", 2)])
                S.op("dve", lambda: nc.vector.tensor_copy(out=l3[:, 1, :], in_=fl[2][:]), r=[("fl", 2)], w=[("l3",)])
                S.op("dve", lambda: nc.vector.tensor_tensor(out=fl[0][:], in0=fl[2][:], in1=l3[:, 1, :], op=ALU.subtract),
                     r=[("fl", 2), ("l3",)], w=[("fl", 0)])
                S.op("dve", lambda: nc.vector.tensor_copy(out=l3[:, 2, :], in_=fl[0][:]), r=[("fl", 0)], w=[("l3",)])
                S.op("dve", lambda: nc.vector.tensor_scalar(out=l3[:, 3:6, :], in0=l3[:, 0:3, :], scalar1=-1.0,
                                                            scalar2=None, op0=ALU.mult), r=[("l3",)], w=[("l3",)])
                S.dma("sp", L3[b, :, :, tsl], l3[:, 0:3, :], r=[("l3",)], w=[dkey("L3", b)], key=("l3",))
                S.dma("sp", nL3[b, :, :, tsl], l3[:, 3:6, :], r=[("l3",)], w=[dkey("nL3", b)], key=("l3",))
                stop_here("A%d%s" % (l, "x8"))
                for g in range(16):
                    pi = group(CA_GA + g * 128, 128, hTi)
                    si = nxt("stg", NST)
                    S.op("act", lambda pi=pi, si=si: nc.scalar.activation(out=stg[si][:], in_=pp[pi][:], func=AF.Sigmoid),
                         r=[("pp", pi)], w=[("stg", si)])
                    store_stage(si, 128, sgT[b, g * 128:(g + 1) * 128, tsl], dkey("sgT", b, g))
                return Lt[li][:, 511:512]

            stop_here("A%dw" % l)
            for b in range(NSEQ):
                S.dma("sp", abc[:], modrows[l, 0, b, 0, :].partition_broadcast(128), r=[dkey("modrows", l, 0)], w=[("abc",)])
                S.dma("sp", bbc[:], modrows[l, 0, b, 1, :].partition_broadcast(128), r=[dkey("modrows", l, 0)], w=[("abc",)],
                      key=("abc",))
                carry = None
                stage1(b, 0, 0)
                stop_here("A%ds1" % l)
                for tt in range(NT):
                    if tt + 1 < NT:
                        stage1(b, tt + 1, (tt + 1) % 2)
                    carry = stage2(b, tt, tt % 2, carry)
            S.barrier()
        stop_here("A%d" % l)

        with contextlib.ExitStack() as st:
            QA = [st.enter_context(SB("QA%d" % i, [128, SEQ], BF16)) for i in range(2)]
            KA = [st.enter_context(SB("KA%d" % i, [128, SEQ], BF16)) for i in range(2)]
            VA = [st.enter_context(SB("VA%d" % i, [128, SEQ // 128, 128], BF16)) for i in range(2)]
            NPT = 4
            NPS = 5
            LA = 3
            PT = [st.enter_context(SB("PT%d" % i, [128, 512], BF16)) for i in range(NPT)]
            rcp = [st.enter_context(SB("rcp%d" % i, [128, 512], F32)) for i in range(2)]
            yst = [st.enter_context(SB("yst%d" % i, [64, 512], BF16)) for i in range(3)]
            pS = [st.enter_context(PS("pS%d" % i, [128, 512], F32)) for i in range(NPS)]
            pO = [st.enter_context(PS("pO%d" % i, [128, 512], F32)) for i in range(2)]
            cB = {"po": 0, "y": 0}
            heads = [(b, mixer, h) for b in range(NSEQ) for mixer in range(2) for h in range(NH)]

            def load_head(n):
                b, mixer, h = heads[n]
                bi = n % 2
                if mixer == 0:
                    S.dma("sp", QA[bi][0:96, :], qmT[b, h, :, :], r=[dkey("qmT", b, h)], w=[("QA", bi)])
                    S.dma("sp", KA[bi][64:96, :], kropeT[b, :, :], r=[dkey("kropeT", b)], w=[("KA", bi)])
                    S.dma("sp", KA[bi][0:64, :], knopeT[b, h * 64:(h + 1) * 64, :],
                          r=[dkey("knopeT", b, h // 2)], w=[("KA", bi)])
                    vsrc = vmla
                else:
                    S.op("pool", lambda: nc.gpsimd.memset(QA[bi][64:96, :], 1.0), w=[("QA", bi)])
                    S.op("pool", lambda: nc.gpsimd.memset(KA[bi][64:96, :], 1.0), w=[("KA", bi)])
                    S.dma("sp", QA[bi][0:64, :], fqT[b, h * 64:(h + 1) * 64, :], r=[dkey("fqT", b, h // 2)],
                          w=[("QA", bi)])
                    S.dma("sp", QA[bi][64:67, :], L3[b, h, :, :], r=[dkey("L3", b)], w=[("QA", bi)])
                    S.dma("sp", KA[bi][0:64, :], fkT[b, h * 64:(h + 1) * 64, :], r=[dkey("fkT", b, h // 2)],
                          w=[("KA", bi)])
                    S.dma("sp", KA[bi][67:70, :], nL3[b, h, :, :], r=[dkey("nL3", b)], w=[("KA", bi)])
                    vsrc = vfox
                S.dma("sp", VA[bi][:], vsrc[b, :, h * 128:(h + 1) * 128].rearrange("(k p) d -> p k d", p=128),
                      r=[dkey("v", b)], w=[("VA", bi)])

            load_head(0)
            for n, (b, mixer, h) in enumerate(heads):
                if n + 1 < len(heads):
                    load_head(n + 1)
                bi = n % 2
                dq = 96 if mixer == 0 else 70
                tiles = []
                for qt in range(NT):
                    nkb = 4 * qt + 4
                    for kb in range(nkb):
                        tiles.append((qt, kb, nkb))

                def emit_qk(i):
                    qt, kb, nkb = tiles[i]
                    j = kb - 4 * qt
                    q0 = 0 if j < 0 else j * 128
                    n_ = 512 - q0
                    si = i % NPS
                    diag = j >= 0
                    S.op("pe", lambda: nc.tensor.matmul(
                        pS[si][:, 0:n_], lhsT=KA[bi][0:dq, kb * 128:(kb + 1) * 128],
                        rhs=QA[bi][0:dq, qt * 512 + q0:qt * 512 + 512],
                        start=True, stop=not diag), r=[("KA", bi), ("QA", bi)], w=[("pS", si)])
                    if diag:
                        S.op("pe", lambda: nc.tensor.matmul(
                            pS[si][:, 0:128], lhsT=ident[:], rhs=maskT[:], start=False, stop=True),
                            r=[("ident",), ("maskT",)], w=[("pS", si)])

                for i in range(min(LA, len(tiles))):
                    emit_qk(i)
                oi = None
                for i, (qt, kb, nkb) in enumerate(tiles):
                    if i + LA < len(tiles):
                        emit_qk(i + LA)
                    if kb == 0:
                        oi = cB["po"] % 2
                        cB["po"] += 1
                    j = kb - 4 * qt
                    q0 = 0 if j < 0 else j * 128
                    n_ = 512 - q0
                    si = i % NPS
                    pi = i % NPT
                    S.op("act", lambda: nc.scalar.activation(out=PT[pi][:, 0:n_], in_=pS[si][:, 0:n_], func=AF.Exp),
                         r=[("pS", si)], w=[("PT", pi)])
                    S.op("pe", lambda: nc.tensor.matmul(
                        pO[oi][:, q0:512], lhsT=VA[bi][:, kb, :], rhs=PT[pi][:, 0:n_],
                        start=(kb == 0), stop=(kb == nkb - 1)), r=[("VA", bi), ("PT", pi)], w=[("pO", oi)])
                    if kb == nkb - 1:
                        ri = oi
                        S.op("dve", lambda: nc.vector.reciprocal(out=rcp[ri][64:128, :], in_=pO[oi][64:128, :]),
                             r=[("pO", oi)], w=[("rcp", ri)])
                        yi = cB["y"] % 3
                        cB["y"] += 1
                        S.op("dve", lambda: nc.vector.tensor_tensor(out=yst[yi][:], in0=pO[oi][0:64, :],
                                                                    in1=rcp[ri][64:128, :], op=ALU.mult),
                             r=[("pO", oi), ("rcp", ri)], w=[("yst", yi)])
                        row0 = mixer * 512 + h * 64
                        S.dma("sp", yT[b, row0:row0 + 64, qt * 512:(qt + 1) * 512], yst[yi][:],
                              r=[("yst", yi)], w=[dkey("yT", b, row0 // 128)])
            S.barrier()
        stop_here("B%d" % l)

        with contextlib.ExitStack() as st:
            WBM = load_w(st, "WBM", wbm_in[l], 512, D)
            WBF = load_w(st, "WBF", wbf_in[l], 512, D)
            WO = load_w(st, "WO", wo_in[l], D, D)
            yt = [st.enter_context(SB("yt%d" % i, [128, 8, 512], BF16)) for i in range(2)]
            sgt = [st.enter_context(SB("sgt%d" % i, [128, 16, 512], BF16)) for i in range(2)]
            xt = [st.enter_context(SB("xt%d" % i, [128, 4, D], F32)) for i in range(2)]
            mT = [st.enter_context(SB("mT%d" % i, [128, 8, 512], BF16)) for i in range(2)]
            tA = [st.enter_context(SB("tA%d" % i, [128, 512], F32)) for i in range(3)]
            tB = [st.enter_context(SB("tB%d" % i, [128, 512], F32)) for i in range(3)]
            tO = [st.enter_context(SB("tO%d" % i, [128, 512], F32)) for i in range(3)]
            pC = [st.enter_context(PS("pC%d" % i, [128, 512], F32)) for i in range(8)]
            cC = {"p": 0, "t": 0, "o": 0}
            it = 0
            tilesC = [(b, tt) for b in range(NSEQ) for tt in range(NT)]

            def loadC(n):
                b, tt = tilesC[n]
                i2 = n % 2
                tsl = slice(tt * 512, tt * 512 + 512)
                r0 = b * SEQ + tt * 512
                S.dma("sp", yt[i2][:], yT[b, :, tsl].rearrange("(k p) t -> p k t", p=128),
                      r=[dkey("yT", b)], w=[("yt", i2)])
                S.dma("sp", sgt[i2][:], sgT[b, :, tsl].rearrange("(k p) t -> p k t", p=128),
                      r=[dkey("sgT", b)], w=[("sgt", i2)])
                S.dma("sp", xt[i2][:], xsrc[r0:r0 + 512, :].rearrange("(k p) d -> p k d", p=128),
                      r=[dkey(xsrc_key)], w=[("xt", i2)])

            gbcC = [st.enter_context(SB("gbcC%d" % i, [128, D], F32)) for i in range(NSEQ)]
            for b in range(NSEQ):
                S.dma("sp", gbcC[b][:], modrows[l, 0, b, 2, :].partition_broadcast(128), r=[dkey("modrows", l, 0)],
                      w=[("gbcC", b)])
            loadC(0)
            for b in range(NSEQ):
                for tt in range(NT):
                    i2 = it % 2
                    it += 1
                    if it < len(tilesC):
                        loadC(it)
                    tsl = slice(tt * 512, tt * 512 + 512)
                    r0 = b * SEQ + tt * 512
                    for dc in range(8):
                        pa = cC["p"] % 8
                        pb = (cC["p"] + 1) % 8
                        cC["p"] += 2
                        for kc in range(4):
                            S.op("pe", lambda kc=kc: nc.tensor.matmul(
                                pC[pa][:], lhsT=WBM[:, kc, dc * 128:(dc + 1) * 128], rhs=yt[i2][:, kc, :],
                                start=(kc == 0), stop=(kc == 3)), r=[("WBM",), ("yt", i2)], w=[("pC", pa)])
                        for kc in range(4):
                            S.op("pe", lambda kc=kc: nc.tensor.matmul(
                                pC[pb][:], lhsT=WBF[:, kc, dc * 128:(dc + 1) * 128], rhs=yt[i2][:, 4 + kc, :],
                                start=(kc == 0), stop=(kc == 3)), r=[("WBF",), ("yt", i2)], w=[("pC", pb)])
                        ti = cC["t"] % 3
                        cC["t"] += 1
                        S.op("dve", lambda: nc.vector.tensor_tensor(out=tA[ti][:], in0=pC[pa][:], in1=sgt[i2][:, dc, :],
                                                                    op=ALU.mult), r=[("pC", pa), ("sgt", i2)], w=[("tA", ti)])
                        S.op("dve", lambda: nc.vector.tensor_tensor(out=tB[ti][:], in0=pC[pb][:], in1=sgt[i2][:, 8 + dc, :],
                                                                    op=ALU.mult), r=[("pC", pb), ("sgt", i2)], w=[("tB", ti)])
                        S.op("pool", lambda: nc.gpsimd.tensor_tensor(out=mT[i2][:, dc, :], in0=tA[ti][:], in1=tB[ti][:],
                                                                     op=ALU.add), r=[("tA", ti), ("tB", ti)], w=[("mT", i2, dc)])
                    for blk in range(4):
                        for half in range(2):
                            po = cC["p"] % 8
                            cC["p"] += 1
                            for kc in range(8):
                                S.op("pe", lambda kc=kc: nc.tensor.matmul(
                                    pC[po][:], lhsT=mT[i2][:, kc, blk * 128:(blk + 1) * 128],
                                    rhs=WO[:, kc, half * 512:(half + 1) * 512], start=(kc == 0), stop=(kc == 7)),
                                    r=[("WO",), ("mT", i2, kc)], w=[("pC", po)])
                            oi = cC["o"] % 3
                            cC["o"] += 1
                            hs = slice(half * 512, half * 512 + 512)
                            S.op("dve", lambda: nc.vector.tensor_tensor(out=tO[oi][:], in0=pC[po][:], in1=gbcC[b][:, hs],
                                                                        op=ALU.mult), r=[("pC", po), ("gbcC", b)], w=[("tO", oi)])
                            S.op("pool", lambda: nc.gpsimd.tensor_tensor(out=xt[i2][:, blk, hs], in0=xt[i2][:, blk, hs],
                                                                         in1=tO[oi][:], op=ALU.add),
                                 r=[("tO", oi), ("xt", i2)], w=[("xt", i2)])
                    S.dma("sp", xs[r0:r0 + 512, :].rearrange("(k p) d -> p k d", p=128), xt[i2][:],
                          r=[("xt", i2)], w=[dkey("xs")])
            S.barrier()
        stop_here("C%d" % l)

        is_moe = (l % 2 == 1)
        li = l // 2
        with contextlib.ExitStack() as st:
            NXB = 4
            xbuf = [st.enter_context(SB("dxb%d" % i, [128, D], F32)) for i in range(NXB)]
            junk = st.enter_context(SB("djunk", [128, D], BF16))
            tmpf = [st.enter_context(SB("dtmpf%d" % i, [128, D], F32)) for i in range(2)]
            hbuf = [st.enter_context(SB("dhb%d" % i, [128, D], BF16)) for i in range(2)]
            hf = [st.enter_context(SB("dhf%d" % i, [128, D], F32)) for i in range(2)]
            ssb = [st.enter_context(SB("dss%d" % i, [128, 1], F32)) for i in range(4)]
            rsb = [st.enter_context(SB("drs%d" % i, [128, 1], F32)) for i in range(4)]
            hTs = [st.enter_context(SB("dhT%d" % i, [128, 8, 128], BF16)) for i in range(3)]
            abc = st.enter_context(SB("dabc", [128, D], F32))
            bbc = st.enter_context(SB("dbbc", [128, D], F32))
            pst = st.enter_context(PS("dpst", [128, D], BF16))
            if is_moe:
                wrb = st.enter_context(SB("wrb", [128, NEXP, D], F32))
                lg = [st.enter_context(SB("lg%d" % i, [128, NEXP], F32)) for i in range(2)]
                m8 = [st.enter_context(SB("m8%d" % i, [128, 8], F32)) for i in range(2)]
                w12 = [st.enter_context(SB("w12%d" % i, [128, 4], F32)) for i in range(2)]
                cmb = [st.enter_context(SB("cmb%d" % i, [128, 2, NEXP], F32)) for i in range(2)]
                junkf = st.enter_context(SB("junkf", [128, D], F32))
                for e in range(NEXP):
                    S.dma("sp", wrb[:, e, :], wr_in[li, e, :].partition_broadcast(128), r=[dkey("wr")], w=[("wrb",)])
                base8 = st.enter_context(SB("base8", [128, NEXP], F32))
                mk8 = [st.enter_context(SB("mk8%d" % i, [128, NEXP], F32)) for i in range(2)]
                fl8 = [st.enter_context(SB("fl8%d" % i, [128, 2, NEXP], F32)) for i in range(2)]
                gf2 = [st.enter_context(SB("gf2%d" % i, [128, 4], F32)) for i in range(2)]
                gi2 = [st.enter_context(SB("gi2%d" % i, [128, 2], I32)) for i in range(2)]
                gw2 = [st.enter_context(SB("gw2%d" % i, [128, 2], F32)) for i in range(2)]
                cnti = st.enter_context(SB("cnti", [1, NEXP], I32))
                prt = st.enter_context(PS("prt", [128, 2 * NEXP], F32))
                S.op("pool", lambda: nc.gpsimd.memset(base8[:], 0.0), w=[("base8",)])
            ci = 0
            for b in range(NSEQ):
                S.dma("sp", abc[:], modrows[l, 1, b, 0, :].partition_broadcast(128), r=[dkey("modrows", l, 1)], w=[("abc",)])
                S.dma("sp", bbc[:], modrows[l, 1, b, 1, :].partition_broadcast(128), r=[dkey("modrows", l, 1)], w=[("abc",)],
                      key=("abc",))
                for blk in range(SEQ // 128):
                    r0 = b * SEQ + blk * 128
                    xi = ci % NXB
                    si = ci % 4
                    hi = ci % 2
                    ti = ci % 3
                    ci += 1
                    S.dma("sp", xbuf[xi][:], xs[r0:r0 + 128, :], r=[dkey("xs")], w=[("xb", xi)])
                    S.op("act", lambda: nc.scalar.activation(out=junk[:], in_=xbuf[xi][:], func=AF.Square,
                                                             accum_out=ssb[si][:]), r=[("xb", xi)], w=[("junk",), ("ss", si)])
                    rms_rstd((ssb[si][:], ("ss", si)), (rsb[si][:], ("rs", si)), D)
                    S.op("dve", lambda: nc.vector.scalar_tensor_tensor(out=tmpf[hi][:], in0=xbuf[xi][:], scalar=rsb[si][:],
                                                                       in1=abc[:], op0=ALU.mult, op1=ALU.mult),
                         r=[("xb", xi), ("rs", si), ("abc",)], w=[("tmpf", hi)])
                    S.op("pool", lambda: nc.gpsimd.tensor_tensor(out=hf[hi][:], in0=tmpf[hi][:], in1=bbc[:], op=ALU.add),
                         r=[("tmpf", hi), ("abc",)], w=[("hf", hi)])
                    S.op("act", lambda: nc.scalar.copy(out=hbuf[hi][:], in_=hf[hi][:]), r=[("hf", hi)], w=[("hb", hi)])
                    for kc in range(8):
                        S.op("pe", lambda kc=kc: nc.tensor.transpose(
                            pst[:, kc * 128:(kc + 1) * 128], hbuf[hi][:, kc * 128:(kc + 1) * 128], ident[:]),
                            r=[("hb", hi), ("ident",)], w=[("pst",)])
                    S.op("act", lambda: nc.scalar.copy(out=hTs[ti][:], in_=pst[:].rearrange("p (k t) -> p k t", k=8)),
                         r=[("pst",)], w=[("hTs", ti)])
                    S.dma("sp", h2T[:, r0:r0 + 128].rearrange("(k p) t -> p k t", p=128), hTs[ti][:],
                          r=[("hTs", ti)], w=[dkey("h2T")])
                    if is_moe:
                        for e in range(NEXP):
                            S.op("dve", lambda e=e: nc.vector.scalar_tensor_tensor(
                                out=junkf[:], in0=hf[hi][:], scalar=1.0, in1=wrb[:, e, :], op0=ALU.mult, op1=ALU.mult,
                                accum_out=lg[hi][:, e:e + 1]),
                                r=[("hf", hi), ("wrb",)], w=[("junkf",), ("lg", hi)])
                        S.op("dve", lambda: nc.vector.max(out=m8[hi][:], in_=lg[hi][:]), r=[("lg", hi)], w=[("m8", hi)])
                        S.op("dve", lambda: nc.vector.tensor_tensor(out=w12[hi][:, 0:1], in0=m8[hi][:, 1:2], in1=m8[hi][:, 0:1],
                                                                    op=ALU.subtract), r=[("m8", hi)], w=[("w12", hi)])
                        S.op("act", lambda: nc.scalar.activation(out=w12[hi][:, 1:2], in_=w12[hi][:, 0:1], func=AF.Exp),
                             r=[("w12", hi)], w=[("w12", hi)])
                        S.op("dve", lambda: nc.vector.tensor_scalar(out=w12[hi][:, 1:2], in0=w12[hi][:, 1:2], scalar1=1.0,
                                                                    scalar2=None, op0=ALU.add), r=[("w12", hi)], w=[("w12", hi)])
                        S.op("dve", lambda: nc.vector.reciprocal(out=w12[hi][:, 2:3], in_=w12[hi][:, 1:2]),
                             r=[("w12", hi)], w=[("w12", hi)])
                        S.op("dve", lambda: nc.vector.tensor_scalar(out=w12[hi][:, 3:4], in0=w12[hi][:, 2:3], scalar1=-1.0,
                                                                    scalar2=1.0, op0=ALU.mult, op1=ALU.add),
                             r=[("w12", hi)], w=[("w12", hi)])
                        S.op("dve", lambda: nc.vector.tensor_scalar(out=cmb[hi][:, 0, :], in0=lg[hi][:], scalar1=m8[hi][:, 0:1],
                                                                    scalar2=w12[hi][:, 2:3], op0=ALU.is_equal, op1=ALU.mult),
                             r=[("lg", hi), ("m8", hi), ("w12", hi)], w=[("cmb", hi)])
                        S.op("dve", lambda: nc.vector.tensor_scalar(out=cmb[hi][:, 1, :], in0=lg[hi][:], scalar1=m8[hi][:, 1:2],
                                                                    scalar2=w12[hi][:, 3:4], op0=ALU.is_equal, op1=ALU.mult),
                             r=[("lg", hi), ("m8", hi), ("w12", hi)], w=[("cmb", hi)])
                        S.op("dve", lambda: nc.vector.tensor_tensor(out=cmb[hi][:, 0, :], in0=cmb[hi][:, 0, :],
                                                                    in1=cmb[hi][:, 1, :], op=ALU.add),
                             r=[("cmb", hi)], w=[("cmb", hi)])
                        S.dma("sp", comb[r0:r0 + 128, :], cmb[hi][:, 0, :], r=[("cmb", hi)], w=[dkey("comb")])
                        S.op("dve", lambda: nc.vector.tensor_scalar(out=mk8[hi][:], in0=cmb[hi][:, 0, :], scalar1=0.0,
                                                                    scalar2=None, op0=ALU.is_gt), r=[("cmb", hi)], w=[("mk8", hi)])
                        S.op("pe", lambda: nc.tensor.matmul(prt[:, 0:NEXP], lhsT=ustr[:], rhs=mk8[hi][:], start=True, stop=True),
                             r=[("ustr",), ("mk8", hi)], w=[("prt",)])
                        S.op("pe", lambda: nc.tensor.matmul(prt[:, NEXP:2 * NEXP], lhsT=onesq[:], rhs=mk8[hi][:], start=True,
                                                            stop=True), r=[("onesq",), ("mk8", hi)], w=[("prt",)])
                        S.op("dve", lambda: nc.vector.tensor_tensor(out=fl8[hi][:, 0, :], in0=prt[:, 0:NEXP], in1=base8[:],
                                                                    op=ALU.add), r=[("prt",), ("base8",)], w=[("fl8", hi)])
                        S.op("dve", lambda: nc.vector.tensor_tensor(out=fl8[hi][:, 0, :], in0=fl8[hi][:, 0, :], in1=eoff[:],
                                                                    op=ALU.add), r=[("fl8", hi), ("eoff",)], w=[("fl8", hi)])
                        S.op("dve", lambda: nc.vector.tensor_tensor(out=fl8[hi][:, 0, :], in0=fl8[hi][:, 0, :], in1=mk8[hi][:],
                                                                    op=ALU.mult), r=[("fl8", hi), ("mk8", hi)], w=[("fl8", hi)])
                        S.op("dve", lambda: nc.vector.tensor_tensor(out=base8[:], in0=prt[:, NEXP:2 * NEXP], in1=base8[:],
                                                                    op=ALU.add), r=[("prt",), ("base8",)], w=[("base8",)])
                        S.op("dve", lambda: nc.vector.tensor_reduce(out=gf2[hi][:, 1:2], in_=fl8[hi][:, 0, :],
                                                                    axis=mybir.AxisListType.X, op=ALU.max),
                             r=[("fl8", hi)], w=[("gf2", hi)])
                        S.op("dve", lambda: nc.vector.tensor_reduce(out=gf2[hi][:, 2:3], in_=fl8[hi][:, 0, :],
                                                                    axis=mybir.AxisListType.X, op=ALU.add),
                             r=[("fl8", hi)], w=[("gf2", hi)])
                        S.op("dve", lambda: nc.vector.tensor_tensor(out=gf2[hi][:, 0:1], in0=gf2[hi][:, 2:3], in1=gf2[hi][:, 1:2],
                                                                    op=ALU.subtract), r=[("gf2", hi)], w=[("gf2", hi)])
                        S.op("dve", lambda: nc.vector.tensor_copy(out=gi2[hi][:], in_=gf2[hi][:, 0:2]), r=[("gf2", hi)],
                             w=[("gi2", hi)])
                        S.op("dve", lambda: nc.vector.tensor_scalar(out=fl8[hi][:, 1, :], in0=fl8[hi][:, 0, :],
                                                                    scalar1=gf2[hi][:, 1:2], scalar2=None, op0=ALU.is_equal),
                             r=[("fl8", hi), ("gf2", hi)], w=[("fl8", hi)])
                        S.op("dve", lambda: nc.vector.tensor_tensor(out=fl8[hi][:, 1, :], in0=fl8[hi][:, 1, :],
                                                                    in1=cmb[hi][:, 0, :], op=ALU.mult),
                             r=[("fl8", hi), ("cmb", hi)], w=[("fl8", hi)])
                        S.op("dve", lambda: nc.vector.tensor_reduce(out=gw2[hi][:, 1:2], in_=fl8[hi][:, 1, :],
                                                                    axis=mybir.AxisListType.X, op=ALU.add),
                             r=[("fl8", hi)], w=[("gw2", hi)])
                        S.op("dve", lambda: nc.vector.tensor_scalar(out=gw2[hi][:, 0:1], in0=gw2[hi][:, 1:2], scalar1=-1.0,
                                                                    scalar2=1.0, op0=ALU.mult, op1=ALU.add),
                             r=[("gw2", hi)], w=[("gw2", hi)])
                        S.dma("sp", gidx[r0:r0 + 128, :], gi2[hi][:], r=[("gi2", hi)], w=[dkey("gidx")])
                        S.dma("sp", gwd[r0:r0 + 128, :], gw2[hi][:], r=[("gw2", hi)], w=[dkey("gwd")])
                        for k in range(2):
                            S.dma_indirect("pool", out=hg, out_offset=bass.IndirectOffsetOnAxis(ap=gi2[hi][:, k:k + 1], axis=0),
                                           in_=hbuf[hi][:], in_offset=None, r=[("hb", hi), ("gi2", hi)], w=[dkey("hg")],
                                           key=("hb", hi))
            if is_moe:
                S.op("dve", lambda: nc.vector.tensor_copy(out=cnt_sb[:], in_=base8[0:1, :]), r=[("base8",)], w=[("cnt_sb",)])
                S.dma("sp", cnts[:, :], cnt_sb[:], r=[("cnt_sb",)], w=[dkey("cnts")])
            S.barrier()
        stop_here("D%d" % l)

        if is_moe:
            FQ = DFF // 4
            NFC = FQ // 128
            passes = [(e, q) for e in range(NEXP) for q in range(4)]
            with contextlib.ExitStack() as st:
                WG = [st.enter_context(SB("rWG%d" % i, [128, 8, FQ], BF16)) for i in range(2)]
                WU = [st.enter_context(SB("rWU%d" % i, [128, 8, FQ], BF16)) for i in range(2)]
                WD = [st.enter_context(SB("rWD%d" % i, [128, NFC, D], BF16)) for i in range(2)]
                htm = [st.enter_context(SB("htm%d" % i, [128, 4, D], BF16)) for i in range(2)]
                h2 = [st.enter_context(SB("rh2%d" % i, [128, 8, 512], BF16)) for i in range(2)]
                AT = [st.enter_context(SB("rAT%d" % i, [128, NFC, 512], BF16)) for i in range(2)]
                sg = [st.enter_context(SB("rsg%d" % i, [128, 512], F32)) for i in range(3)]
                ot = [st.enter_context(SB("rot%d" % i, [128, 4, D], F32)) for i in range(2)]
                pE = [st.enter_context(PS("rpE%d" % i, [128, 512], F32)) for i in range(7)]
                pstE = st.enter_context(PS("rpst", [128, D], BF16))
                cE = {"p": 0, "s": 0}

                def load_set(pi):
                    e, q = passes[pi]
                    ws = pi % 2
                    gsrc, usrc, dsrc = mwg_in[li, e], mwu_in[li, e], mwd_in[li, e]
                    for k in range(8):
                        S.dma("pool", WG[ws][:, k, :], gsrc[k * 128:(k + 1) * 128, q * FQ:(q + 1) * FQ],
                              r=[dkey("wg")], w=[("WG", ws)])
                    for k in range(8):
                        S.dma("pool", WU[ws][:, k, :], usrc[k * 128:(k + 1) * 128, q * FQ:(q + 1) * FQ],
                              r=[dkey("wu")], w=[("WU", ws)])
                    for k in range(NFC):
                        S.dma("pool", WD[ws][:, k, :], dsrc[q * FQ + k * 128:q * FQ + (k + 1) * 128, :],
                              r=[dkey("wd")], w=[("WD", ws)])

                def load_slots(Sx, pi, it_, i2):
                    e, q = passes[pi]
                    row0 = e * CAPR + it_ * 512
                    if q == 0:
                        Sx.dma("sp", htm[i2][:], hg[row0:row0 + 512, :].rearrange("(k p) d -> p k d", p=128),
                               r=[dkey("hg")], w=[("htm", i2)])
                    else:
                        Sx.dma("sp", h2[i2][:], hgT[:, row0:row0 + 512].rearrange("(k p) t -> p k t", p=128),
                               r=[("hgTrow", e, it_)], w=[("h2", i2)], key=("h2", i2))
                    if q > 0:
                        Sx.dma("sp", ot[i2][:], og[row0:row0 + 512, :].rearrange("(k p) d -> p k d", p=128),
                               r=[("ogrow", e, it_)], w=[("ot", i2)], key=("ot", i2))

                def emit_tile(Sx, pi, it_, i2):
                    e, q = passes[pi]
                    ws = pi % 2
                    row0 = e * CAPR + it_ * 512
                    if q == 0:
                        for blk in range(4):
                            for kc in range(8):
                                Sx.op("pe", lambda kc=kc: nc.tensor.transpose(
                                    pstE[:, kc * 128:(kc + 1) * 128], htm[i2][:, blk, kc * 128:(kc + 1) * 128], ident[:]),
                                    r=[("htm", i2), ("ident",)], w=[("pstE",)])
                            Sx.op("act", lambda: nc.scalar.copy(out=h2[i2][:, :, blk * 128:(blk + 1) * 128],
                                                                in_=pstE[:].rearrange("p (k t) -> p k t", k=8)),
                                  r=[("pstE",)], w=[("h2", i2)])
                        Sx.dma("sp", hgT[:, row0:row0 + 512].rearrange("(k p) t -> p k t", p=128), h2[i2][:],
                               r=[("h2", i2)], w=[("hgTrow", e, it_)], key=("h2", i2))
                    for fc in range(NFC):
                        pg = cE["p"] % 7
                        pu = (cE["p"] + 1) % 7
                        cE["p"] += 2
                        for kc in range(8):
                            Sx.op("pe", lambda kc=kc: nc.tensor.matmul(
                                pE[pg][:], lhsT=WG[ws][:, kc, fc * 128:(fc + 1) * 128], rhs=h2[i2][:, kc, :],
                                start=(kc == 0), stop=(kc == 7)), r=[("WG", ws), ("h2", i2)], w=[("pE", pg)])
                        for kc in range(8):
                            Sx.op("pe", lambda kc=kc: nc.tensor.matmul(
                                pE[pu][:], lhsT=WU[ws][:, kc, fc * 128:(fc + 1) * 128], rhs=h2[i2][:, kc, :],
                                start=(kc == 0), stop=(kc == 7)), r=[("WU", ws), ("h2", i2)], w=[("pE", pu)])
                        si = cE["s"] % 3
                        cE["s"] += 1
                        Sx.op("act", lambda: nc.scalar.activation(out=sg[si][:], in_=pE[pg][:], func=AF.Silu),
                              r=[("pE", pg)], w=[("sg", si)])
                        Sx.op("dve", lambda: nc.vector.tensor_tensor(out=AT[i2][:, fc, :], in0=pE[pu][:], in1=sg[si][:],
                                                                     op=ALU.mult),
                              r=[("pE", pu), ("sg", si)], w=[("AT", i2, fc)])
                    for blk in range(4):
                        for half in range(2):
                            po = cE["p"] % 7
                            cE["p"] += 1
                            for fc in range(NFC):
                                Sx.op("pe", lambda fc=fc: nc.tensor.matmul(
                                    pE[po][:], lhsT=AT[i2][:, fc, blk * 128:(blk + 1) * 128],
                                    rhs=WD[ws][:, fc, half * 512:(half + 1) * 512], start=(fc == 0), stop=(fc == NFC - 1)),
                                    r=[("WD", ws), ("AT", i2, fc)], w=[("pE", po)])
                            hs = slice(half * 512, half * 512 + 512)
                            if q == 0:
                                Sx.op("dve", lambda: nc.vector.tensor_copy(out=ot[i2][:, blk, hs], in_=pE[po][:]),
                                      r=[("pE", po)], w=[("ot", i2)])
                            else:
                                Sx.op("dve", lambda: nc.vector.tensor_tensor(out=ot[i2][:, blk, hs], in0=pE[po][:],
                                                                             in1=ot[i2][:, blk, hs], op=ALU.add),
                                      r=[("pE", po), ("ot", i2)], w=[("ot", i2)])
                    Sx.dma("sp", og[row0:row0 + 512, :].rearrange("(k p) d -> p k d", p=128), ot[i2][:],
                           r=[("ot", i2)], w=[("ogrow", e, it_)], key=("ot", i2))

                load_set(0)
                for pi, (e, q) in enumerate(passes):
                    if pi + 1 < len(passes):
                        load_set(pi + 1)
                    load_slots(S, pi, 0, 0)
                    for it_ in range(KT):
                        if it_ + 1 < KT:
                            load_slots(S, pi, it_ + 1, (it_ + 1) % 2)
                        emit_tile(S, pi, it_, it_ % 2)
                    if KT < CAPT:
                        S.barrier()
                        for reg in cnt_reg:
                            nc.reg_load(reg, cnt_sb[0:1, e:e + 1])
                        dyn = [g for g in ([KT], [KT + 1], list(range(KT + 2, CAPT))) if g and g[0] < CAPT]
                        for grp in dyn:
                            with nc.If_cmp(cnt_reg, grp[0] * 512, "IS_GT"):
                                for it_ in grp:
                                    load_slots(S2, pi, it_, it_ % 2)
                                    emit_tile(S2, pi, it_, it_ % 2)
                                S2.finish_local_block()
                S.barrier()
            stop_here("E%d" % l)
            with contextlib.ExitStack() as st:
                xb = [st.enter_context(SB("gxb%d" % i, [128, D], F32)) for i in range(3)]
                o1 = [st.enter_context(SB("go1%d" % i, [128, D], F32)) for i in range(2)]
                o2 = [st.enter_context(SB("go2%d" % i, [128, D], F32)) for i in range(2)]
                tt_ = [st.enter_context(SB("gtt%d" % i, [128, D], F32)) for i in range(2)]
                gi = [st.enter_context(SB("ggi%d" % i, [128, 2], I32)) for i in range(2)]
                gw = [st.enter_context(SB("ggw%d" % i, [128, 2], F32)) for i in range(2)]
                gb = [st.enter_context(SB("ggb%d" % i, [128, D], F32)) for i in range(NSEQ)]
                for b in range(NSEQ):
                    S.dma("sp", gb[b][:], modrows[l, 1, b, 2, :].partition_broadcast(128), r=[dkey("modrows", l, 1)],
                          w=[("ggb", b)])
                for n in range(T // 128):
                    b = (n * 128) // SEQ
                    r0 = n * 128
                    i2, i3 = n % 2, n % 3
                    S.dma("sp", xb[i3][:], xs[r0:r0 + 128, :], r=[dkey("xs")], w=[("gxb", i3)])
                    S.dma("sp", gi[i2][:], gidx[r0:r0 + 128, :], r=[dkey("gidx")], w=[("ggi", i2)])
                    S.dma("sp", gw[i2][:], gwd[r0:r0 + 128, :], r=[dkey("gwd")], w=[("ggw", i2)])
                    S.dma_indirect("pool", out=o1[i2][:], out_offset=None, in_=og,
                                   in_offset=bass.IndirectOffsetOnAxis(ap=gi[i2][:, 0:1], axis=0),
                                   r=[("ggi", i2)], w=[("go1", i2)], key=("go1", i2))
                    S.dma_indirect("pool", out=o2[i2][:], out_offset=None, in_=og,
                                   in_offset=bass.IndirectOffsetOnAxis(ap=gi[i2][:, 1:2], axis=0),
                                   r=[("ggi", i2)], w=[("go2", i2)], key=("go2", i2))
                    S.op("dve", lambda: nc.vector.tensor_scalar(out=tt_[i2][:], in0=o1[i2][:], scalar1=gw[i2][:, 0:1],
                                                                scalar2=None, op0=ALU.mult),
                         r=[("go1", i2), ("ggw", i2)], w=[("gtt", i2)])
                    S.op("dve", lambda: nc.vector.scalar_tensor_tensor(out=tt_[i2][:], in0=o2[i2][:], scalar=gw[i2][:, 1:2],
                                                                       in1=tt_[i2][:], op0=ALU.mult, op1=ALU.add),
                         r=[("go2", i2), ("ggw", i2), ("gtt", i2)], w=[("gtt", i2)])
                    S.op("pool", lambda: nc.gpsimd.tensor_tensor(out=tt_[i2][:], in0=tt_[i2][:], in1=gb[b][:], op=ALU.mult),
                         r=[("gtt", i2), ("ggb", b)], w=[("gtt", i2)])
                    S.op("dve", lambda: nc.vector.tensor_tensor(out=xb[i3][:], in0=xb[i3][:], in1=tt_[i2][:], op=ALU.add),
                         r=[("gtt", i2), ("gxb", i3)], w=[("gxb", i3)])
                    S.dma("sp", xs[r0:r0 + 128, :], xb[i3][:], r=[("gxb", i3)], w=[dkey("xs")])
                S.barrier()
            continue

        FQ = DFF // 4
        NFC = FQ // 128
        n_exp = NEXP if is_moe else 1
        passes = [(e, q) for e in range(n_exp) for q in range(4)]
        with contextlib.ExitStack() as st:
            WG = [st.enter_context(SB("WG%d" % i, [128, 8, FQ], BF16)) for i in range(2)]
            WU = [st.enter_context(SB("WU%d" % i, [128, 8, FQ], BF16)) for i in range(2)]
            WD = [st.enter_context(SB("WD%d" % i, [128, NFC, D], BF16)) for i in range(2)]
            h2 = [st.enter_context(SB("h2%d" % i, [128, 8, 512], BF16)) for i in range(2)]
            AT = [st.enter_context(SB("AT%d" % i, [128, NFC, 512], BF16)) for i in range(2)]
            sg = [st.enter_context(SB("sg%d" % i, [128, 512], F32)) for i in range(3)]
            xt = [st.enter_context(SB("ext%d" % i, [128, 4, D], F32)) for i in range(2)]
            tO = [st.enter_context(SB("etO%d" % i, [128, 512], F32)) for i in range(3)]
            gb = [st.enter_context(SB("egb%d" % i, [128, D], F32)) for i in range(NSEQ)]
            cmt = [st.enter_context(SB("cmt%d" % i, [128, 4, NEXP], F32)) for i in range(2)]
            pE = [st.enter_context(PS("pE%d" % i, [128, 512], F32)) for i in range(8)]
            cE = {"p": 0, "s": 0, "o": 0}
            for b in range(NSEQ):
                S.dma("sp", gb[b][:], modrows[l, 1, b, 2, :].partition_broadcast(128), r=[dkey("modrows", l, 1)],
                      w=[("egb", b)])

            def load_set(pi):
                e, q = passes[pi]
                ws = pi % 2
                if is_moe:
                    gsrc, usrc, dsrc = mwg_in[li, e], mwu_in[li, e], mwd_in[li, e]
                else:
                    gsrc, usrc, dsrc = dwg_in[li], dwu_in[li], dwd_in[li]
                for k in range(8):
                    S.dma("pool", WG[ws][:, k, :], gsrc[k * 128:(k + 1) * 128, q * FQ:(q + 1) * FQ],
                          r=[dkey("wg")], w=[("WG", ws)])
                for k in range(8):
                    S.dma("pool", WU[ws][:, k, :], usrc[k * 128:(k + 1) * 128, q * FQ:(q + 1) * FQ],
                          r=[dkey("wu")], w=[("WU", ws)])
                for k in range(NFC):
                    S.dma("pool", WD[ws][:, k, :], dsrc[q * FQ + k * 128:q * FQ + (k + 1) * 128, :],
                          r=[dkey("wd")], w=[("WD", ws)])

            tiles = [(pi, b, tt) for pi in range(len(passes)) for b in range(NSEQ) for tt in range(NT)]

            def load_tile(n):
                pi, b, tt = tiles[n]
                i2 = n % 2
                r0 = b * SEQ + tt * 512
                S.dma("sp", h2[i2][:], h2T[:, r0:r0 + 512].rearrange("(k p) t -> p k t", p=128),
                      r=[dkey("h2T")], w=[("h2", i2)])
                S.dma("sp", xt[i2][:], xs[r0:r0 + 512, :].rearrange("(k p) d -> p k d", p=128),
                      r=[("xsrow", b, tt)], w=[("ext", i2)], key=("ext", i2))
                if is_moe:
                    with nc.allow_non_contiguous_dma(reason="per-token combine weights, 32B rows"):
                        S.dma("sp", cmt[i2][:], comb[r0:r0 + 512, :].rearrange("(k p) e -> p k e", p=128),
                              r=[dkey("comb")], w=[("cmt", i2)])

            load_set(0)
            load_tile(0)
            n_tiles_pass = NSEQ * NT
            for n, (pi, b, tt) in enumerate(tiles):
                e, q = passes[pi]
                ws = pi % 2
                i2 = n % 2
                r0 = b * SEQ + tt * 512
                if n % n_tiles_pass == 0 and pi + 1 < len(passes):
                    load_set(pi + 1)
                if n + 1 < len(tiles):
                    load_tile(n + 1)
                for fc in range(NFC):
                    pg = cE["p"] % 8
                    pu = (cE["p"] + 1) % 8
                    cE["p"] += 2
                    for kc in range(8):
                        S.op("pe", lambda kc=kc: nc.tensor.matmul(
                            pE[pg][:], lhsT=WG[ws][:, kc, fc * 128:(fc + 1) * 128], rhs=h2[i2][:, kc, :],
                            start=(kc == 0), stop=(kc == 7)), r=[("WG", ws), ("h2", i2)], w=[("pE", pg)])
                    for kc in range(8):
                        S.op("pe", lambda kc=kc: nc.tensor.matmul(
                            pE[pu][:], lhsT=WU[ws][:, kc, fc * 128:(fc + 1) * 128], rhs=h2[i2][:, kc, :],
                            start=(kc == 0), stop=(kc == 7)), r=[("WU", ws), ("h2", i2)], w=[("pE", pu)])
                    si = cE["s"] % 3
                    cE["s"] += 1
                    S.op("act", lambda: nc.scalar.activation(out=sg[si][:], in_=pE[pg][:], func=AF.Silu),
                         r=[("pE", pg)], w=[("sg", si)])
                    S.op("dve", lambda: nc.vector.tensor_tensor(out=AT[i2][:, fc, :], in0=pE[pu][:], in1=sg[si][:],
                                                                op=ALU.mult),
                         r=[("pE", pu), ("sg", si)], w=[("AT", i2, fc)])
                for blk in range(4):
                    for half in range(2):
                        po = cE["p"] % 8
                        cE["p"] += 1
                        for fc in range(NFC):
                            S.op("pe", lambda fc=fc: nc.tensor.matmul(
                                pE[po][:], lhsT=AT[i2][:, fc, blk * 128:(blk + 1) * 128],
                                rhs=WD[ws][:, fc, half * 512:(half + 1) * 512], start=(fc == 0), stop=(fc == NFC - 1)),
                                r=[("WD", ws), ("AT", i2, fc)], w=[("pE", po)])
                        oi = cE["o"] % 3
                        cE["o"] += 1
                        hs = slice(half * 512, half * 512 + 512)
                        if is_moe:
                            S.op("dve", lambda: nc.vector.scalar_tensor_tensor(
                                out=tO[oi][:], in0=pE[po][:], scalar=cmt[i2][:, blk, e:e + 1], in1=gb[b][:, hs],
                                op0=ALU.mult, op1=ALU.mult), r=[("pE", po), ("egb", b), ("cmt", i2)], w=[("etO", oi)])
                        else:
                            S.op("dve", lambda: nc.vector.tensor_tensor(out=tO[oi][:], in0=pE[po][:], in1=gb[b][:, hs],
                                                                        op=ALU.mult),
                                 r=[("pE", po), ("egb", b)], w=[("etO", oi)])
                        S.op("pool", lambda: nc.gpsimd.tensor_tensor(out=xt[i2][:, blk, hs], in0=xt[i2][:, blk, hs],
                                                                     in1=tO[oi][:], op=ALU.add),
                             r=[("etO", oi), ("ext", i2)], w=[("ext", i2)])
                S.dma("sp", xs[r0:r0 + 512, :].rearrange("(k p) d -> p k d", p=128), xt[i2][:],
                      r=[("ext", i2)], w=[("xsrow", b, tt)], key=("ext", i2))
            S.barrier()

    with contextlib.ExitStack() as st:
        NXB = 4
        xbuf = [st.enter_context(SB("fxb%d" % i, [128, D], F32)) for i in range(NXB)]
        junk = st.enter_context(SB("fjunk", [128, D], BF16))
        ob = [st.enter_context(SB("fob%d" % i, [128, D], F32)) for i in range(3)]
        ssb = [st.enter_context(SB("fss%d" % i, [128, 1], F32)) for i in range(4)]
        rsb = [st.enter_context(SB("frs%d" % i, [128, 1], F32)) for i in range(4)]
        gf = st.enter_context(SB("gf", [128, D], F32))
        S.dma("sp", gf[:], gfin_in[:].partition_broadcast(128), r=[dkey("gfin")], w=[("gf",)])
        for i in range(T // 128):
            r0 = i * 128
            xi, si, oi = i % NXB, i % 4, i % 3
            S.dma("sp", xbuf[xi][:], xs[r0:r0 + 128, :], r=[dkey("xs")], w=[("xb", xi)])
            S.op("act", lambda: nc.scalar.activation(out=junk[:], in_=xbuf[xi][:], func=AF.Square, accum_out=ssb[si][:]),
                 r=[("xb", xi)], w=[("junk",), ("ss", si)])
            rms_rstd((ssb[si][:], ("ss", si)), (rsb[si][:], ("rs", si)), D)
            S.op("dve", lambda: nc.vector.scalar_tensor_tensor(out=ob[oi][:], in0=xbuf[xi][:], scalar=rsb[si][:], in1=gf[:],
                                                               op0=ALU.mult, op1=ALU.mult),
                 r=[("xb", xi), ("rs", si), ("gf",)], w=[("ob", oi)])
            S.dma("sp", out_d[r0:r0 + 128, :], ob[oi][:], r=[("ob", oi)], w=[dkey("out")])
        S.barrier()
    return nc, S


def _consts():
    c = np.zeros((128, 8), np.float32)
    p = np.arange(128)
    j = p % 16
    c[:, 0] = (10000.0 ** (-(2.0 * j) / 32.0)).astype(np.float32)
    is_sin = p >= 64
    sgn = np.where((p % 32) < 16, -1.0, 1.0)
    c[:, 2] = np.where(is_sin, sgn, -1.0)
    c[:, 3] = np.where(is_sin, 0.0, np.pi / 2)
    c[:, 4] = np.where((p // 32) % 2 == 1, MLA_SCALE, 1.0)
    return c


def _prep_shared(inp, L):
    w_in = np.asarray(inp["w_in"])
    offs = np.cumsum([256, 128, 32, 512, 512, 512, 8, 1024, 1024])[:-1]
    cq, ckv, kr, fq, fk, fv, flg, ga, gb = np.split(w_in, offs, axis=-1)
    kr_perm = np.concatenate([kr[..., 16:], kr[..., :16]], axis=-1)
    wa = np.ascontiguousarray(np.concatenate([cq, ckv, kr, kr_perm, fq, fk, fv, ga, gb, flg], axis=-1))
    assert wa.shape[-1] == CA
    w_uq = np.asarray(inp["w_uq"]).reshape(L, QL, NH, 96)
    nope, rope = w_uq[..., :64], w_uq[..., 64:]
    rope_perm = np.concatenate([rope[..., 16:], rope[..., :16]], axis=-1)
    wq = np.ascontiguousarray(np.concatenate([nope, rope, rope_perm], axis=-1).reshape(L, QL, NH * 128))
    w_ukv = np.asarray(inp["w_ukv"]).reshape(L, KVL, NH, 128)
    wkv = np.ascontiguousarray(np.concatenate([w_ukv[..., :64].reshape(L, KVL, 512),
                                               w_ukv[..., 64:].reshape(L, KVL, 512)], axis=-1))
    gq = np.asarray(inp["q_norm_g"])
    gqT = np.ascontiguousarray(gq.reshape(L, 2, 128).transpose(2, 0, 1).reshape(128, L * 2))
    gkvT = np.ascontiguousarray(np.asarray(inp["kv_norm_g"]).T)
    shared = {
        "ada_w": np.asarray(inp["ada_w"]), "ada_b": np.asarray(inp["ada_b"]),
        "norm_mix_g": np.asarray(inp["norm_mix_g"]), "norm_ffn_g": np.asarray(inp["norm_ffn_g"]),
        "wa": wa, "b_forget": np.asarray(inp["b_forget"]), "q_norm_gT": gqT, "wq": wq, "kv_norm_gT": gkvT, "wkv": wkv,
        "w_branch_mla": np.asarray(inp["w_branch_mla"]), "w_branch_fox": np.asarray(inp["w_branch_fox"]),
        "w_out": np.asarray(inp["w_out"]),
        "dense_w_gate": np.asarray(inp["dense_w_gate"]), "dense_w_up": np.asarray(inp["dense_w_up"]),
        "dense_w_down": np.asarray(inp["dense_w_down"]),
        "final_norm_g": np.asarray(inp["final_norm_g"]), "consts": _consts(),
    }
    if L // 2 > 0:
        shared["moe_w_routerT"] = np.ascontiguousarray(np.asarray(inp["moe_w_router"]).transpose(0, 2, 1))
        shared["moe_w_gate"] = np.asarray(inp["moe_w_gate"])
        shared["moe_w_up"] = np.asarray(inp["moe_w_up"])
        shared["moe_w_down"] = np.asarray(inp["moe_w_down"])
    else:
        shared["moe_w_routerT"] = np.zeros((1, NEXP, D), np.float32)
    return shared


def run(inp, cfg, extra_out=()):
    L = cfg.depth
    shared = _prep_shared(inp, L)
    x = np.asarray(inp["x"])
    c = np.asarray(inp["c"])
    pos = np.asarray(inp["positions"]).astype(np.int32)
    in_maps = []
    for i in range(cfg.ncores):
        sl = slice(i * cfg.nseq, (i + 1) * cfg.nseq)
        m = dict(shared)
        m["x"] = np.ascontiguousarray(x[sl].reshape(cfg.T, D))
        m["cT"] = np.ascontiguousarray(c[sl].T)
        m["pos"] = np.ascontiguousarray(pos[sl])
        in_maps.append(m)
    nc, S = build_program(cfg)
    res = run_bass_kernel_spmd(nc, in_maps, core_ids=list(range(cfg.ncores)))
    out = np.concatenate([r["out"].reshape(cfg.nseq, cfg.seq, D) for r in res.results], axis=0)
    if extra_out:
        return out, [{k: r[k] for k in extra_out} for r in res.results]
    return out


def kernel(**inputs):
    B, SEQ, _ = inputs["x"].shape
    L = inputs["ada_w"].shape[0]
    cfg = Cfg(nseq=B // 8, seq=SEQ, depth=L, ncores=8)
    out = run(inputs, cfg)
    return out.astype(np.float32)
```

```python
import contextlib
import numpy as np
import concourse.bass as bass
import concourse.mybir as mybir
from concourse.bass_utils import run_bass_kernel_spmd

F32 = mybir.dt.float32
BF16 = mybir.dt.bfloat16
I32 = mybir.dt.int32
AF = mybir.ActivationFunctionType
ALU = mybir.AluOpType

D = 1024
NH = 8
QL = 256
KVL = 128
ROPE = 32
DFF = 3584
NEXP = 8
EPS = 1e-6
MLA_SCALE = 96 ** -0.5
FOX_SCALE = 64 ** -0.5
NEG = -30000.0
CW1 = 6.28125
CW2 = float(2.0 * np.pi - 6.28125)
PI_LO = 3.1415925

CA_CQ = 0
CA_CKV = 256
CA_KR = 384
CA_FQ = 448
CA_FK = 960
CA_FV = 1472
CA_GA = 1984
CA_GB = 3008
CA_FL = 4032
CA = 4040
CA_PAD = 4096


class Cfg:
    def __init__(self, nseq=2, seq=4096, depth=4, ncores=8, debug=False):
        self.nseq, self.seq, self.depth, self.ncores, self.debug = nseq, seq, depth, ncores, debug
        self.kt = 6
        self.stop = None
        self.T = nseq * seq


class Sched:
    COMPUTE = ("pe", "act", "dve", "pool")

    def __init__(self, nc, n_dma_sems=44, n_sw=12, prefix=""):
        self.nc = nc
        self.prefix = prefix
        self.eng = {"pe": nc.tensor, "act": nc.scalar, "dve": nc.vector, "pool": nc.gpsimd, "sp": nc.sync}
        self.sems = {}
        self.cnt = {}
        for e in self.COMPUTE:
            self.sems[e] = nc.alloc_semaphore(prefix + "c_" + e)
            self.cnt[e] = 0
        self.dma_pool = {"hw": [], "sw": []}
        for kind, n in (("hw", n_dma_sems), ("sw", n_sw)):
            for i in range(n):
                k = "%s%d" % (kind, i)
                self.sems[k] = nc.alloc_semaphore(prefix + k)
                self.cnt[k] = 0
                self.dma_pool[kind].append(k)
        self.dma_map = {}
        self.dma_next = {"hw": 0, "sw": 0}
        self.waited = {}
        self.res = {}
        self.n_wait = 0
        self.n_ins = 0

    def _wait(self, eng, dep):
        semk, val, deng = dep
        if semk == "pe" and eng == "pe":
            return
        key = (eng, semk)
        if self.waited.get(key, 0) >= val:
            return
        self.eng[eng].wait_ge(self.sems[semk], val)
        self.waited[key] = val
        self.n_wait += 1

    def _deps(self, eng, r, w):
        for k in r:
            st = self.res.get(k)
            if st is not None and st[0] is not None:
                self._wait(eng, st[0])
        for k in w:
            st = self.res.get(k)
            if st is not None:
                if st[0] is not None:
                    self._wait(eng, st[0])
                for semk, (val, deng) in st[1].items():
                    self._wait(eng, (semk, val, deng))

    def _record(self, comp, r, w):
        for k in r:
            st = self.res.get(k)
            if st is None:
                st = [None, {}]
                self.res[k] = st
            old = st[1].get(comp[0])
            if old is None or old[0] < comp[1]:
                st[1][comp[0]] = (comp[1], comp[2])
        for k in w:
            self.res[k] = [comp, {}]

    def op(self, eng, fn, r=(), w=()):
        self._deps(eng, r, w)
        ins = fn()
        self.cnt[eng] += 1
        ins.then_inc(self.sems[eng], 1)
        self._record((eng, self.cnt[eng], eng), r, w)
        self.n_ins += 1
        return ins

    def dma(self, eng, out, in_, r=(), w=(), key=None, **kw):
        if key is None:
            key = w[0] if (len(w) and isinstance(w[0], tuple) and w[0][0] != "dram") else r[0]
        r = [k for k in r if k[0] != "dram"]
        w = [k for k in w if k[0] != "dram"]
        kind = "sw" if eng == "pool" else "hw"
        key = (kind, key)
        semk = self.dma_map.get(key)
        if semk is None:
            assert self.dma_next[kind] < len(self.dma_pool[kind]), "out of DMA semaphores"
            semk = self.dma_pool[kind][self.dma_next[kind]]
            self.dma_next[kind] += 1
            self.dma_map[key] = semk
        self._deps(eng, r, w)
        ins = self.eng[eng].dma_start(out=out, in_=in_, **kw)
        self.cnt[semk] += 16
        ins.then_inc(self.sems[semk], 16)
        self._record((semk, self.cnt[semk], "dma"), r, w)
        self.n_ins += 1
        return ins

    def dma_indirect(self, eng, out, out_offset, in_, in_offset, r=(), w=(), key=None, bound=None):
        kind = "sw"
        r = [k for k in r if k[0] != "dram"]
        w = [k for k in w if k[0] != "dram"]
        key = (kind, key)
        semk = self.dma_map.get(key)
        if semk is None:
            assert self.dma_next[kind] < len(self.dma_pool[kind]), "out of DMA semaphores"
            semk = self.dma_pool[kind][self.dma_next[kind]]
            self.dma_next[kind] += 1
            self.dma_map[key] = semk
        self._deps(eng, r, w)
        ins = self.nc.gpsimd.indirect_dma_start(out=out, out_offset=out_offset, in_=in_, in_offset=in_offset,
                                                bounds_check=bound, oob_is_err=False if bound is not None else True)
        self.cnt[semk] += 16
        ins.then_inc(self.sems[semk], 16)
        self._record((semk, self.cnt[semk], "dma"), r, w)
        self.n_ins += 1
        return ins

    def barrier(self, engines=("pe", "act", "dve", "pool", "sp")):
        for e in engines:
            for semk, c in self.cnt.items():
                if c > 0:
                    self._wait(e, (semk, c, "x"))
        self.res = {}
        self.dma_map = {}
        self.dma_next = {"hw": 0, "sw": 0}


    def finish_local_block(self):
        self.barrier()
        self.nc.all_engine_barrier()
        for semk, c in self.cnt.items():
            if c > 0:
                owner = semk if semk in self.COMPUTE else "sp"
                self.eng[owner].sem_clear(self.sems[semk])
        self.nc.all_engine_barrier()
        for k in self.cnt:
            self.cnt[k] = 0
        self.waited = {}
        self.res = {}


class _Stop(Exception):
    pass


def build_program(cfg):
    try:
        return _build_program(cfg)
    except _Stop as e:
        return e.args


def _build_program(cfg):
    nc = bass.Bass("TRN2", target_bir_lowering=False)
    S = Sched(nc)
    S2 = Sched(nc, n_dma_sems=10, n_sw=0, prefix="L_")
    NSEQ, SEQ, L, T = cfg.nseq, cfg.seq, cfg.depth, cfg.T
    NT = SEQ // 512
    n_dense, n_moe = (L + 1) // 2, L // 2
    dbg = cfg.debug

    uid = [0]

    def SB(name, shape, dt):
        uid[0] += 1
        return nc.sbuf_tensor("%s_u%d" % (name, uid[0]), shape, dt)

    def PS(name, shape, dt):
        uid[0] += 1
        return nc.psum_tensor("%s_u%d" % (name, uid[0]), shape, dt)

    def dram(name, shape, dt, kind="Internal"):
        return nc.dram_tensor(name, list(shape), dt, kind=kind).ap()

    scr_kind = "ExternalOutput" if dbg else "Internal"
    x_in = dram("x", [T, D], F32, "ExternalInput")
    cT_in = dram("cT", [D, NSEQ], F32, "ExternalInput")
    pos_in = dram("pos", [NSEQ, SEQ], I32, "ExternalInput")
    ada_w = dram("ada_w", [L, 2, D, 3 * D], F32, "ExternalInput")
    ada_b = dram("ada_b", [L, 2, 3 * D], F32, "ExternalInput")
    g_mix = dram("norm_mix_g", [L, D], F32, "ExternalInput")
    g_ffn = dram("norm_ffn_g", [L, D], F32, "ExternalInput")
    wa_in = dram("wa", [L, D, CA], F32, "ExternalInput")
    bfg_in = dram("b_forget", [L, NH], F32, "ExternalInput")
    gq_in = dram("q_norm_gT", [128, L * 2], F32, "ExternalInput")
    wq_in = dram("wq", [L, QL, NH * 128], F32, "ExternalInput")
    gkv_in = dram("kv_norm_gT", [128, L], F32, "ExternalInput")
    wkv_in = dram("wkv", [L, KVL, 1024], F32, "ExternalInput")
    wbm_in = dram("w_branch_mla", [L, 512, D], F32, "ExternalInput")
    wbf_in = dram("w_branch_fox", [L, 512, D], F32, "ExternalInput")
    wo_in = dram("w_out", [L, D, D], F32, "ExternalInput")
    dwg_in = dram("dense_w_gate", [n_dense, D, DFF], F32, "ExternalInput")
    dwu_in = dram("dense_w_up", [n_dense, D, DFF], F32, "ExternalInput")
    dwd_in = dram("dense_w_down", [n_dense, DFF, D], F32, "ExternalInput")
    n_moe_a = max(n_moe, 1)
    wr_in = dram("moe_w_routerT", [n_moe_a, NEXP, D], F32, "ExternalInput")
    if n_moe > 0:
        mwg_in = dram("moe_w_gate", [n_moe_a, NEXP, D, DFF], F32, "ExternalInput")
        mwu_in = dram("moe_w_up", [n_moe_a, NEXP, D, DFF], F32, "ExternalInput")
        mwd_in = dram("moe_w_down", [n_moe_a, NEXP, DFF, D], F32, "ExternalInput")
    gfin_in = dram("final_norm_g", [D], F32, "ExternalInput")
    cst_in = dram("consts", [128, 8], F32, "ExternalInput")
    out_d = dram("out", [T, D], F32, "ExternalOutput")
    xs = dram("xs", [T, D], F32, scr_kind)
    tabs = dram("tabs", [NSEQ, 128, SEQ], F32, scr_kind)
    modrows = dram("modrows", [L, 2, NSEQ, 3, D], F32, scr_kind)
    kropeT = dram("kropeT", [NSEQ, 32, SEQ], BF16, scr_kind)
    knopeT = dram("knopeT", [NSEQ, 512, SEQ], BF16, scr_kind)
    qmT = dram("qmT", [NSEQ, NH, 96, SEQ], BF16, scr_kind)
    vmla = dram("vmla", [NSEQ, SEQ, 1024], BF16, scr_kind)
    fqT = dram("fqT", [NSEQ, 512, SEQ], BF16, scr_kind)
    fkT = dram("fkT", [NSEQ, 512, SEQ], BF16, scr_kind)
    vfox = dram("vfox", [NSEQ, SEQ, 1024], BF16, scr_kind)
    L3 = dram("L3", [NSEQ, NH, 3, SEQ], BF16, scr_kind)
    nL3 = dram("nL3", [NSEQ, NH, 3, SEQ], BF16, scr_kind)
    sgT = dram("sgT", [NSEQ, 2048, SEQ], BF16, scr_kind)
    yT = dram("yT", [NSEQ, 1024, SEQ], BF16, scr_kind)
    h2T = dram("h2T", [D, T], BF16, scr_kind)
    comb = dram("comb", [T, NEXP], F32, scr_kind)
    CAPT = T // 512
    CAPR = CAPT * 512
    KT = min(CAPT, cfg.kt)
    if n_moe > 0:
        hg = dram("hg", [NEXP * CAPR, D], BF16, "Internal")
        og = dram("og", [NEXP * CAPR, D], F32, "Internal")
        hgT = dram("hgT", [D, NEXP * CAPR], BF16, "Internal")
        gidx = dram("gidx", [T, 2], I32, scr_kind)
        gwd = dram("gwd", [T, 2], F32, scr_kind)
        cnts = dram("cnts", [1, NEXP], I32, scr_kind)
        cnt_reg = nc.alloc_registers("cnt_e", mybir.ALL_ENGINES)

    def dkey(name, *idx):
        return ("dram", name) + tuple(idx)

    def stop_here(tag):
        if getattr(cfg, "stop", None) == tag:
            S.barrier()
            raise _Stop(nc, S)

    cst = nc.alloc_sbuf_tensor("cst", [128, 8], F32)
    ident = nc.alloc_sbuf_tensor("ident", [128, 128], BF16)
    ones_bf = nc.alloc_sbuf_tensor("ones_bf", [128, 128], BF16)
    maskT = nc.alloc_sbuf_tensor("maskT", [128, 128], BF16)
    zero_bf = nc.alloc_sbuf_tensor("zero_bf", [128, 128], BF16)
    ones_f = nc.alloc_sbuf_tensor("ones_f", [128, 512], F32)

    epsc = nc.alloc_sbuf_tensor("epsc", [128, 1], F32)
    S.op("pool", lambda: nc.gpsimd.memset(epsc[:], EPS), w=[("epsc",)])
    cnt_sb = nc.alloc_sbuf_tensor("cnt_sb", [1, NEXP], I32)
    ustr = nc.alloc_sbuf_tensor("ustr", [128, 128], F32)
    onesq = nc.alloc_sbuf_tensor("onesq", [128, 128], F32)
    eoff = nc.alloc_sbuf_tensor("eoff", [128, NEXP], F32)
    S.op("pool", lambda: nc.gpsimd.memset(onesq[:], 1.0), w=[("onesq",)])
    S.op("pool", lambda: nc.gpsimd.affine_select(out=ustr[:], in_=onesq[:], pattern=[[1, 128]],
                                                 compare_op=ALU.is_gt, fill=0.0, base=0, channel_multiplier=-1),
         r=[("onesq",)], w=[("ustr",)])
    for e in range(NEXP):
        S.op("pool", lambda e=e: nc.gpsimd.memset(eoff[:, e:e + 1], float(e * (T // 512) * 512)), w=[("eoff",)])
    S.dma("sp", cst[:], cst_in[:, :], r=[dkey("cst")], w=[("cst",)])
    S.op("pool", lambda: nc.gpsimd.memset(zero_bf[:], 0.0), w=[("zero_bf",)])
    S.op("pool", lambda: nc.gpsimd.memset(ones_bf[:], 1.0), w=[("ones_bf",)])
    S.op("pool", lambda: nc.gpsimd.memset(ones_f[:], 1.0), w=[("ones_f",)])
    S.op("pool", lambda: nc.gpsimd.affine_select(out=ident[:], in_=zero_bf[:], pattern=[[-1, 128]],
                                                 compare_op=ALU.not_equal, fill=1.0, base=0,
                                                 channel_multiplier=1),
         r=[("zero_bf",)], w=[("ident",)])
    S.op("pool", lambda: nc.gpsimd.affine_select(out=maskT[:], in_=zero_bf[:], pattern=[[1, 128]],
                                                 compare_op=ALU.is_ge, fill=NEG, base=0,
                                                 channel_multiplier=-1),
         r=[("zero_bf",)], w=[("maskT",)])
    S.barrier()

    if n_moe > 0:
        ztf = nc.alloc_sbuf_tensor("zfill_f", [128, D], F32)
        S.op("pool", lambda: nc.gpsimd.memset(ztf[:], 0.0), w=[("zfill_f",)])
        hg2 = hg.rearrange("(n p r) d -> n p (r d)", p=128, r=2)
        for i in range(NEXP * CAPR // 256):
            S.dma("sp", hg2[i], ztf[:].bitcast(BF16), r=[("zfill_f",)], w=[dkey("hg")])
        og1 = og.rearrange("(n p) d -> n p d", p=128)
        for i in range(NEXP * CAPR // 128):
            S.dma("act", og1[i], ztf[:], r=[("zfill_f",)], w=[dkey("og")])

    def load_w(stack, name, src2d, K, N, eng="pool", npad=None):
        kc = K // 128
        t = stack.enter_context(SB(name, [128, kc, npad or N], BF16))
        for k in range(kc):
            S.dma(eng, t[:, k, 0:N], src2d[k * 128:(k + 1) * 128, :], r=[dkey(name)], w=[(name,)], key=(name,))
        return t

    def rms_rstd(ss, rstd, n):
        S.op("act", lambda: nc.scalar.activation(out=rstd[0], in_=ss[0], func=AF.Ln, bias=epsc[0:rstd[0].shape[0], 0:1],
                                                 scale=1.0 / n), r=[ss[1], ("epsc",)], w=[rstd[1]])
        S.op("act", lambda: nc.scalar.activation(out=rstd[0], in_=rstd[0], func=AF.Exp, scale=-0.5),
             r=[rstd[1]], w=[rstd[1]])

    with contextlib.ExitStack() as st:
        posi = st.enter_context(SB("posi", [128, SEQ], I32))
        ang = st.enter_context(SB("ang", [128, SEQ], F32))
        tb = st.enter_context(SB("tb", [128, SEQ], F32))
        for b in range(NSEQ):
            S.dma("sp", posi[:], pos_in[b, :].partition_broadcast(128), r=[dkey("pos")], w=[("posi",)])
            S.op("dve", lambda: nc.vector.tensor_copy(out=ang[:], in_=posi[:]), r=[("posi",)], w=[("ang",)])
            S.op("dve", lambda: nc.vector.tensor_scalar(out=ang[:], in0=ang[:], scalar1=cst[:, 0:1], scalar2=None,
                                                        op0=ALU.mult), r=[("ang",), ("cst",)], w=[("ang",)])
            S.op("dve", lambda: nc.vector.tensor_scalar(out=tb[:], in0=ang[:], scalar1=float(1.0 / (2.0 * np.pi)), scalar2=None,
                                                        op0=ALU.mult), r=[("ang",)], w=[("tb",)])
            S.op("dve", lambda: nc.vector.tensor_copy(out=posi[:], in_=tb[:]), r=[("tb",)], w=[("posi",)])
            S.op("dve", lambda: nc.vector.tensor_copy(out=tb[:], in_=posi[:]), r=[("posi",)], w=[("tb",)])
            S.op("dve", lambda: nc.vector.scalar_tensor_tensor(out=ang[:], in0=tb[:], scalar=-CW1, in1=ang[:],
                                                               op0=ALU.mult, op1=ALU.add), r=[("tb",), ("ang",)], w=[("ang",)])
            S.op("dve", lambda: nc.vector.scalar_tensor_tensor(out=ang[:], in0=tb[:], scalar=-CW2, in1=ang[:],
                                                               op0=ALU.mult, op1=ALU.add), r=[("tb",), ("ang",)], w=[("ang",)])
            S.op("dve", lambda: nc.vector.tensor_scalar(out=ang[:], in0=ang[:], scalar1=-PI_LO, scalar2=PI_LO,
                                                        op0=ALU.max, op1=ALU.min), r=[("ang",)], w=[("ang",)])
            S.op("dve", lambda: nc.vector.scalar_tensor_tensor(out=ang[0:64, :], in0=ang[0:64, :], scalar=-1.0,
                                                               in1=ang[0:64, :], op0=ALU.mult, op1=ALU.max),
                 r=[("ang",)], w=[("ang",)])
            S.op("act", lambda: nc.scalar.activation(out=tb[:], in_=ang[:], func=AF.Sin, bias=cst[:, 3:4],
                                                     scale=cst[:, 2:3]), r=[("ang",), ("cst",)], w=[("tb",)])
            S.op("dve", lambda: nc.vector.tensor_scalar(out=tb[:], in0=tb[:], scalar1=cst[:, 4:5], scalar2=None,
                                                        op0=ALU.mult), r=[("tb",), ("cst",)], w=[("tb",)])
            S.dma("sp", tabs[b, :, :], tb[:], r=[("tb",)], w=[dkey("tabs", b)])
        S.barrier()
    stop_here("p0")

    with contextlib.ExitStack() as st:
        cTs = st.enter_context(SB("cTs", [128, 8, NSEQ], F32))
        scT = st.enter_context(SB("scT", [128, 8, NSEQ], F32))
        wch = [st.enter_context(SB("wch%d" % i, [128, 1536], F32)) for i in range(4)]
        mod = st.enter_context(SB("mod", [NSEQ, 3 * D], F32))
        bia = st.enter_context(SB("bia", [NSEQ, 3 * D], F32))
        gbc = st.enter_context(SB("gbc", [NSEQ, D], F32))
        arow = st.enter_context(SB("arow", [NSEQ, D], F32))
        pmod = [st.enter_context(PS("pmod%d" % i, [NSEQ, 512], F32)) for i in range(3)]
        with nc.allow_non_contiguous_dma(reason="tiny transposed conditioning vector"):
            S.dma("sp", cTs[:], cT_in.rearrange("(k p) b -> p k b", p=128), r=[dkey("cT")], w=[("cTs",)])
        S.op("act", lambda: nc.scalar.activation(out=scT[:], in_=cTs[:], func=AF.Silu), r=[("cTs",)], w=[("scT",)])
        wi = 0
        for l in range(L):
            for sub in range(2):
                S.dma("sp", bia[:], ada_b[l, sub, :].partition_broadcast(NSEQ), r=[dkey("ada_b")], w=[("bia",)])
                gsrc = g_mix if sub == 0 else g_ffn
                S.dma("sp", gbc[:], gsrc[l, :].partition_broadcast(NSEQ), r=[dkey("g")], w=[("gbc",)])
                for half in range(2):
                    for kc in range(8):
                        wt = wch[wi % 4]
                        wk = ("wch", wi % 4)
                        wi += 1
                        S.dma("sp", wt[:], ada_w[l, sub, kc * 128:(kc + 1) * 128, half * 1536:(half + 1) * 1536],
                              r=[dkey("ada_w")], w=[wk])
                        for j in range(3):
                            S.op("pe", lambda j=j, wt=wt, kc=kc: nc.tensor.matmul(
                                pmod[j][:], lhsT=scT[:, kc, :], rhs=wt[:, j * 512:(j + 1) * 512],
                                start=(kc == 0), stop=(kc == 7)), r=[wk, ("scT",)], w=[("pmod", j)])
                    for j in range(3):
                        c0 = half * 1536 + j * 512
                        S.op("dve", lambda j=j, c0=c0: nc.vector.tensor_tensor(
                            out=mod[:, c0:c0 + 512], in0=pmod[j][:], in1=bia[:, c0:c0 + 512], op=ALU.add),
                            r=[("pmod", j), ("bia",)], w=[("mod",)])
                S.op("dve", lambda: nc.vector.scalar_tensor_tensor(out=arow[:], in0=mod[:, D:2 * D], scalar=1.0,
                                                                   in1=gbc[:], op0=ALU.add, op1=ALU.mult),
                     r=[("mod",), ("gbc",)], w=[("arow",)])
                S.dma("sp", modrows[l, sub, :, 0, :], arow[:], r=[("arow",)], w=[dkey("modrows", l, sub)])
                S.dma("sp", modrows[l, sub, :, 1, :], mod[:, 0:D], r=[("mod",)], w=[dkey("modrows", l, sub)])
                S.dma("sp", modrows[l, sub, :, 2, :], mod[:, 2 * D:3 * D], r=[("mod",)], w=[dkey("modrows", l, sub)])
        S.barrier()
    stop_here("p1")

    def norm_block(xb, xk, ss, ssk, rstd, rk, junk, jk, tmp, tk, hb, hk, abc, bbc, bck):
        S.op("act", lambda: nc.scalar.activation(out=junk, in_=xb, func=AF.Square, accum_out=ss),
             r=[xk], w=[jk, ssk])
        rms_rstd((ss, ssk), (rstd, rk), D)
        S.op("dve", lambda: nc.vector.scalar_tensor_tensor(out=tmp, in0=xb, scalar=rstd, in1=abc,
                                                           op0=ALU.mult, op1=ALU.mult),
             r=[xk, rk, bck], w=[tk])
        if bbc is None:
            S.op("pool", lambda: nc.gpsimd.tensor_copy(out=hb, in_=tmp), r=[tk], w=[hk])
        else:
            S.op("pool", lambda: nc.gpsimd.tensor_tensor(out=hb, in0=tmp, in1=bbc, op=ALU.add),
                 r=[tk, bck], w=[hk])

    for l in range(L):
        xsrc = x_in if l == 0 else xs
        xsrc_key = "x_in" if l == 0 else "xs"

        with contextlib.ExitStack() as st:
            WA = load_w(st, "WA", wa_in[l], D, CA, npad=CA_PAD)
            WQ = load_w(st, "WQ", wq_in[l], QL, NH * 128)
            WKV = load_w(st, "WKV", wkv_in[l], KVL, 1024)
            gq = st.enter_context(SB("gq", [128, 2 * L], F32))
            gkv = st.enter_context(SB("gkv", [128, L], F32))
            nbf = st.enter_context(SB("nbf", [NH, 1], F32))
            S.dma("sp", gq[:], gq_in[:, :], r=[dkey("gq")], w=[("gq",)])
            S.dma("sp", gkv[:], gkv_in[:, :], r=[dkey("gkv")], w=[("gkv",)])
            with nc.allow_non_contiguous_dma(reason="8-element bias column"):
                S.dma("sp", nbf[:], bfg_in[l, :].rearrange("(h o) -> h o", o=1), r=[dkey("bf")], w=[("nbf",)])
            S.op("dve", lambda: nc.vector.tensor_scalar(out=nbf[:], in0=nbf[:], scalar1=-1.0, scalar2=None,
                                                        op0=ALU.mult), r=[("nbf",)], w=[("nbf",)])
            NXB = 5
            xbuf = [st.enter_context(SB("xb%d" % i, [128, D], F32)) for i in range(NXB)]
            junk = st.enter_context(SB("junk", [128, D], BF16))
            tmpf = [st.enter_context(SB("tmpf%d" % i, [128, D], F32)) for i in range(2)]
            hbuf = [st.enter_context(SB("hb%d" % i, [128, D], BF16)) for i in range(2)]
            ssb = [st.enter_context(SB("ss%d" % i, [128, 1], F32)) for i in range(4)]
            rsb = [st.enter_context(SB("rs%d" % i, [128, 1], F32)) for i in range(4)]
            hT = [st.enter_context(SB("hT%d" % i, [128, 8, 512], BF16)) for i in range(2)]
            abc = st.enter_context(SB("abc", [128, D], F32))
            bbc = st.enter_context(SB("bbc", [128, D], F32))
            tabt = [st.enter_context(SB("tabt%d" % i, [128, 512], F32)) for i in range(2)]
            craw = st.enter_context(SB("craw", [128, 3, 512], F32))
            sq = st.enter_context(SB("sq", [128, 3, 512], BF16))
            rq = st.enter_context(SB("rq", [128, 2, 512], F32))
            cqn = st.enter_context(SB("cqn", [128, 2, 512], BF16))
            ckvn = st.enter_context(SB("ckvn", [128, 512], BF16))
            rt = [st.enter_context(SB("rt%d" % i, [32, 512], F32)) for i in range(4)]
            NST = 8
            stg = [st.enter_context(SB("stg%d" % i, [128, 512], BF16)) for i in range(NST)]
            vst = [st.enter_context(SB("vst%d" % i, [128, NH, 128], BF16)) for i in range(4)]
            fl = [st.enter_context(SB("fl%d" % i, [NH, 512], F32)) for i in range(3)]
            Lt = [st.enter_context(SB("Lt%d" % i, [NH, 512], F32)) for i in range(2)]
            l3 = st.enter_context(SB("l3", [NH, 6, 512], BF16))
            pst = st.enter_context(PS("pst", [128, D], BF16))
            pp = [st.enter_context(PS("pp%d" % i, [128, 512], F32)) for i in range(7)]
            for i in range(4):
                S.op("pool", lambda i=i: nc.gpsimd.memset(vst[i][:, :, 64:128], 1.0), w=[("vst", i)])
            cnt = {"x": 0, "pp": 0, "stg": 0, "vst": 0, "ss": 0, "rt": 0}

            def nxt(name, n):
                v = cnt[name] % n
                cnt[name] += 1
                return v

            def stage1(b, tt, hTi):
                for blk in range(4):
                    r0 = b * SEQ + tt * 512 + blk * 128
                    xi = nxt("x", NXB)
                    S.dma("sp", xbuf[xi][:], xsrc[r0:r0 + 128, :], r=[dkey(xsrc_key)], w=[("xb", xi)])
                    si = nxt("ss", 4)
                    hi = blk % 2
                    norm_block(xbuf[xi][:], ("xb", xi), ssb[si][:], ("ss", si), rsb[si][:], ("rs", si),
                               junk[:], ("junk",), tmpf[hi][:], ("tmpf", hi), hbuf[hi][:], ("hb", hi),
                               abc[:], bbc[:], ("abc",))
                    for kc in range(8):
                        S.op("pe", lambda kc=kc, hi=hi: nc.tensor.transpose(
                            pst[:, kc * 128:(kc + 1) * 128], hbuf[hi][:, kc * 128:(kc + 1) * 128], ident[:]),
                            r=[("hb", hi), ("ident",)], w=[("pst",)])
                    S.op("act", lambda blk=blk: nc.scalar.copy(
                        out=hT[hTi][:, :, blk * 128:(blk + 1) * 128],
                        in_=pst[:].rearrange("p (k t) -> p k t", k=8)),
                        r=[("pst",)], w=[("hT", hTi)])

            def group(c0, M, hTi):
                pi = nxt("pp", 7)
                for kc in range(8):
                    S.op("pe", lambda kc=kc: nc.tensor.matmul(pp[pi][0:M, :], lhsT=WA[:, kc, c0:c0 + M],
                                                              rhs=hT[hTi][:, kc, :], start=(kc == 0), stop=(kc == 7)),
                         r=[("WA",), ("hT", hTi)], w=[("pp", pi)])
                return pi

            def store_stage(si, nrows, dst, dk):
                S.dma("sp", dst, stg[si][0:nrows, :], r=[("stg", si)], w=[dk])

            def stage2(b, tt, hTi, carry):
                t0 = tt * 512
                tsl = slice(t0, t0 + 512)
                ti = tt % 2
                stop_here("A%ds2" % l)
                S.dma("sp", tabt[ti][:], tabs[b, :, tsl], r=[dkey("tabs", b)], w=[("tabt", ti)])
                tab = tabt[ti]
                tabk = ("tabt", ti)
                stop_here("A%dt" % l)
                for j in range(3):
                    pi = group(CA_CQ + j * 128, 128, hTi)
                    stop_here("A%dm" % l)
                    S.op("act", lambda j=j, pi=pi: nc.scalar.copy(out=craw[:, j, :], in_=pp[pi][:]),
                         r=[("pp", pi)], w=[("craw", j)])
                    stop_here("A%da" % l)
                    S.op("dve", lambda j=j: nc.vector.tensor_tensor(out=sq[:, j, :], in0=craw[:, j, :], in1=craw[:, j, :],
                                                                    op=ALU.mult), r=[("craw", j)], w=[("sq", j)])
                    stop_here("A%dd" % l)
                    if j == 1:
                        stop_here("A%dd1" % l)
                stop_here("A%dg" % l)
                pq = nxt("pp", 7)
                S.op("pe", lambda: nc.tensor.matmul(pp[pq][:], lhsT=ones_bf[:], rhs=sq[:, 0, :], start=True, stop=False),
                     r=[("sq", 0), ("ones_bf",)], w=[("pp", pq)])
                S.op("pe", lambda: nc.tensor.matmul(pp[pq][:], lhsT=ones_bf[:], rhs=sq[:, 1, :], start=False, stop=True),
                     r=[("sq", 1), ("ones_bf",)], w=[("pp", pq)])
                rms_rstd((pp[pq][:], ("pp", pq)), (rq[:, 0, :], ("rq", 0)), QL)
                pk = nxt("pp", 7)
                S.op("pe", lambda: nc.tensor.matmul(pp[pk][:], lhsT=ones_bf[:], rhs=sq[:, 2, :], start=True, stop=True),
                     r=[("sq", 2), ("ones_bf",)], w=[("pp", pk)])
                rms_rstd((pp[pk][:], ("pp", pk)), (rq[:, 1, :], ("rq", 1)), KVL)
                for j in range(2):
                    S.op("dve", lambda j=j: nc.vector.scalar_tensor_tensor(
                        out=cqn[:, j, :], in0=craw[:, j, :], scalar=gq[:, 2 * l + j:2 * l + j + 1], in1=rq[:, 0, :],
                        op0=ALU.mult, op1=ALU.mult), r=[("craw", j), ("gq",), ("rq", 0)], w=[("cqn", j)])
                S.op("dve", lambda: nc.vector.scalar_tensor_tensor(
                    out=ckvn[:], in0=craw[:, 2, :], scalar=gkv[:, l:l + 1], in1=rq[:, 1, :],
                    op0=ALU.mult, op1=ALU.mult), r=[("craw", 2), ("gkv",), ("rq", 1)], w=[("ckvn",)])

                def rope_evict(pi, dst, dk, cos_rows, sin_rows, pbase=0):
                    r1, r2 = nxt("rt", 4), nxt("rt", 4)
                    S.op("dve", lambda: nc.vector.tensor_tensor(out=rt[r1][:], in0=pp[pi][pbase:pbase + 32, :],
                                                                in1=tab[cos_rows, :], op=ALU.mult),
                         r=[("pp", pi), tabk], w=[("rt", r1)])
                    S.op("dve", lambda: nc.vector.tensor_tensor(out=rt[r2][:], in0=pp[pi][pbase + 32:pbase + 64, :],
                                                                in1=tab[sin_rows, :], op=ALU.mult),
                         r=[("pp", pi), tabk], w=[("rt", r2)])
                    S.op("pool", lambda: nc.gpsimd.tensor_tensor(out=dst, in0=rt[r1][:], in1=rt[r2][:], op=ALU.add),
                         r=[("rt", r1), ("rt", r2)], w=[dk])

                stop_here("A%d%s" % (l, "x1"))
                pi = group(CA_KR, 64, hTi)
                si = nxt("stg", NST)
                rope_evict(pi, stg[si][0:32, :], ("stg", si), slice(0, 32), slice(64, 96))
                store_stage(si, 32, kropeT[b, :, tsl], dkey("kropeT", b))
                stop_here("A%d%s" % (l, "x2"))
                for h in range(NH):
                    pi = nxt("pp", 7)
                    for kc in range(2):
                        S.op("pe", lambda kc=kc, h=h: nc.tensor.matmul(
                            pp[pi][:], lhsT=WQ[:, kc, h * 128:(h + 1) * 128], rhs=cqn[:, kc, :],
                            start=(kc == 0), stop=(kc == 1)), r=[("WQ",), ("cqn", kc)], w=[("pp", pi)])
                    si = nxt("stg", NST)
                    S.op("act", lambda pi=pi, si=si: nc.scalar.mul(out=stg[si][0:64, :], in_=pp[pi][0:64, :],
                                                                   mul=MLA_SCALE),
                         r=[("pp", pi)], w=[("stg", si)])
                    rope_evict(pi, stg[si][64:96, :], ("stg", si), slice(32, 64), slice(96, 128), 64)
                    store_stage(si, 96, qmT[b, h, :, tsl], dkey("qmT", b, h))
                stop_here("A%d%s" % (l, "x3"))
                for g in range(4):
                    pi = nxt("pp", 7)
                    S.op("pe", lambda g=g: nc.tensor.matmul(pp[pi][:], lhsT=WKV[:, 0, g * 128:(g + 1) * 128],
                                                            rhs=ckvn[:], start=True, stop=True),
                         r=[("WKV",), ("ckvn",)], w=[("pp", pi)])
                    si = nxt("stg", NST)
                    S.op("dve", lambda pi=pi, si=si: nc.vector.tensor_copy(out=stg[si][:], in_=pp[pi][:]),
                         r=[("pp", pi)], w=[("stg", si)])
                    store_stage(si, 128, knopeT[b, g * 128:(g + 1) * 128, tsl], dkey("knopeT", b, g))
                stop_here("A%d%s" % (l, "x4"))
                for blk in range(4):
                    pi = nxt("pp", 7)
                    S.op("pe", lambda blk=blk: nc.tensor.matmul(pp[pi][:], lhsT=ckvn[:, blk * 128:(blk + 1) * 128],
                                                                rhs=WKV[:, 0, 512:1024], start=True, stop=True),
                         r=[("WKV",), ("ckvn",)], w=[("pp", pi)])
                    vi = nxt("vst", 4)
                    S.op("act", lambda pi=pi, vi=vi: nc.scalar.copy(
                        out=vst[vi][:, :, 0:64], in_=pp[pi][:].rearrange("p (h d) -> p h d", h=NH)),
                        r=[("pp", pi)], w=[("vst", vi)])
                    r0 = t0 + blk * 128
                    S.dma("sp", vmla[b, r0:r0 + 128, :], vst[vi][:].rearrange("p h d -> p (h d)"),
                          r=[("vst", vi)], w=[dkey("vmla", b)])
                stop_here("A%d%s" % (l, "x5"))
                for g in range(4):
                    pi = group(CA_FQ + g * 128, 128, hTi)
                    si = nxt("stg", NST)
                    S.op("act", lambda pi=pi, si=si: nc.scalar.mul(out=stg[si][:], in_=pp[pi][:], mul=FOX_SCALE),
                         r=[("pp", pi)], w=[("stg", si)])
                    store_stage(si, 128, fqT[b, g * 128:(g + 1) * 128, tsl], dkey("fqT", b, g))
                for g in range(4):
                    pi = group(CA_FK + g * 128, 128, hTi)
                    si = nxt("stg", NST)
                    S.op("dve", lambda pi=pi, si=si: nc.vector.tensor_copy(out=stg[si][:], in_=pp[pi][:]),
                         r=[("pp", pi)], w=[("stg", si)])
                    store_stage(si, 128, fkT[b, g * 128:(g + 1) * 128, tsl], dkey("fkT", b, g))
                stop_here("A%d%s" % (l, "x6"))
                for blk in range(4):
                    pi = nxt("pp", 7)
                    for kc in range(8):
                        S.op("pe", lambda kc=kc, blk=blk: nc.tensor.matmul(
                            pp[pi][:], lhsT=hT[hTi][:, kc, blk * 128:(blk + 1) * 128],
                            rhs=WA[:, kc, CA_FV:CA_FV + 512], start=(kc == 0), stop=(kc == 7)),
                            r=[("WA",), ("hT", hTi)], w=[("pp", pi)])
                    vi = nxt("vst", 4)
                    S.op("act", lambda pi=pi, vi=vi: nc.scalar.copy(
                        out=vst[vi][:, :, 0:64], in_=pp[pi][:].rearrange("p (h d) -> p h d", h=NH)),
                        r=[("pp", pi)], w=[("vst", vi)])
                    r0 = t0 + blk * 128
                    S.dma("sp", vfox[b, r0:r0 + 128, :], vst[vi][:].rearrange("p h d -> p (h d)"),
                          r=[("vst", vi)], w=[dkey("vfox", b)])
                stop_here("A%d%s" % (l, "x7"))
                pi = group(CA_FL, NH, hTi)
                S.op("act", lambda: nc.scalar.activation(out=fl[0][:], in_=pp[pi][0:NH, :], func=AF.Exp,
                                                         bias=nbf[:, 0:1], scale=-1.0),
                     r=[("pp", pi), ("nbf",)], w=[("fl", 0)])
                S.op("act", lambda: nc.scalar.activation(out=fl[1][:], in_=fl[0][:], func=AF.Ln, bias=1.0, scale=1.0),
                     r=[("fl", 0)], w=[("fl", 1)])
                li = tt % 2
                init = 0.0 if carry is None else carry
                rr = [("fl", 1), ("ones_f",)] + ([("Lt", 1 - li)] if carry is not None else [])
                S.op("dve", lambda: nc.vector.tensor_tensor_scan(out=Lt[li][:], data0=ones_f[0:NH, :], data1=fl[1][:],
                                                                 initial=init, op0=ALU.mult, op1=ALU.subtract),
                     r=rr, w=[("Lt", li)])
                S.op("dve", lambda: nc.vector.tensor_copy(out=l3[:, 0, :], in_=Lt[li][:]), r=[("Lt", li)], w=[("l3",)])
                S.op("dve", lambda: nc.vector.tensor_tensor(out=fl[2][:], in0=Lt[li][:], in1=l3[:, 0, :], op=ALU.subtract),
                     r=[("Lt", li), ("l3",)], w=[("fl", 2)])
                S.op("dve", lambda: nc.vector.tensor_copy(out=l3[:, 1, :], in_=fl[2][:]), r=[("fl", 2)], w=[("l3",)])
                S.op("dve", lambda: nc.vector.tensor_tensor(out=fl[0][:], in0=fl[2][:], in1=l3[:, 1, :], op=ALU.subtract),
                     r=[("fl", 2), ("l3",)], w=[("fl", 0)])
                S.op("dve", lambda: nc.vector.tensor_copy(out=l3[:, 2, :], in_=fl[0][:]), r=[("fl", 0)], w=[("l3",)])
                S.op("dve", lambda: nc.vector.tensor_scalar(out=l3[:, 3:6, :], in0=l3[:, 0:3, :], scalar1=-1.0,
                                                            scalar2=None, op0=ALU.mult), r=[("l3",)], w=[("l3",)])
                S.dma("sp", L3[b, :, :, tsl], l3[:, 0:3, :], r=[("l3",)], w=[dkey("L3", b)], key=("l3",))
                S.dma("sp", nL3[b, :, :, tsl], l3[:, 3:6, :], r=[("l3",)], w=[dkey("nL3", b)], key=("l3",))
                stop_here("A%d%s" % (l, "x8"))
                for g in range(16):
                    pi = group(CA_GA + g * 128, 128, hTi)
                    si = nxt("stg", NST)
                    S.op("act", lambda pi=pi, si=si: nc.scalar.activation(out=stg[si][:], in_=pp[pi][:], func=AF.Sigmoid),
                         r=[("pp", pi)], w=[("stg", si)])
                    store_stage(si, 128, sgT[b, g * 128:(g + 1) * 128, tsl], dkey("sgT", b, g))
                return Lt[li][:, 511:512]

            stop_here("A%dw" % l)
            for b in range(NSEQ):
                S.dma("sp", abc[:], modrows[l, 0, b, 0, :].partition_broadcast(128), r=[dkey("modrows", l, 0)], w=[("abc",)])
                S.dma("sp", bbc[:], modrows[l, 0, b, 1, :].partition_broadcast(128), r=[dkey("modrows", l, 0)], w=[("abc",)],
                      key=("abc",))
                carry = None
                stage1(b, 0, 0)
                stop_here("A%ds1" % l)
                for tt in range(NT):
                    if tt + 1 < NT:
                        stage1(b, tt + 1, (tt + 1) % 2)
                    carry = stage2(b, tt, tt % 2, carry)
            S.barrier()
        stop_here("A%d" % l)

        with contextlib.ExitStack() as st:
            QA = [st.enter_context(SB("QA%d" % i, [128, SEQ], BF16)) for i in range(2)]
            KA = [st.enter_context(SB("KA%d" % i, [128, SEQ], BF16)) for i in range(2)]
            VA = [st.enter_context(SB("VA%d" % i, [128, SEQ // 128, 128], BF16)) for i in range(2)]
            NPT = 4
            NPS = 5
            LA = 3
            PT = [st.enter_context(SB("PT%d" % i, [128, 512], BF16)) for i in range(NPT)]
            rcp = [st.enter_context(SB("rcp%d" % i, [128, 512], F32)) for i in range(2)]
            yst = [st.enter_context(SB("yst%d" % i, [64, 512], BF16)) for i in range(3)]
            pS = [st.enter_context(PS("pS%d" % i, [128, 512], F32)) for i in range(NPS)]
            pO = [st.enter_context(PS("pO%d" % i, [128, 512], F32)) for i in range(2)]
            cB = {"po": 0, "y": 0}
            heads = [(b, mixer, h) for b in range(NSEQ) for mixer in range(2) for h in range(NH)]

            def load_head(n):
                b, mixer, h = heads[n]
                bi = n % 2
                if mixer == 0:
                    S.dma("sp", QA[bi][0:96, :], qmT[b, h, :, :], r=[dkey("qmT", b, h)], w=[("QA", bi)])
                    S.dma("sp", KA[bi][64:96, :], kropeT[b, :, :], r=[dkey("kropeT", b)], w=[("KA", bi)])
                    S.dma("sp", KA[bi][0:64, :], knopeT[b, h * 64:(h + 1) * 64, :],
                          r=[dkey("knopeT", b, h // 2)], w=[("KA", bi)])
                    vsrc = vmla
                else:
                    S.op("pool", lambda: nc.gpsimd.memset(QA[bi][64:96, :], 1.0), w=[("QA", bi)])
                    S.op("pool", lambda: nc.gpsimd.memset(KA[bi][64:96, :], 1.0), w=[("KA", bi)])
                    S.dma("sp", QA[bi][0:64, :], fqT[b, h * 64:(h + 1) * 64, :], r=[dkey("fqT", b, h // 2)],
                          w=[("QA", bi)])
                    S.dma("sp", QA[bi][64:67, :], L3[b, h, :, :], r=[dkey("L3", b)], w=[("QA", bi)])
                    S.dma("sp", KA[bi][0:64, :], fkT[b, h * 64:(h + 1) * 64, :], r=[dkey("fkT", b, h // 2)],
                          w=[("KA", bi)])
                    S.dma("sp", KA[bi][67:70, :], nL3[b, h, :, :], r=[dkey("nL3", b)], w=[("KA", bi)])
                    vsrc = vfox
                S.dma("sp", VA[bi][:], vsrc[b, :, h * 128:(h + 1) * 128].rearrange("(k p) d -> p k d", p=128),
                      r=[dkey("v", b)], w=[("VA", bi)])

            load_head(0)
            for n, (b, mixer, h) in enumerate(heads):
                if n + 1 < len(heads):
                    load_head(n + 1)
                bi = n % 2
                dq = 96 if mixer == 0 else 70
                tiles = []
                for qt in range(NT):
                    nkb = 4 * qt + 4
                    for kb in range(nkb):
                        tiles.append((qt, kb, nkb))

                def emit_qk(i):
                    qt, kb, nkb = tiles[i]
                    j = kb - 4 * qt
                    q0 = 0 if j < 0 else j * 128
                    n_ = 512 - q0
                    si = i % NPS
                    diag = j >= 0
                    S.op("pe", lambda: nc.tensor.matmul(
                        pS[si][:, 0:n_], lhsT=KA[bi][0:dq, kb * 128:(kb + 1) * 128],
                        rhs=QA[bi][0:dq, qt * 512 + q0:qt * 512 + 512],
                        start=True, stop=not diag), r=[("KA", bi), ("QA", bi)], w=[("pS", si)])
                    if diag:
                        S.op("pe", lambda: nc.tensor.matmul(
                            pS[si][:, 0:128], lhsT=ident[:], rhs=maskT[:], start=False, stop=True),
                            r=[("ident",), ("maskT",)], w=[("pS", si)])

                for i in range(min(LA, len(tiles))):
                    emit_qk(i)
                oi = None
                for i, (qt, kb, nkb) in enumerate(tiles):
                    if i + LA < len(tiles):
                        emit_qk(i + LA)
                    if kb == 0:
                        oi = cB["po"] % 2
                        cB["po"] += 1
                    j = kb - 4 * qt
                    q0 = 0 if j < 0 else j * 128
                    n_ = 512 - q0
                    si = i % NPS
                    pi = i % NPT
                    S.op("act", lambda: nc.scalar.activation(out=PT[pi][:, 0:n_], in_=pS[si][:, 0:n_], func=AF.Exp),
                         r=[("pS", si)], w=[("PT", pi)])
                    S.op("pe", lambda: nc.tensor.matmul(
                        pO[oi][:, q0:512], lhsT=VA[bi][:, kb, :], rhs=PT[pi][:, 0:n_],
                        start=(kb == 0), stop=(kb == nkb - 1)), r=[("VA", bi), ("PT", pi)], w=[("pO", oi)])
                    if kb == nkb - 1:
                        ri = oi
                        S.op("dve", lambda: nc.vector.reciprocal(out=rcp[ri][64:128, :], in_=pO[oi][64:128, :]),
                             r=[("pO", oi)], w=[("rcp", ri)])
                        yi = cB["y"] % 3
                        cB["y"] += 1
                        S.op("dve", lambda: nc.vector.tensor_tensor(out=yst[yi][:], in0=pO[oi][0:64, :],
                                                                    in1=rcp[ri][64:128, :], op=ALU.mult),
                             r=[("pO", oi), ("rcp", ri)], w=[("yst", yi)])
                        row0 = mixer * 512 + h * 64
                        S.dma("sp", yT[b, row0:row0 + 64, qt * 512:(qt + 1) * 512], yst[yi][:],
                              r=[("yst", yi)], w=[dkey("yT", b, row0 // 128)])
            S.barrier()
        stop_here("B%d" % l)

        with contextlib.ExitStack() as st:
            WBM = load_w(st, "WBM", wbm_in[l], 512, D)
            WBF = load_w(st, "WBF", wbf_in[l], 512, D)
            WO = load_w(st, "WO", wo_in[l], D, D)
            yt = [st.enter_context(SB("yt%d" % i, [128, 8, 512], BF16)) for i in range(2)]
            sgt = [st.enter_context(SB("sgt%d" % i, [128, 16, 512], BF16)) for i in range(2)]
            xt = [st.enter_context(SB("xt%d" % i, [128, 4, D], F32)) for i in range(2)]
            mT = [st.enter_context(SB("mT%d" % i, [128, 8, 512], BF16)) for i in range(2)]
            tA = [st.enter_context(SB("tA%d" % i, [128, 512], F32)) for i in range(3)]
            tB = [st.enter_context(SB("tB%d" % i, [128, 512], F32)) for i in range(3)]
            tO = [st.enter_context(SB("tO%d" % i, [128, 512], F32)) for i in range(3)]
            pC = [st.enter_context(PS("pC%d" % i, [128, 512], F32)) for i in range(8)]
            cC = {"p": 0, "t": 0, "o": 0}
            it = 0
            tilesC = [(b, tt) for b in range(NSEQ) for tt in range(NT)]

            def loadC(n):
                b, tt = tilesC[n]
                i2 = n % 2
                tsl = slice(tt * 512, tt * 512 + 512)
                r0 = b * SEQ + tt * 512
                S.dma("sp", yt[i2][:], yT[b, :, tsl].rearrange("(k p) t -> p k t", p=128),
                      r=[dkey("yT", b)], w=[("yt", i2)])
                S.dma("sp", sgt[i2][:], sgT[b, :, tsl].rearrange("(k p) t -> p k t", p=128),
                      r=[dkey("sgT", b)], w=[("sgt", i2)])
                S.dma("sp", xt[i2][:], xsrc[r0:r0 + 512, :].rearrange("(k p) d -> p k d", p=128),
                      r=[dkey(xsrc_key)], w=[("xt", i2)])

            gbcC = [st.enter_context(SB("gbcC%d" % i, [128, D], F32)) for i in range(NSEQ)]
            for b in range(NSEQ):
                S.dma("sp", gbcC[b][:], modrows[l, 0, b, 2, :].partition_broadcast(128), r=[dkey("modrows", l, 0)],
                      w=[("gbcC", b)])
            loadC(0)
            for b in range(NSEQ):
                for tt in range(NT):
                    i2 = it % 2
                    it += 1
                    if it < len(tilesC):
                        loadC(it)
                    tsl = slice(tt * 512, tt * 512 + 512)
                    r0 = b * SEQ + tt * 512
                    for dc in range(8):
                        pa = cC["p"] % 8
                        pb = (cC["p"] + 1) % 8
                        cC["p"] += 2
                        for kc in range(4):
                            S.op("pe", lambda kc=kc: nc.tensor.matmul(
                                pC[pa][:], lhsT=WBM[:, kc, dc * 128:(dc + 1) * 128], rhs=yt[i2][:, kc, :],
                                start=(kc == 0), stop=(kc == 3)), r=[("WBM",), ("yt", i2)], w=[("pC", pa)])
                        for kc in range(4):
                            S.op("pe", lambda kc=kc: nc.tensor.matmul(
                                pC[pb][:], lhsT=WBF[:, kc, dc * 128:(dc + 1) * 128], rhs=yt[i2][:, 4 + kc, :],
                                start=(kc == 0), stop=(kc == 3)), r=[("WBF",), ("yt", i2)], w=[("pC", pb)])
                        ti = cC["t"] % 3
                        cC["t"] += 1
                        S.op("dve", lambda: nc.vector.tensor_tensor(out=tA[ti][:], in0=pC[pa][:], in1=sgt[i2][:, dc, :],
                                                                    op=ALU.mult), r=[("pC", pa), ("sgt", i2)], w=[("tA", ti)])
                        S.op("dve", lambda: nc.vector.tensor_tensor(out=tB[ti][:], in0=pC[pb][:], in1=sgt[i2][:, 8 + dc, :],
                                                                    op=ALU.mult), r=[("pC", pb), ("sgt", i2)], w=[("tB", ti)])
                        S.op("pool", lambda: nc.gpsimd.tensor_tensor(out=mT[i2][:, dc, :], in0=tA[ti][:], in1=tB[ti][:],
                                                                     op=ALU.add), r=[("tA", ti), ("tB", ti)], w=[("mT", i2, dc)])
                    for blk in range(4):
                        for half in range(2):
                            po = cC["p"] % 8
                            cC["p"] += 1
                            for kc in range(8):
                                S.op("pe", lambda kc=kc: nc.tensor.matmul(
                                    pC[po][:], lhsT=mT[i2][:, kc, blk * 128:(blk + 1) * 128],
                                    rhs=WO[:, kc, half * 512:(half + 1) * 512], start=(kc == 0), stop=(kc == 7)),
                                    r=[("WO",), ("mT", i2, kc)], w=[("pC", po)])
                            oi = cC["o"] % 3
                            cC["o"] += 1
                            hs = slice(half * 512, half * 512 + 512)
                            S.op("dve", lambda: nc.vector.tensor_tensor(out=tO[oi][:], in0=pC[po][:], in1=gbcC[b][:, hs],
                                                                        op=ALU.mult), r=[("pC", po), ("gbcC", b)], w=[("tO", oi)])
                            S.op("pool", lambda: nc.gpsimd.tensor_tensor(out=xt[i2][:, blk, hs], in0=xt[i2][:, blk, hs],
                                                                         in1=tO[oi][:], op=ALU.add),
                                 r=[("tO", oi), ("xt", i2)], w=[("xt", i2)])
                    S.dma("sp", xs[r0:r0 + 512, :].rearrange("(k p) d -> p k d", p=128), xt[i2][:],
                          r=[("xt", i2)], w=[dkey("xs")])
            S.barrier()
        stop_here("C%d" % l)

        is_moe = (l % 2 == 1)
        li = l // 2
        with contextlib.ExitStack() as st:
            NXB = 4
            xbuf = [st.enter_context(SB("dxb%d" % i, [128, D], F32)) for i in range(NXB)]
            junk = st.enter_context(SB("djunk", [128, D], BF16))
            tmpf = [st.enter_context(SB("dtmpf%d" % i, [128, D], F32)) for i in range(2)]
            hbuf = [st.enter_context(SB("dhb%d" % i, [128, D], BF16)) for i in range(2)]
            hf = [st.enter_context(SB("dhf%d" % i, [128, D], F32)) for i in range(2)]
            ssb = [st.enter_context(SB("dss%d" % i, [128, 1], F32)) for i in range(4)]
            rsb = [st.enter_context(SB("drs%d" % i, [128, 1], F32)) for i in range(4)]
            hTs = [st.enter_context(SB("dhT%d" % i, [128, 8, 128], BF16)) for i in range(3)]
            abc = st.enter_context(SB("dabc", [128, D], F32))
            bbc = st.enter_context(SB("dbbc", [128, D], F32))
            pst = st.enter_context(PS("dpst", [128, D], BF16))
            if is_moe:
                wrb = st.enter_context(SB("wrb", [128, NEXP, D], F32))
                lg = [st.enter_context(SB("lg%d" % i, [128, NEXP], F32)) for i in range(2)]
                m8 = [st.enter_context(SB("m8%d" % i, [128, 8], F32)) for i in range(2)]
                w12 = [st.enter_context(SB("w12%d" % i, [128, 4], F32)) for i in range(2)]
                cmb = [st.enter_context(SB("cmb%d" % i, [128, 2, NEXP], F32)) for i in range(2)]
                junkf = st.enter_context(SB("junkf", [128, D], F32))
                for e in range(NEXP):
                    S.dma("sp", wrb[:, e, :], wr_in[li, e, :].partition_broadcast(128), r=[dkey("wr")], w=[("wrb",)])
                base8 = st.enter_context(SB("base8", [128, NEXP], F32))
                mk8 = [st.enter_context(SB("mk8%d" % i, [128, NEXP], F32)) for i in range(2)]
                fl8 = [st.enter_context(SB("fl8%d" % i, [128, 2, NEXP], F32)) for i in range(2)]
                gf2 = [st.enter_context(SB("gf2%d" % i, [128, 4], F32)) for i in range(2)]
                gi2 = [st.enter_context(SB("gi2%d" % i, [128, 2], I32)) for i in range(2)]
                gw2 = [st.enter_context(SB("gw2%d" % i, [128, 2], F32)) for i in range(2)]
                cnti = st.enter_context(SB("cnti", [1, NEXP], I32))
                prt = st.enter_context(PS("prt", [128, 2 * NEXP], F32))
                S.op("pool", lambda: nc.gpsimd.memset(base8[:], 0.0), w=[("base8",)])
            ci = 0
            for b in range(NSEQ):
                S.dma("sp", abc[:], modrows[l, 1, b, 0, :].partition_broadcast(128), r=[dkey("modrows", l, 1)], w=[("abc",)])
                S.dma("sp", bbc[:], modrows[l, 1, b, 1, :].partition_broadcast(128), r=[dkey("modrows", l, 1)], w=[("abc",)],
                      key=("abc",))
                for blk in range(SEQ // 128):
                    r0 = b * SEQ + blk * 128
                    xi = ci % NXB
                    si = ci % 4
                    hi = ci % 2
                    ti = ci % 3
                    ci += 1
                    S.dma("sp", xbuf[xi][:], xs[r0:r0 + 128, :], r=[dkey("xs")], w=[("xb", xi)])
                    S.op("act", lambda: nc.scalar.activation(out=junk[:], in_=xbuf[xi][:], func=AF.Square,
                                                             accum_out=ssb[si][:]), r=[("xb", xi)], w=[("junk",), ("ss", si)])
                    rms_rstd((ssb[si][:], ("ss", si)), (rsb[si][:], ("rs", si)), D)
                    S.op("dve", lambda: nc.vector.scalar_tensor_tensor(out=tmpf[hi][:], in0=xbuf[xi][:], scalar=rsb[si][:],
                                                                       in1=abc[:], op0=ALU.mult, op1=ALU.mult),
                         r=[("xb", xi), ("rs", si), ("abc",)], w=[("tmpf", hi)])
                    S.op("pool", lambda: nc.gpsimd.tensor_tensor(out=hf[hi][:], in0=tmpf[hi][:], in1=bbc[:], op=ALU.add),
                         r=[("tmpf", hi), ("abc",)], w=[("hf", hi)])
                    S.op("act", lambda: nc.scalar.copy(out=hbuf[hi][:], in_=hf[hi][:]), r=[("hf", hi)], w=[("hb", hi)])
                    for kc in range(8):
                        S.op("pe", lambda kc=kc: nc.tensor.transpose(
                            pst[:, kc * 128:(kc + 1) * 128], hbuf[hi][:, kc * 128:(kc + 1) * 128], ident[:]),
                            r=[("hb", hi), ("ident",)], w=[("pst",)])
                    S.op("act", lambda: nc.scalar.copy(out=hTs[ti][:], in_=pst[:].rearrange("p (k t) -> p k t", k=8)),
                         r=[("pst",)], w=[("hTs", ti)])
                    S.dma("sp", h2T[:, r0:r0 + 128].rearrange("(k p) t -> p k t", p=128), hTs[ti][:],
                          r=[("hTs", ti)], w=[dkey("h2T")])
                    if is_moe:
                        for e in range(NEXP):
                            S.op("dve", lambda e=e: nc.vector.scalar_tensor_tensor(
                                out=junkf[:], in0=hf[hi][:], scalar=1.0, in1=wrb[:, e, :], op0=ALU.mult, op1=ALU.mult,
                                accum_out=lg[hi][:, e:e + 1]),
                                r=[("hf", hi), ("wrb",)], w=[("junkf",), ("lg", hi)])
                        S.op("dve", lambda: nc.vector.max(out=m8[hi][:], in_=lg[hi][:]), r=[("lg", hi)], w=[("m8", hi)])
                        S.op("dve", lambda: nc.vector.tensor_tensor(out=w12[hi][:, 0:1], in0=m8[hi][:, 1:2], in1=m8[hi][:, 0:1],
                                                                    op=ALU.subtract), r=[("m8", hi)], w=[("w12", hi)])
                        S.op("act", lambda: nc.scalar.activation(out=w12[hi][:, 1:2], in_=w12[hi][:, 0:1], func=AF.Exp),
                             r=[("w12", hi)], w=[("w12", hi)])
                        S.op("dve", lambda: nc.vector.tensor_scalar(out=w12[hi][:, 1:2], in0=w12[hi][:, 1:2], scalar1=1.0,
                                                                    scalar2=None, op0=ALU.add), r=[("w12", hi)], w=[("w12", hi)])
                        S.op("dve", lambda: nc.vector.reciprocal(out=w12[hi][:, 2:3], in_=w12[hi][:, 1:2]),
                             r=[("w12", hi)], w=[("w12", hi)])
                        S.op("dve", lambda: nc.vector.tensor_scalar(out=w12[hi][:, 3:4], in0=w12[hi][:, 2:3], scalar1=-1.0,
                                                                    scalar2=1.0, op0=ALU.mult, op1=ALU.add),
                             r=[("w12", hi)], w=[("w12", hi)])
                        S.op("dve", lambda: nc.vector.tensor_scalar(out=cmb[hi][:, 0, :], in0=lg[hi][:], scalar1=m8[hi][:, 0:1],
                                                                    scalar2=w12[hi][:, 2:3], op0=ALU.is_equal, op1=ALU.mult),
                             r=[("lg", hi), ("m8", hi), ("w12", hi)], w=[("cmb", hi)])
                        S.op("dve", lambda: nc.vector.tensor_scalar(out=cmb[hi][:, 1, :], in0=lg[hi][:], scalar1=m8[hi][:, 1:2],
                                                                    scalar2=w12[hi][:, 3:4], op0=ALU.is_equal, op1=ALU.mult),
                             r=[("lg", hi), ("m8", hi), ("w12", hi)], w=[("cmb", hi)])
                        S.op("dve", lambda: nc.vector.tensor_tensor(out=cmb[hi][:, 0, :], in0=cmb[hi][:, 0, :],
                                                                    in1=cmb[hi][:, 1, :], op=ALU.add),
                             r=[("cmb", hi)], w=[("cmb", hi)])
                        S.dma("sp", comb[r0:r0 + 128, :], cmb[hi][:, 0, :], r=[("cmb", hi)], w=[dkey("comb")])
                        S.op("dve", lambda: nc.vector.tensor_scalar(out=mk8[hi][:], in0=cmb[hi][:, 0, :], scalar1=0.0,
                                                                    scalar2=None, op0=ALU.is_gt), r=[("cmb", hi)], w=[("mk8", hi)])
                        S.op("pe", lambda: nc.tensor.matmul(prt[:, 0:NEXP], lhsT=ustr[:], rhs=mk8[hi][:], start=True, stop=True),
                             r=[("ustr",), ("mk8", hi)], w=[("prt",)])
                        S.op("pe", lambda: nc.tensor.matmul(prt[:, NEXP:2 * NEXP], lhsT=onesq[:], rhs=mk8[hi][:], start=True,
                                                            stop=True), r=[("onesq",), ("mk8", hi)], w=[("prt",)])
                        S.op("dve", lambda: nc.vector.tensor_tensor(out=fl8[hi][:, 0, :], in0=prt[:, 0:NEXP], in1=base8[:],
                                                                    op=ALU.add), r=[("prt",), ("base8",)], w=[("fl8", hi)])
                        S.op("dve", lambda: nc.vector.tensor_tensor(out=fl8[hi][:, 0, :], in0=fl8[hi][:, 0, :], in1=eoff[:],
                                                                    op=ALU.add), r=[("fl8", hi), ("eoff",)], w=[("fl8", hi)])
                        S.op("dve", lambda: nc.vector.tensor_tensor(out=fl8[hi][:, 0, :], in0=fl8[hi][:, 0, :], in1=mk8[hi][:],
                                                                    op=ALU.mult), r=[("fl8", hi), ("mk8", hi)], w=[("fl8", hi)])
                        S.op("dve", lambda: nc.vector.tensor_tensor(out=base8[:], in0=prt[:, NEXP:2 * NEXP], in1=base8[:],
                                                                    op=ALU.add), r=[("prt",), ("base8",)], w=[("base8",)])
                        S.op("dve", lambda: nc.vector.tensor_reduce(out=gf2[hi][:, 1:2], in_=fl8[hi][:, 0, :],
                                                                    axis=mybir.AxisListType.X, op=ALU.max),
                             r=[("fl8", hi)], w=[("gf2", hi)])
                        S.op("dve", lambda: nc.vector.tensor_reduce(out=gf2[hi][:, 2:3], in_=fl8[hi][:, 0, :],
                                                                    axis=mybir.AxisListType.X, op=ALU.add),
                             r=[("fl8", hi)], w=[("gf2", hi)])
                        S.op("dve", lambda: nc.vector.tensor_tensor(out=gf2[hi][:, 0:1], in0=gf2[hi][:, 2:3], in1=gf2[hi][:, 1:2],
                                                                    op=ALU.subtract), r=[("gf2", hi)], w=[("gf2", hi)])
                        S.op("dve", lambda: nc.vector.tensor_copy(out=gi2[hi][:], in_=gf2[hi][:, 0:2]), r=[("gf2", hi)],
                             w=[("gi2", hi)])
                        S.op("dve", lambda: nc.vector.tensor_scalar(out=fl8[hi][:, 1, :], in0=fl8[hi][:, 0, :],
                                                                    scalar1=gf2[hi][:, 1:2], scalar2=None, op0=ALU.is_equal),
                             r=[("fl8", hi), ("gf2", hi)], w=[("fl8", hi)])
                        S.op("dve", lambda: nc.vector.tensor_tensor(out=fl8[hi][:, 1, :], in0=fl8[hi][:, 1, :],
                                                                    in1=cmb[hi][:, 0, :], op=ALU.mult),
                             r=[("fl8", hi), ("cmb", hi)], w=[("fl8", hi)])
                        S.op("dve", lambda: nc.vector.tensor_reduce(out=gw2[hi][:, 1:2], in_=fl8[hi][:, 1, :],
                                                                    axis=mybir.AxisListType.X, op=ALU.add),
                             r=[("fl8", hi)], w=[("gw2", hi)])
                        S.op("dve", lambda: nc.vector.tensor_scalar(out=gw2[hi][:, 0:1], in0=gw2[hi][:, 1:2], scalar1=-1.0,
                                                                    scalar2=1.0, op0=ALU.mult, op1=ALU.add),
                             r=[("gw2", hi)], w=[("gw2", hi)])
                        S.dma("sp", gidx[r0:r0 + 128, :], gi2[hi][:], r=[("gi2", hi)], w=[dkey("gidx")])
                        S.dma("sp", gwd[r0:r0 + 128, :], gw2[hi][:], r=[("gw2", hi)], w=[dkey("gwd")])
                        for k in range(2):
                            S.dma_indirect("pool", out=hg, out_offset=bass.IndirectOffsetOnAxis(ap=gi2[hi][:, k:k + 1], axis=0),
                                           in_=hbuf[hi][:], in_offset=None, r=[("hb", hi), ("gi2", hi)], w=[dkey("hg")],
                                           key=("hb", hi))
            if is_moe:
                S.op("dve", lambda: nc.vector.tensor_copy(out=cnt_sb[:], in_=base8[0:1, :]), r=[("base8",)], w=[("cnt_sb",)])
                S.dma("sp", cnts[:, :], cnt_sb[:], r=[("cnt_sb",)], w=[dkey("cnts")])
            S.barrier()
        stop_here("D%d" % l)

        if is_moe:
            FQ = DFF // 4
            NFC = FQ // 128
            passes = [(e, q) for e in range(NEXP) for q in range(4)]
            with contextlib.ExitStack() as st:
                WG = [st.enter_context(SB("rWG%d" % i, [128, 8, FQ], BF16)) for i in range(2)]
                WU = [st.enter_context(SB("rWU%d" % i, [128, 8, FQ], BF16)) for i in range(2)]
                WD = [st.enter_context(SB("rWD%d" % i, [128, NFC, D], BF16)) for i in range(2)]
                htm = [st.enter_context(SB("htm%d" % i, [128, 4, D], BF16)) for i in range(2)]
                h2 = [st.enter_context(SB("rh2%d" % i, [128, 8, 512], BF16)) for i in range(2)]
                AT = [st.enter_context(SB("rAT%d" % i, [128, NFC, 512], BF16)) for i in range(2)]
                sg = [st.enter_context(SB("rsg%d" % i, [128, 512], F32)) for i in range(3)]
                ot = [st.enter_context(SB("rot%d" % i, [128, 4, D], F32)) for i in range(2)]
                pE = [st.enter_context(PS("rpE%d" % i, [128, 512], F32)) for i in range(7)]
                pstE = st.enter_context(PS("rpst", [128, D], BF16))
                cE = {"p": 0, "s": 0}

                def load_set(pi):
                    e, q = passes[pi]
                    ws = pi % 2
                    gsrc, usrc, dsrc = mwg_in[li, e], mwu_in[li, e], mwd_in[li, e]
                    for k in range(8):
                        S.dma("pool", WG[ws][:, k, :], gsrc[k * 128:(k + 1) * 128, q * FQ:(q + 1) * FQ],
                              r=[dkey("wg")], w=[("WG", ws)])
                    for k in range(8):
                        S.dma("pool", WU[ws][:, k, :], usrc[k * 128:(k + 1) * 128, q * FQ:(q + 1) * FQ],
                              r=[dkey("wu")], w=[("WU", ws)])
                    for k in range(NFC):
                        S.dma("pool", WD[ws][:, k, :], dsrc[q * FQ + k * 128:q * FQ + (k + 1) * 128, :],
                              r=[dkey("wd")], w=[("WD", ws)])

                def load_slots(Sx, pi, it_, i2):
                    e, q = passes[pi]
                    row0 = e * CAPR + it_ * 512
                    if q == 0:
                        Sx.dma("sp", htm[i2][:], hg[row0:row0 + 512, :].rearrange("(k p) d -> p k d", p=128),
                               r=[dkey("hg")], w=[("htm", i2)])
                    else:
                        Sx.dma("sp", h2[i2][:], hgT[:, row0:row0 + 512].rearrange("(k p) t -> p k t", p=128),
                               r=[("hgTrow", e, it_)], w=[("h2", i2)], key=("h2", i2))
                    if q > 0:
                        Sx.dma("sp", ot[i2][:], og[row0:row0 + 512, :].rearrange("(k p) d -> p k d", p=128),
                               r=[("ogrow", e, it_)], w=[("ot", i2)], key=("ot", i2))

                def emit_tile(Sx, pi, it_, i2):
                    e, q = passes[pi]
                    ws = pi % 2
                    row0 = e * CAPR + it_ * 512
                    if q == 0:
                        for blk in range(4):
                            for kc in range(8):
                                Sx.op("pe", lambda kc=kc: nc.tensor.transpose(
                                    pstE[:, kc * 128:(kc + 1) * 128], htm[i2][:, blk, kc * 128:(kc + 1) * 128], ident[:]),
                                    r=[("htm", i2), ("ident",)], w=[("pstE",)])
                            Sx.op("act", lambda: nc.scalar.copy(out=h2[i2][:, :, blk * 128:(blk + 1) * 128],
                                                                in_=pstE[:].rearrange("p (k t) -> p k t", k=8)),
                                  r=[("pstE",)], w=[("h2", i2)])
                        Sx.dma("sp", hgT[:, row0:row0 + 512].rearrange("(k p) t -> p k t", p=128), h2[i2][:],
                               r=[("h2", i2)], w=[("hgTrow", e, it_)], key=("h2", i2))
                    for fc in range(NFC):
                        pg = cE["p"] % 7
                        pu = (cE["p"] + 1) % 7
                        cE["p"] += 2
                        for kc in range(8):
                            Sx.op("pe", lambda kc=kc: nc.tensor.matmul(
                                pE[pg][:], lhsT=WG[ws][:, kc, fc * 128:(fc + 1) * 128], rhs=h2[i2][:, kc, :],
                                start=(kc == 0), stop=(kc == 7)), r=[("WG", ws), ("h2", i2)], w=[("pE", pg)])
                        for kc in range(8):
                            Sx.op("pe", lambda kc=kc: nc.tensor.matmul(
                                pE[pu][:], lhsT=WU[ws][:, kc, fc * 128:(fc + 1) * 128], rhs=h2[i2][:, kc, :],
                                start=(kc == 0), stop=(kc == 7)), r=[("WU", ws), ("h2", i2)], w=[("pE", pu)])
                        si = cE["s"] % 3
                        cE["s"] += 1
                        Sx.op("act", lambda: nc.scalar.activation(out=sg[si][:], in_=pE[pg][:], func=AF.Silu),
                              r=[("pE", pg)], w=[("sg", si)])
                        Sx.op("dve", lambda: nc.vector.tensor_tensor(out=AT[i2][:, fc, :], in0=pE[pu][:], in1=sg[si][:],
                                                                     op=ALU.mult),
                              r=[("pE", pu), ("sg", si)], w=[("AT", i2, fc)])
                    for blk in range(4):
                        for half in range(2):
                            po = cE["p"] % 7
                            cE["p"] += 1
                            for fc in range(NFC):
                                Sx.op("pe", lambda fc=fc: nc.tensor.matmul(
                                    pE[po][:], lhsT=AT[i2][:, fc, blk * 128:(blk + 1) * 128],
                                    rhs=WD[ws][:, fc, half * 512:(half + 1) * 512], start=(fc == 0), stop=(fc == NFC - 1)),
                                    r=[("WD", ws), ("AT", i2, fc)], w=[("pE", po)])
                            hs = slice(half * 512, half * 512 + 512)
                            if q == 0:
                                Sx.op("dve", lambda: nc.vector.tensor_copy(out=ot[i2][:, blk, hs], in_=pE[po][:]),
                                      r=[("pE", po)], w=[("ot", i2)])
                            else:
                                Sx.op("dve", lambda: nc.vector.tensor_tensor(out=ot[i2][:, blk, hs], in0=pE[po][:],
                                                                             in1=ot[i2][:, blk, hs], op=ALU.add),
                                      r=[("pE", po), ("ot", i2)], w=[("ot", i2)])
                    Sx.dma("sp", og[row0:row0 + 512, :].rearrange("(k p) d -> p k d", p=128), ot[i2][:],
                           r=[("ot", i2)], w=[("ogrow", e, it_)], key=("ot", i2))

                load_set(0)
                for pi, (e, q) in enumerate(passes):
                    if pi + 1 < len(passes):
                        load_set(pi + 1)
                    load_slots(S, pi, 0, 0)
                    for it_ in range(KT):
                        if it_ + 1 < KT:
                            load_slots(S, pi, it_ + 1, (it_ + 1) % 2)
                        emit_tile(S, pi, it_, it_ % 2)
                    if KT < CAPT:
                        S.barrier()
                        for reg in cnt_reg:
                            nc.reg_load(reg, cnt_sb[0:1, e:e + 1])
                        dyn = [list(range(KT, CAPT))]
                        for grp in dyn:
                            with nc.If_cmp(cnt_reg, grp[0] * 512, "IS_GT"):
                                for it_ in grp:
                                    load_slots(S2, pi, it_, it_ % 2)
                                    emit_tile(S2, pi, it_, it_ % 2)
                                S2.finish_local_block()
                S.barrier()
            stop_here("E%d" % l)
            with contextlib.ExitStack() as st:
                xb = [st.enter_context(SB("gxb%d" % i, [128, D], F32)) for i in range(3)]
                o1 = [st.enter_context(SB("go1%d" % i, [128, D], F32)) for i in range(2)]
                o2 = [st.enter_context(SB("go2%d" % i, [128, D], F32)) for i in range(2)]
                tt_ = [st.enter_context(SB("gtt%d" % i, [128, D], F32)) for i in range(2)]
                gi = [st.enter_context(SB("ggi%d" % i, [128, 2], I32)) for i in range(2)]
                gw = [st.enter_context(SB("ggw%d" % i, [128, 2], F32)) for i in range(2)]
                gb = [st.enter_context(SB("ggb%d" % i, [128, D], F32)) for i in range(NSEQ)]
                for b in range(NSEQ):
                    S.dma("sp", gb[b][:], modrows[l, 1, b, 2, :].partition_broadcast(128), r=[dkey("modrows", l, 1)],
                          w=[("ggb", b)])
                for n in range(T // 128):
                    b = (n * 128) // SEQ
                    r0 = n * 128
                    i2, i3 = n % 2, n % 3
                    S.dma("sp", xb[i3][:], xs[r0:r0 + 128, :], r=[dkey("xs")], w=[("gxb", i3)])
                    S.dma("sp", gi[i2][:], gidx[r0:r0 + 128, :], r=[dkey("gidx")], w=[("ggi", i2)])
                    S.dma("sp", gw[i2][:], gwd[r0:r0 + 128, :], r=[dkey("gwd")], w=[("ggw", i2)])
                    S.dma_indirect("pool", out=o1[i2][:], out_offset=None, in_=og,
                                   in_offset=bass.IndirectOffsetOnAxis(ap=gi[i2][:, 0:1], axis=0),
                                   r=[("ggi", i2)], w=[("go1", i2)], key=("go1", i2))
                    S.dma_indirect("pool", out=o2[i2][:], out_offset=None, in_=og,
                                   in_offset=bass.IndirectOffsetOnAxis(ap=gi[i2][:, 1:2], axis=0),
                                   r=[("ggi", i2)], w=[("go2", i2)], key=("go2", i2))
                    S.op("dve", lambda: nc.vector.tensor_scalar(out=tt_[i2][:], in0=o1[i2][:], scalar1=gw[i2][:, 0:1],
                                                                scalar2=None, op0=ALU.mult),
                         r=[("go1", i2), ("ggw", i2)], w=[("gtt", i2)])
                    S.op("dve", lambda: nc.vector.scalar_tensor_tensor(out=tt_[i2][:], in0=o2[i2][:], scalar=gw[i2][:, 1:2],
                                                                       in1=tt_[i2][:], op0=ALU.mult, op1=ALU.add),
                         r=[("go2", i2), ("ggw", i2), ("gtt", i2)], w=[("gtt", i2)])
                    S.op("pool", lambda: nc.gpsimd.tensor_tensor(out=tt_[i2][:], in0=tt_[i2][:], in1=gb[b][:], op=ALU.mult),
                         r=[("gtt", i2), ("ggb", b)], w=[("gtt", i2)])
                    S.op("dve", lambda: nc.vector.tensor_tensor(out=xb[i3][:], in0=xb[i3][:], in1=tt_[i2][:], op=ALU.add),
                         r=[("gtt", i2), ("gxb", i3)], w=[("gxb", i3)])
                    S.dma("sp", xs[r0:r0 + 128, :], xb[i3][:], r=[("gxb", i3)], w=[dkey("xs")])
                S.barrier()
            continue

        FQ = DFF // 4
        NFC = FQ // 128
        n_exp = NEXP if is_moe else 1
        passes = [(e, q) for e in range(n_exp) for q in range(4)]
        with contextlib.ExitStack() as st:
            WG = [st.enter_context(SB("WG%d" % i, [128, 8, FQ], BF16)) for i in range(2)]
            WU = [st.enter_context(SB("WU%d" % i, [128, 8, FQ], BF16)) for i in range(2)]
            WD = [st.enter_context(SB("WD%d" % i, [128, NFC, D], BF16)) for i in range(2)]
            h2 = [st.enter_context(SB("h2%d" % i, [128, 8, 512], BF16)) for i in range(2)]
            AT = [st.enter_context(SB("AT%d" % i, [128, NFC, 512], BF16)) for i in range(2)]
            sg = [st.enter_context(SB("sg%d" % i, [128, 512], F32)) for i in range(3)]
            xt = [st.enter_context(SB("ext%d" % i, [128, 4, D], F32)) for i in range(2)]
            tO = [st.enter_context(SB("etO%d" % i, [128, 512], F32)) for i in range(3)]
            gb = [st.enter_context(SB("egb%d" % i, [128, D], F32)) for i in range(NSEQ)]
            cmt = [st.enter_context(SB("cmt%d" % i, [128, 4, NEXP], F32)) for i in range(2)]
            pE = [st.enter_context(PS("pE%d" % i, [128, 512], F32)) for i in range(8)]
            cE = {"p": 0, "s": 0, "o": 0}
            for b in range(NSEQ):
                S.dma("sp", gb[b][:], modrows[l, 1, b, 2, :].partition_broadcast(128), r=[dkey("modrows", l, 1)],
                      w=[("egb", b)])

            def load_set(pi):
                e, q = passes[pi]
                ws = pi % 2
                if is_moe:
                    gsrc, usrc, dsrc = mwg_in[li, e], mwu_in[li, e], mwd_in[li, e]
                else:
                    gsrc, usrc, dsrc = dwg_in[li], dwu_in[li], dwd_in[li]
                for k in range(8):
                    S.dma("pool", WG[ws][:, k, :], gsrc[k * 128:(k + 1) * 128, q * FQ:(q + 1) * FQ],
                          r=[dkey("wg")], w=[("WG", ws)])
                for k in range(8):
                    S.dma("pool", WU[ws][:, k, :], usrc[k * 128:(k + 1) * 128, q * FQ:(q + 1) * FQ],
                          r=[dkey("wu")], w=[("WU", ws)])
                for k in range(NFC):
                    S.dma("pool", WD[ws][:, k, :], dsrc[q * FQ + k * 128:q * FQ + (k + 1) * 128, :],
                          r=[dkey("wd")], w=[("WD", ws)])

            tiles = [(pi, b, tt) for pi in range(len(passes)) for b in range(NSEQ) for tt in range(NT)]

            def load_tile(n):
                pi, b, tt = tiles[n]
                i2 = n % 2
                r0 = b * SEQ + tt * 512
                S.dma("sp", h2[i2][:], h2T[:, r0:r0 + 512].rearrange("(k p) t -> p k t", p=128),
                      r=[dkey("h2T")], w=[("h2", i2)])
                S.dma("sp", xt[i2][:], xs[r0:r0 + 512, :].rearrange("(k p) d -> p k d", p=128),
                      r=[("xsrow", b, tt)], w=[("ext", i2)], key=("ext", i2))
                if is_moe:
                    with nc.allow_non_contiguous_dma(reason="per-token combine weights, 32B rows"):
                        S.dma("sp", cmt[i2][:], comb[r0:r0 + 512, :].rearrange("(k p) e -> p k e", p=128),
                              r=[dkey("comb")], w=[("cmt", i2)])

            load_set(0)
            load_tile(0)
            n_tiles_pass = NSEQ * NT
            for n, (pi, b, tt) in enumerate(tiles):
                e, q = passes[pi]
                ws = pi % 2
                i2 = n % 2
                r0 = b * SEQ + tt * 512
                if n % n_tiles_pass == 0 and pi + 1 < len(passes):
                    load_set(pi + 1)
                if n + 1 < len(tiles):
                    load_tile(n + 1)
                for fc in range(NFC):
                    pg = cE["p"] % 8
                    pu = (cE["p"] + 1) % 8
                    cE["p"] += 2
                    for kc in range(8):
                        S.op("pe", lambda kc=kc: nc.tensor.matmul(
                            pE[pg][:], lhsT=WG[ws][:, kc, fc * 128:(fc + 1) * 128], rhs=h2[i2][:, kc, :],
                            start=(kc == 0), stop=(kc == 7)), r=[("WG", ws), ("h2", i2)], w=[("pE", pg)])
                    for kc in range(8):
                        S.op("pe", lambda kc=kc: nc.tensor.matmul(
                            pE[pu][:], lhsT=WU[ws][:, kc, fc * 128:(fc + 1) * 128], rhs=h2[i2][:, kc, :],
                            start=(kc == 0), stop=(kc == 7)), r=[("WU", ws), ("h2", i2)], w=[("pE", pu)])
                    si = cE["s"] % 3
                    cE["s"] += 1
                    S.op("act", lambda: nc.scalar.activation(out=sg[si][:], in_=pE[pg][:], func=AF.Silu),
                         r=[("pE", pg)], w=[("sg", si)])
                    S.op("dve", lambda: nc.vector.tensor_tensor(out=AT[i2][:, fc, :], in0=pE[pu][:], in1=sg[si][:],
                                                                op=ALU.mult),
                         r=[("pE", pu), ("sg", si)], w=[("AT", i2, fc)])
                for blk in range(4):
                    for half in range(2):
                        po = cE["p"] % 8
                        cE["p"] += 1
                        for fc in range(NFC):
                            S.op("pe", lambda fc=fc: nc.tensor.matmul(
                                pE[po][:], lhsT=AT[i2][:, fc, blk * 128:(blk + 1) * 128],
                                rhs=WD[ws][:, fc, half * 512:(half + 1) * 512], start=(fc == 0), stop=(fc == NFC - 1)),
                                r=[("WD", ws), ("AT", i2, fc)], w=[("pE", po)])
                        oi = cE["o"] % 3
                        cE["o"] += 1
                        hs = slice(half * 512, half * 512 + 512)
                        if is_moe:
                            S.op("dve", lambda: nc.vector.scalar_tensor_tensor(
                                out=tO[oi][:], in0=pE[po][:], scalar=cmt[i2][:, blk, e:e + 1], in1=gb[b][:, hs],
                                op0=ALU.mult, op1=ALU.mult), r=[("pE", po), ("egb", b), ("cmt", i2)], w=[("etO", oi)])
                        else:
                            S.op("dve", lambda: nc.vector.tensor_tensor(out=tO[oi][:], in0=pE[po][:], in1=gb[b][:, hs],
                                                                        op=ALU.mult),
                                 r=[("pE", po), ("egb", b)], w=[("etO", oi)])
                        S.op("pool", lambda: nc.gpsimd.tensor_tensor(out=xt[i2][:, blk, hs], in0=xt[i2][:, blk, hs],
                                                                     in1=tO[oi][:], op=ALU.add),
                             r=[("etO", oi), ("ext", i2)], w=[("ext", i2)])
                S.dma("sp", xs[r0:r0 + 512, :].rearrange("(k p) d -> p k d", p=128), xt[i2][:],
                      r=[("ext", i2)], w=[("xsrow", b, tt)], key=("ext", i2))
            S.barrier()

    with contextlib.ExitStack() as st:
        NXB = 4
        xbuf = [st.enter_context(SB("fxb%d" % i, [128, D], F32)) for i in range(NXB)]
        junk = st.enter_context(SB("fjunk", [128, D], BF16))
        ob = [st.enter_context(SB("fob%d" % i, [128, D], F32)) for i in range(3)]
        ssb = [st.enter_context(SB("fss%d" % i, [128, 1], F32)) for i in range(4)]
        rsb = [st.enter_context(SB("frs%d" % i, [128, 1], F32)) for i in range(4)]
        gf = st.enter_context(SB("gf", [128, D], F32))
        S.dma("sp", gf[:], gfin_in[:].partition_broadcast(128), r=[dkey("gfin")], w=[("gf",)])
        for i in range(T // 128):
            r0 = i * 128
            xi, si, oi = i % NXB, i % 4, i % 3
            S.dma("sp", xbuf[xi][:], xs[r0:r0 + 128, :], r=[dkey("xs")], w=[("xb", xi)])
            S.op("act", lambda: nc.scalar.activation(out=junk[:], in_=xbuf[xi][:], func=AF.Square, accum_out=ssb[si][:]),
                 r=[("xb", xi)], w=[("junk",), ("ss", si)])
            rms_rstd((ssb[si][:], ("ss", si)), (rsb[si][:], ("rs", si)), D)
            S.op("dve", lambda: nc.vector.scalar_tensor_tensor(out=ob[oi][:], in0=xbuf[xi][:], scalar=rsb[si][:], in1=gf[:],
                                                               op0=ALU.mult, op1=ALU.mult),
                 r=[("xb", xi), ("rs", si), ("gf",)], w=[("ob", oi)])
            S.dma("sp", out_d[r0:r0 + 128, :], ob[oi][:], r=[("ob", oi)], w=[dkey("out")])
        S.barrier()
    return nc, S


def _consts():
    c = np.zeros((128, 8), np.float32)
    p = np.arange(128)
    j = p % 16
    c[:, 0] = (10000.0 ** (-(2.0 * j) / 32.0)).astype(np.float32)
    is_sin = p >= 64
    sgn = np.where((p % 32) < 16, -1.0, 1.0)
    c[:, 2] = np.where(is_sin, sgn, -1.0)
    c[:, 3] = np.where(is_sin, 0.0, np.pi / 2)
    c[:, 4] = np.where((p // 32) % 2 == 1, MLA_SCALE, 1.0)
    return c


def _prep_shared(inp, L):
    w_in = np.asarray(inp["w_in"])
    offs = np.cumsum([256, 128, 32, 512, 512, 512, 8, 1024, 1024])[:-1]
    cq, ckv, kr, fq, fk, fv, flg, ga, gb = np.split(w_in, offs, axis=-1)
    kr_perm = np.concatenate([kr[..., 16:], kr[..., :16]], axis=-1)
    wa = np.ascontiguousarray(np.concatenate([cq, ckv, kr, kr_perm, fq, fk, fv, ga, gb, flg], axis=-1))
    assert wa.shape[-1] == CA
    w_uq = np.asarray(inp["w_uq"]).reshape(L, QL, NH, 96)
    nope, rope = w_uq[..., :64], w_uq[..., 64:]
    rope_perm = np.concatenate([rope[..., 16:], rope[..., :16]], axis=-1)
    wq = np.ascontiguousarray(np.concatenate([nope, rope, rope_perm], axis=-1).reshape(L, QL, NH * 128))
    w_ukv = np.asarray(inp["w_ukv"]).reshape(L, KVL, NH, 128)
    wkv = np.ascontiguousarray(np.concatenate([w_ukv[..., :64].reshape(L, KVL, 512),
                                               w_ukv[..., 64:].reshape(L, KVL, 512)], axis=-1))
    gq = np.asarray(inp["q_norm_g"])
    gqT = np.ascontiguousarray(gq.reshape(L, 2, 128).transpose(2, 0, 1).reshape(128, L * 2))
    gkvT = np.ascontiguousarray(np.asarray(inp["kv_norm_g"]).T)
    shared = {
        "ada_w": np.asarray(inp["ada_w"]), "ada_b": np.asarray(inp["ada_b"]),
        "norm_mix_g": np.asarray(inp["norm_mix_g"]), "norm_ffn_g": np.asarray(inp["norm_ffn_g"]),
        "wa": wa, "b_forget": np.asarray(inp["b_forget"]), "q_norm_gT": gqT, "wq": wq, "kv_norm_gT": gkvT, "wkv": wkv,
        "w_branch_mla": np.asarray(inp["w_branch_mla"]), "w_branch_fox": np.asarray(inp["w_branch_fox"]),
        "w_out": np.asarray(inp["w_out"]),
        "dense_w_gate": np.asarray(inp["dense_w_gate"]), "dense_w_up": np.asarray(inp["dense_w_up"]),
        "dense_w_down": np.asarray(inp["dense_w_down"]),
        "final_norm_g": np.asarray(inp["final_norm_g"]), "consts": _consts(),
    }
    if L // 2 > 0:
        shared["moe_w_routerT"] = np.ascontiguousarray(np.asarray(inp["moe_w_router"]).transpose(0, 2, 1))
        shared["moe_w_gate"] = np.asarray(inp["moe_w_gate"])
        shared["moe_w_up"] = np.asarray(inp["moe_w_up"])
        shared["moe_w_down"] = np.asarray(inp["moe_w_down"])
    else:
        shared["moe_w_routerT"] = np.zeros((1, NEXP, D), np.float32)
    return shared


def run(inp, cfg, extra_out=()):
    L = cfg.depth
    shared = _prep_shared(inp, L)
    x = np.asarray(inp["x"])
    c = np.asarray(inp["c"])
    pos = np.asarray(inp["positions"]).astype(np.int32)
    in_maps = []
    for i in range(cfg.ncores):
        sl = slice(i * cfg.nseq, (i + 1) * cfg.nseq)
        m = dict(shared)
        m["x"] = np.ascontiguousarray(x[sl].reshape(cfg.T, D))
        m["cT"] = np.ascontiguousarray(c[sl].T)
        m["pos"] = np.ascontiguousarray(pos[sl])
        in_maps.append(m)
    nc, S = build_program(cfg)
    res = run_bass_kernel_spmd(nc, in_maps, core_ids=list(range(cfg.ncores)))
    out = np.concatenate([r["out"].reshape(cfg.nseq, cfg.seq, D) for r in res.results], axis=0)
    if extra_out:
        return out, [{k: r[k] for k in extra_out} for r in res.results]
    return out


def kernel(**inputs):
    B, SEQ, _ = inputs["x"].shape
    L = inputs["ada_w"].shape[0]
    cfg = Cfg(nseq=B // 8, seq=SEQ, depth=L, ncores=8)
    out = run(inputs, cfg)
    return out.astype(np.float32)
```

```python
import contextlib
import numpy as np
import concourse.bass as bass
import concourse.mybir as mybir
from concourse.bass_utils import run_bass_kernel_spmd

F32 = mybir.dt.float32
BF16 = mybir.dt.bfloat16
I32 = mybir.dt.int32
AF = mybir.ActivationFunctionType
ALU = mybir.AluOpType

D = 1024
NH = 8
QL = 256
KVL = 128
ROPE = 32
DFF = 3584
NEXP = 8
EPS = 1e-6
MLA_SCALE = 96 ** -0.5
FOX_SCALE = 64 ** -0.5
NEG = -30000.0
CW1 = 6.28125
CW2 = float(2.0 * np.pi - 6.28125)
PI_LO = 3.1415925

CA_CQ = 0
CA_CKV = 256
CA_KR = 384
CA_FQ = 448
CA_FK = 960
CA_FV = 1472
CA_GA = 1984
CA_GB = 3008
CA_FL = 4032
CA = 4040
CA_PAD = 4096


class Cfg:
    def __init__(self, nseq=2, seq=4096, depth=4, ncores=8, debug=False):
        self.nseq, self.seq, self.depth, self.ncores, self.debug = nseq, seq, depth, ncores, debug
        self.kt = 6
        self.stop = None
        self.T = nseq * seq


class Sched:
    COMPUTE = ("pe", "act", "dve", "pool")

    def __init__(self, nc, n_dma_sems=44, n_sw=12, prefix=""):
        self.nc = nc
        self.prefix = prefix
        self.eng = {"pe": nc.tensor, "act": nc.scalar, "dve": nc.vector, "pool": nc.gpsimd, "sp": nc.sync}
        self.sems = {}
        self.cnt = {}
        for e in self.COMPUTE:
            self.sems[e] = nc.alloc_semaphore(prefix + "c_" + e)
            self.cnt[e] = 0
        self.dma_pool = {"hw": [], "sw": []}
        for kind, n in (("hw", n_dma_sems), ("sw", n_sw)):
            for i in range(n):
                k = "%s%d" % (kind, i)
                self.sems[k] = nc.alloc_semaphore(prefix + k)
                self.cnt[k] = 0
                self.dma_pool[kind].append(k)
        self.dma_map = {}
        self.dma_next = {"hw": 0, "sw": 0}
        self.waited = {}
        self.res = {}
        self.n_wait = 0
        self.n_ins = 0

    def _wait(self, eng, dep):
        semk, val, deng = dep
        if semk == "pe" and eng == "pe":
            return
        key = (eng, semk)
        if self.waited.get(key, 0) >= val:
            return
        self.eng[eng].wait_ge(self.sems[semk], val)
        self.waited[key] = val
        self.n_wait += 1

    def _deps(self, eng, r, w):
        for k in r:
            st = self.res.get(k)
            if st is not None and st[0] is not None:
                self._wait(eng, st[0])
        for k in w:
            st = self.res.get(k)
            if st is not None:
                if st[0] is not None:
                    self._wait(eng, st[0])
                for semk, (val, deng) in st[1].items():
                    self._wait(eng, (semk, val, deng))

    def _record(self, comp, r, w):
        for k in r:
            st = self.res.get(k)
            if st is None:
                st = [None, {}]
                self.res[k] = st
            old = st[1].get(comp[0])
            if old is None or old[0] < comp[1]:
                st[1][comp[0]] = (comp[1], comp[2])
        for k in w:
            self.res[k] = [comp, {}]

    def op(self, eng, fn, r=(), w=()):
        self._deps(eng, r, w)
        ins = fn()
        self.cnt[eng] += 1
        ins.then_inc(self.sems[eng], 1)
        self._record((eng, self.cnt[eng], eng), r, w)
        self.n_ins += 1
        return ins

    def dma(self, eng, out, in_, r=(), w=(), key=None, **kw):
        if key is None:
            key = w[0] if (len(w) and isinstance(w[0], tuple) and w[0][0] != "dram") else r[0]
        r = [k for k in r if k[0] != "dram"]
        w = [k for k in w if k[0] != "dram"]
        kind = "sw" if eng == "pool" else "hw"
        key = (kind, key)
        semk = self.dma_map.get(key)
        if semk is None:
            assert self.dma_next[kind] < len(self.dma_pool[kind]), "out of DMA semaphores"
            semk = self.dma_pool[kind][self.dma_next[kind]]
            self.dma_next[kind] += 1
            self.dma_map[key] = semk
        self._deps(eng, r, w)
        ins = self.eng[eng].dma_start(out=out, in_=in_, **kw)
        self.cnt[semk] += 16
        ins.then_inc(self.sems[semk], 16)
        self._record((semk, self.cnt[semk], "dma"), r, w)
        self.n_ins += 1
        return ins

    def dma_indirect(self, eng, out, out_offset, in_, in_offset, r=(), w=(), key=None, bound=None):
        kind = "sw"
        r = [k for k in r if k[0] != "dram"]
        w = [k for k in w if k[0] != "dram"]
        key = (kind, key)
        semk = self.dma_map.get(key)
        if semk is None:
            assert self.dma_next[kind] < len(self.dma_pool[kind]), "out of DMA semaphores"
            semk = self.dma_pool[kind][self.dma_next[kind]]
            self.dma_next[kind] += 1
            self.dma_map[key] = semk
        self._deps(eng, r, w)
        ins = self.nc.gpsimd.indirect_dma_start(out=out, out_offset=out_offset, in_=in_, in_offset=in_offset,
                                                bounds_check=bound, oob_is_err=False if bound is not None else True)
        self.cnt[semk] += 16
        ins.then_inc(self.sems[semk], 16)
        self._record((semk, self.cnt[semk], "dma"), r, w)
        self.n_ins += 1
        return ins

    def barrier(self, engines=("pe", "act", "dve", "pool", "sp")):
        for e in engines:
            for semk, c in self.cnt.items():
                if c > 0:
                    self._wait(e, (semk, c, "x"))
        self.res = {}
        self.dma_map = {}
        self.dma_next = {"hw": 0, "sw": 0}


    def finish_local_block(self):
        self.barrier()
        self.nc.all_engine_barrier()
        for semk, c in self.cnt.items():
            if c > 0:
                owner = semk if semk in self.COMPUTE else "sp"
                self.eng[owner].sem_clear(self.sems[semk])
        self.nc.all_engine_barrier()
        for k in self.cnt:
            self.cnt[k] = 0
        self.waited = {}
        self.res = {}


class _Stop(Exception):
    pass


def build_program(cfg):
    try:
        return _build_program(cfg)
    except _Stop as e:
        return e.args


def _build_program(cfg):
    nc = bass.Bass("TRN2", target_bir_lowering=False)
    S = Sched(nc)
    S2 = Sched(nc, n_dma_sems=10, n_sw=0, prefix="L_")
    NSEQ, SEQ, L, T = cfg.nseq, cfg.seq, cfg.depth, cfg.T
    NT = SEQ // 512
    n_dense, n_moe = (L + 1) // 2, L // 2
    dbg = cfg.debug

    uid = [0]

    def SB(name, shape, dt):
        uid[0] += 1
        return nc.sbuf_tensor("%s_u%d" % (name, uid[0]), shape, dt)

    def PS(name, shape, dt):
        uid[0] += 1
        return nc.psum_tensor("%s_u%d" % (name, uid[0]), shape, dt)

    def dram(name, shape, dt, kind="Internal"):
        return nc.dram_tensor(name, list(shape), dt, kind=kind).ap()

    scr_kind = "ExternalOutput" if dbg else "Internal"
    x_in = dram("x", [T, D], F32, "ExternalInput")
    cT_in = dram("cT", [D, NSEQ], F32, "ExternalInput")
    pos_in = dram("pos", [NSEQ, SEQ], I32, "ExternalInput")
    ada_w = dram("ada_w", [L, 2, D, 3 * D], F32, "ExternalInput")
    ada_b = dram("ada_b", [L, 2, 3 * D], F32, "ExternalInput")
    g_mix = dram("norm_mix_g", [L, D], F32, "ExternalInput")
    g_ffn = dram("norm_ffn_g", [L, D], F32, "ExternalInput")
    wa_in = dram("wa", [L, D, CA], F32, "ExternalInput")
    bfg_in = dram("b_forget", [L, NH], F32, "ExternalInput")
    gq_in = dram("q_norm_gT", [128, L * 2], F32, "ExternalInput")
    wq_in = dram("wq", [L, QL, NH * 128], F32, "ExternalInput")
    gkv_in = dram("kv_norm_gT", [128, L], F32, "ExternalInput")
    wkv_in = dram("wkv", [L, KVL, 1024], F32, "ExternalInput")
    wbm_in = dram("w_branch_mla", [L, 512, D], F32, "ExternalInput")
    wbf_in = dram("w_branch_fox", [L, 512, D], F32, "ExternalInput")
    wo_in = dram("w_out", [L, D, D], F32, "ExternalInput")
    dwg_in = dram("dense_w_gate", [n_dense, D, DFF], F32, "ExternalInput")
    dwu_in = dram("dense_w_up", [n_dense, D, DFF], F32, "ExternalInput")
    dwd_in = dram("dense_w_down", [n_dense, DFF, D], F32, "ExternalInput")
    n_moe_a = max(n_moe, 1)
    wr_in = dram("moe_w_routerT", [n_moe_a, NEXP, D], F32, "ExternalInput")
    if n_moe > 0:
        mwg_in = dram("moe_w_gate", [n_moe_a, NEXP, D, DFF], F32, "ExternalInput")
        mwu_in = dram("moe_w_up", [n_moe_a, NEXP, D, DFF], F32, "ExternalInput")
        mwd_in = dram("moe_w_down", [n_moe_a, NEXP, DFF, D], F32, "ExternalInput")
    gfin_in = dram("final_norm_g", [D], F32, "ExternalInput")
    cst_in = dram("consts", [128, 8], F32, "ExternalInput")
    out_d = dram("out", [T, D], F32, "ExternalOutput")
    xs = dram("xs", [T, D], F32, scr_kind)
    tabs = dram("tabs", [NSEQ, 128, SEQ], F32, scr_kind)
    modrows = dram("modrows", [L, 2, NSEQ, 3, D], F32, scr_kind)
    kropeT = dram("kropeT", [NSEQ, 32, SEQ], BF16, scr_kind)
    knopeT = dram("knopeT", [NSEQ, 512, SEQ], BF16, scr_kind)
    qmT = dram("qmT", [NSEQ, NH, 96, SEQ], BF16, scr_kind)
    vmla = dram("vmla", [NSEQ, SEQ, 1024], BF16, scr_kind)
    fqT = dram("fqT", [NSEQ, 512, SEQ], BF16, scr_kind)
    fkT = dram("fkT", [NSEQ, 512, SEQ], BF16, scr_kind)
    vfox = dram("vfox", [NSEQ, SEQ, 1024], BF16, scr_kind)
    L3 = dram("L3", [NSEQ, NH, 3, SEQ], BF16, scr_kind)
    nL3 = dram("nL3", [NSEQ, NH, 3, SEQ], BF16, scr_kind)
    sgT = dram("sgT", [NSEQ, 2048, SEQ], BF16, scr_kind)
    yT = dram("yT", [NSEQ, 1024, SEQ], BF16, scr_kind)
    h2T = dram("h2T", [D, T], BF16, scr_kind)
    comb = dram("comb", [T, NEXP], F32, scr_kind)
    CAPT = T // 512
    CAPR = CAPT * 512
    KT = min(CAPT, cfg.kt)
    if n_moe > 0:
        hg = dram("hg", [NEXP * CAPR, D], BF16, "Internal")
        og = dram("og", [NEXP * CAPR, D], F32, "Internal")
        hgT = dram("hgT", [D, NEXP * CAPR], BF16, "Internal")
        gidx = dram("gidx", [T, 2], I32, scr_kind)
        gwd = dram("gwd", [T, 2], F32, scr_kind)
        cnts = dram("cnts", [1, NEXP], I32, scr_kind)
        cnt_reg = nc.alloc_registers("cnt_e", mybir.ALL_ENGINES)

    def dkey(name, *idx):
        return ("dram", name) + tuple(idx)

    def stop_here(tag):
        if getattr(cfg, "stop", None) == tag:
            S.barrier()
            raise _Stop(nc, S)

    cst = nc.alloc_sbuf_tensor("cst", [128, 8], F32)
    ident = nc.alloc_sbuf_tensor("ident", [128, 128], BF16)
    ones_bf = nc.alloc_sbuf_tensor("ones_bf", [128, 128], BF16)
    maskT = nc.alloc_sbuf_tensor("maskT", [128, 128], BF16)
    zero_bf = nc.alloc_sbuf_tensor("zero_bf", [128, 128], BF16)
    ones_f = nc.alloc_sbuf_tensor("ones_f", [128, 512], F32)

    epsc = nc.alloc_sbuf_tensor("epsc", [128, 1], F32)
    S.op("pool", lambda: nc.gpsimd.memset(epsc[:], EPS), w=[("epsc",)])
    cnt_sb = nc.alloc_sbuf_tensor("cnt_sb", [1, NEXP], I32)
    ustr = nc.alloc_sbuf_tensor("ustr", [128, 128], F32)
    onesq = nc.alloc_sbuf_tensor("onesq", [128, 128], F32)
    eoff = nc.alloc_sbuf_tensor("eoff", [128, NEXP], F32)
    S.op("pool", lambda: nc.gpsimd.memset(onesq[:], 1.0), w=[("onesq",)])
    S.op("pool", lambda: nc.gpsimd.affine_select(out=ustr[:], in_=onesq[:], pattern=[[1, 128]],
                                                 compare_op=ALU.is_gt, fill=0.0, base=0, channel_multiplier=-1),
         r=[("onesq",)], w=[("ustr",)])
    for e in range(NEXP):
        S.op("pool", lambda e=e: nc.gpsimd.memset(eoff[:, e:e + 1], float(e * (T // 512) * 512)), w=[("eoff",)])
    S.dma("sp", cst[:], cst_in[:, :], r=[dkey("cst")], w=[("cst",)])
    S.op("pool", lambda: nc.gpsimd.memset(zero_bf[:], 0.0), w=[("zero_bf",)])
    S.op("pool", lambda: nc.gpsimd.memset(ones_bf[:], 1.0), w=[("ones_bf",)])
    S.op("pool", lambda: nc.gpsimd.memset(ones_f[:], 1.0), w=[("ones_f",)])
    S.op("pool", lambda: nc.gpsimd.affine_select(out=ident[:], in_=zero_bf[:], pattern=[[-1, 128]],
                                                 compare_op=ALU.not_equal, fill=1.0, base=0,
                                                 channel_multiplier=1),
         r=[("zero_bf",)], w=[("ident",)])
    S.op("pool", lambda: nc.gpsimd.affine_select(out=maskT[:], in_=zero_bf[:], pattern=[[1, 128]],
                                                 compare_op=ALU.is_ge, fill=NEG, base=0,
                                                 channel_multiplier=-1),
         r=[("zero_bf",)], w=[("maskT",)])
    S.barrier()

    if n_moe > 0:
        ztf = nc.alloc_sbuf_tensor("zfill_f", [128, D], F32)
        S.op("pool", lambda: nc.gpsimd.memset(ztf[:], 0.0), w=[("zfill_f",)])
        hg2 = hg.rearrange("(n p r) d -> n p (r d)", p=128, r=2)
        og1 = og.rearrange("(n p) d -> n p d", p=128)
        fills = [(hg2[i], ztf[:].bitcast(BF16)) for i in range(NEXP * CAPR // 256)]
        fills += [(og1[i], ztf[:]) for i in range(NEXP * CAPR // 128)]
    else:
        fills = []

    def load_w(stack, name, src2d, K, N, eng="pool", npad=None):
        kc = K // 128
        t = stack.enter_context(SB(name, [128, kc, npad or N], BF16))
        for k in range(kc):
            S.dma(eng, t[:, k, 0:N], src2d[k * 128:(k + 1) * 128, :], r=[dkey(name)], w=[(name,)], key=(name,))
        return t

    def rms_rstd(ss, rstd, n):
        S.op("act", lambda: nc.scalar.activation(out=rstd[0], in_=ss[0], func=AF.Ln, bias=epsc[0:rstd[0].shape[0], 0:1],
                                                 scale=1.0 / n), r=[ss[1], ("epsc",)], w=[rstd[1]])
        S.op("act", lambda: nc.scalar.activation(out=rstd[0], in_=rstd[0], func=AF.Exp, scale=-0.5),
             r=[rstd[1]], w=[rstd[1]])

    with contextlib.ExitStack() as st:
        posi = st.enter_context(SB("posi", [128, SEQ], I32))
        ang = st.enter_context(SB("ang", [128, SEQ], F32))
        tb = st.enter_context(SB("tb", [128, SEQ], F32))
        for b in range(NSEQ):
            S.dma("sp", posi[:], pos_in[b, :].partition_broadcast(128), r=[dkey("pos")], w=[("posi",)])
            S.op("dve", lambda: nc.vector.tensor_copy(out=ang[:], in_=posi[:]), r=[("posi",)], w=[("ang",)])
            S.op("dve", lambda: nc.vector.tensor_scalar(out=ang[:], in0=ang[:], scalar1=cst[:, 0:1], scalar2=None,
                                                        op0=ALU.mult), r=[("ang",), ("cst",)], w=[("ang",)])
            S.op("dve", lambda: nc.vector.tensor_scalar(out=tb[:], in0=ang[:], scalar1=float(1.0 / (2.0 * np.pi)), scalar2=None,
                                                        op0=ALU.mult), r=[("ang",)], w=[("tb",)])
            S.op("dve", lambda: nc.vector.tensor_copy(out=posi[:], in_=tb[:]), r=[("tb",)], w=[("posi",)])
            S.op("dve", lambda: nc.vector.tensor_copy(out=tb[:], in_=posi[:]), r=[("posi",)], w=[("tb",)])
            S.op("dve", lambda: nc.vector.scalar_tensor_tensor(out=ang[:], in0=tb[:], scalar=-CW1, in1=ang[:],
                                                               op0=ALU.mult, op1=ALU.add), r=[("tb",), ("ang",)], w=[("ang",)])
            S.op("dve", lambda: nc.vector.scalar_tensor_tensor(out=ang[:], in0=tb[:], scalar=-CW2, in1=ang[:],
                                                               op0=ALU.mult, op1=ALU.add), r=[("tb",), ("ang",)], w=[("ang",)])
            S.op("dve", lambda: nc.vector.tensor_scalar(out=ang[:], in0=ang[:], scalar1=-PI_LO, scalar2=PI_LO,
                                                        op0=ALU.max, op1=ALU.min), r=[("ang",)], w=[("ang",)])
            S.op("dve", lambda: nc.vector.scalar_tensor_tensor(out=ang[0:64, :], in0=ang[0:64, :], scalar=-1.0,
                                                               in1=ang[0:64, :], op0=ALU.mult, op1=ALU.max),
                 r=[("ang",)], w=[("ang",)])
            S.op("act", lambda: nc.scalar.activation(out=tb[:], in_=ang[:], func=AF.Sin, bias=cst[:, 3:4],
                                                     scale=cst[:, 2:3]), r=[("ang",), ("cst",)], w=[("tb",)])
            S.op("dve", lambda: nc.vector.tensor_scalar(out=tb[:], in0=tb[:], scalar1=cst[:, 4:5], scalar2=None,
                                                        op0=ALU.mult), r=[("tb",), ("cst",)], w=[("tb",)])
            S.dma("sp", tabs[b, :, :], tb[:], r=[("tb",)], w=[dkey("tabs", b)])
        S.barrier()
    stop_here("p0")

    with contextlib.ExitStack() as st:
        cTs = st.enter_context(SB("cTs", [128, 8, NSEQ], F32))
        scT = st.enter_context(SB("scT", [128, 8, NSEQ], F32))
        wch = [st.enter_context(SB("wch%d" % i, [128, 1536], F32)) for i in range(4)]
        mod = st.enter_context(SB("mod", [NSEQ, 3 * D], F32))
        bia = st.enter_context(SB("bia", [NSEQ, 3 * D], F32))
        gbc = st.enter_context(SB("gbc", [NSEQ, D], F32))
        arow = st.enter_context(SB("arow", [NSEQ, D], F32))
        pmod = [st.enter_context(PS("pmod%d" % i, [NSEQ, 512], F32)) for i in range(3)]
        with nc.allow_non_contiguous_dma(reason="tiny transposed conditioning vector"):
            S.dma("sp", cTs[:], cT_in.rearrange("(k p) b -> p k b", p=128), r=[dkey("cT")], w=[("cTs",)])
        S.op("act", lambda: nc.scalar.activation(out=scT[:], in_=cTs[:], func=AF.Silu), r=[("cTs",)], w=[("scT",)])
        wi = 0
        for l in range(L):
            for sub in range(2):
                S.dma("sp", bia[:], ada_b[l, sub, :].partition_broadcast(NSEQ), r=[dkey("ada_b")], w=[("bia",)])
                gsrc = g_mix if sub == 0 else g_ffn
                S.dma("sp", gbc[:], gsrc[l, :].partition_broadcast(NSEQ), r=[dkey("g")], w=[("gbc",)])
                for half in range(2):
                    for kc in range(8):
                        wt = wch[wi % 4]
                        wk = ("wch", wi % 4)
                        wi += 1
                        S.dma("sp", wt[:], ada_w[l, sub, kc * 128:(kc + 1) * 128, half * 1536:(half + 1) * 1536],
                              r=[dkey("ada_w")], w=[wk])
                        for j in range(3):
                            S.op("pe", lambda j=j, wt=wt, kc=kc: nc.tensor.matmul(
                                pmod[j][:], lhsT=scT[:, kc, :], rhs=wt[:, j * 512:(j + 1) * 512],
                                start=(kc == 0), stop=(kc == 7)), r=[wk, ("scT",)], w=[("pmod", j)])
                    for j in range(3):
                        c0 = half * 1536 + j * 512
                        S.op("dve", lambda j=j, c0=c0: nc.vector.tensor_tensor(
                            out=mod[:, c0:c0 + 512], in0=pmod[j][:], in1=bia[:, c0:c0 + 512], op=ALU.add),
                            r=[("pmod", j), ("bia",)], w=[("mod",)])
                S.op("dve", lambda: nc.vector.scalar_tensor_tensor(out=arow[:], in0=mod[:, D:2 * D], scalar=1.0,
                                                                   in1=gbc[:], op0=ALU.add, op1=ALU.mult),
                     r=[("mod",), ("gbc",)], w=[("arow",)])
                S.dma("sp", modrows[l, sub, :, 0, :], arow[:], r=[("arow",)], w=[dkey("modrows", l, sub)])
                S.dma("sp", modrows[l, sub, :, 1, :], mod[:, 0:D], r=[("mod",)], w=[dkey("modrows", l, sub)])
                S.dma("sp", modrows[l, sub, :, 2, :], mod[:, 2 * D:3 * D], r=[("mod",)], w=[dkey("modrows", l, sub)])
        S.barrier()
    stop_here("p1")

    def norm_block(xb, xk, ss, ssk, rstd, rk, junk, jk, tmp, tk, hb, hk, abc, bbc, bck):
        S.op("act", lambda: nc.scalar.activation(out=junk, in_=xb, func=AF.Square, accum_out=ss),
             r=[xk], w=[jk, ssk])
        rms_rstd((ss, ssk), (rstd, rk), D)
        S.op("dve", lambda: nc.vector.scalar_tensor_tensor(out=tmp, in0=xb, scalar=rstd, in1=abc,
                                                           op0=ALU.mult, op1=ALU.mult),
             r=[xk, rk, bck], w=[tk])
        if bbc is None:
            S.op("pool", lambda: nc.gpsimd.tensor_copy(out=hb, in_=tmp), r=[tk], w=[hk])
        else:
            S.op("pool", lambda: nc.gpsimd.tensor_tensor(out=hb, in0=tmp, in1=bbc, op=ALU.add),
                 r=[tk, bck], w=[hk])

    for l in range(L):
        xsrc = x_in if l == 0 else xs
        xsrc_key = "x_in" if l == 0 else "xs"

        with contextlib.ExitStack() as st:
            WA = load_w(st, "WA", wa_in[l], D, CA, npad=CA_PAD)
            WQ = load_w(st, "WQ", wq_in[l], QL, NH * 128)
            WKV = load_w(st, "WKV", wkv_in[l], KVL, 1024)
            gq = st.enter_context(SB("gq", [128, 2 * L], F32))
            gkv = st.enter_context(SB("gkv", [128, L], F32))
            nbf = st.enter_context(SB("nbf", [NH, 1], F32))
            S.dma("sp", gq[:], gq_in[:, :], r=[dkey("gq")], w=[("gq",)])
            S.dma("sp", gkv[:], gkv_in[:, :], r=[dkey("gkv")], w=[("gkv",)])
            with nc.allow_non_contiguous_dma(reason="8-element bias column"):
                S.dma("sp", nbf[:], bfg_in[l, :].rearrange("(h o) -> h o", o=1), r=[dkey("bf")], w=[("nbf",)])
            S.op("dve", lambda: nc.vector.tensor_scalar(out=nbf[:], in0=nbf[:], scalar1=-1.0, scalar2=None,
                                                        op0=ALU.mult), r=[("nbf",)], w=[("nbf",)])
            NXB = 5
            xbuf = [st.enter_context(SB("xb%d" % i, [128, D], F32)) for i in range(NXB)]
            junk = st.enter_context(SB("junk", [128, D], BF16))
            tmpf = [st.enter_context(SB("tmpf%d" % i, [128, D], F32)) for i in range(2)]
            hbuf = [st.enter_context(SB("hb%d" % i, [128, D], BF16)) for i in range(2)]
            ssb = [st.enter_context(SB("ss%d" % i, [128, 1], F32)) for i in range(4)]
            rsb = [st.enter_context(SB("rs%d" % i, [128, 1], F32)) for i in range(4)]
            hT = [st.enter_context(SB("hT%d" % i, [128, 8, 512], BF16)) for i in range(2)]
            abc = st.enter_context(SB("abc", [128, D], F32))
            bbc = st.enter_context(SB("bbc", [128, D], F32))
            tabt = [st.enter_context(SB("tabt%d" % i, [128, 512], F32)) for i in range(2)]
            craw = st.enter_context(SB("craw", [128, 3, 512], F32))
            sq = st.enter_context(SB("sq", [128, 3, 512], BF16))
            rq = st.enter_context(SB("rq", [128, 2, 512], F32))
            cqn = st.enter_context(SB("cqn", [128, 2, 512], BF16))
            ckvn = st.enter_context(SB("ckvn", [128, 512], BF16))
            rt = [st.enter_context(SB("rt%d" % i, [32, 512], F32)) for i in range(4)]
            NST = 8
            stg = [st.enter_context(SB("stg%d" % i, [128, 512], BF16)) for i in range(NST)]
            vst = [st.enter_context(SB("vst%d" % i, [128, NH, 128], BF16)) for i in range(4)]
            fl = [st.enter_context(SB("fl%d" % i, [NH, 512], F32)) for i in range(3)]
            Lt = [st.enter_context(SB("Lt%d" % i, [NH, 512], F32)) for i in range(2)]
            l3 = st.enter_context(SB("l3", [NH, 6, 512], BF16))
            pst = st.enter_context(PS("pst", [128, D], BF16))
            pp = [st.enter_context(PS("pp%d" % i, [128, 512], F32)) for i in range(7)]
            for i in range(4):
                S.op("pool", lambda i=i: nc.gpsimd.memset(vst[i][:, :, 64:128], 1.0), w=[("vst", i)])
            cnt = {"x": 0, "pp": 0, "stg": 0, "vst": 0, "ss": 0, "rt": 0}

            def nxt(name, n):
                v = cnt[name] % n
                cnt[name] += 1
                return v

            def stage1(b, tt, hTi):
                for blk in range(4):
                    r0 = b * SEQ + tt * 512 + blk * 128
                    xi = nxt("x", NXB)
                    S.dma("sp", xbuf[xi][:], xsrc[r0:r0 + 128, :], r=[dkey(xsrc_key)], w=[("xb", xi)])
                    si = nxt("ss", 4)
                    hi = blk % 2
                    norm_block(xbuf[xi][:], ("xb", xi), ssb[si][:], ("ss", si), rsb[si][:], ("rs", si),
                               junk[:], ("junk",), tmpf[hi][:], ("tmpf", hi), hbuf[hi][:], ("hb", hi),
                               abc[:], bbc[:], ("abc",))
                    for kc in range(8):
                        S.op("pe", lambda kc=kc, hi=hi: nc.tensor.transpose(
                            pst[:, kc * 128:(kc + 1) * 128], hbuf[hi][:, kc * 128:(kc + 1) * 128], ident[:]),
                            r=[("hb", hi), ("ident",)], w=[("pst",)])
                    S.op("act", lambda blk=blk: nc.scalar.copy(
                        out=hT[hTi][:, :, blk * 128:(blk + 1) * 128],
                        in_=pst[:].rearrange("p (k t) -> p k t", k=8)),
                        r=[("pst",)], w=[("hT", hTi)])

            def group(c0, M, hTi):
                pi = nxt("pp", 7)
                for kc in range(8):
                    S.op("pe", lambda kc=kc: nc.tensor.matmul(pp[pi][0:M, :], lhsT=WA[:, kc, c0:c0 + M],
                                                              rhs=hT[hTi][:, kc, :], start=(kc == 0), stop=(kc == 7)),
                         r=[("WA",), ("hT", hTi)], w=[("pp", pi)])
                return pi

            def store_stage(si, nrows, dst, dk):
                S.dma("sp", dst, stg[si][0:nrows, :], r=[("stg", si)], w=[dk])

            def stage2(b, tt, hTi, carry):
                t0 = tt * 512
                tsl = slice(t0, t0 + 512)
                ti = tt % 2
                stop_here("A%ds2" % l)
                S.dma("sp", tabt[ti][:], tabs[b, :, tsl], r=[dkey("tabs", b)], w=[("tabt", ti)])
                tab = tabt[ti]
                tabk = ("tabt", ti)
                stop_here("A%dt" % l)
                for j in range(3):
                    pi = group(CA_CQ + j * 128, 128, hTi)
                    stop_here("A%dm" % l)
                    S.op("act", lambda j=j, pi=pi: nc.scalar.copy(out=craw[:, j, :], in_=pp[pi][:]),
                         r=[("pp", pi)], w=[("craw", j)])
                    stop_here("A%da" % l)
                    S.op("dve", lambda j=j: nc.vector.tensor_tensor(out=sq[:, j, :], in0=craw[:, j, :], in1=craw[:, j, :],
                                                                    op=ALU.mult), r=[("craw", j)], w=[("sq", j)])
                    stop_here("A%dd" % l)
                    if j == 1:
                        stop_here("A%dd1" % l)
                stop_here("A%dg" % l)
                pq = nxt("pp", 7)
                S.op("pe", lambda: nc.tensor.matmul(pp[pq][:], lhsT=ones_bf[:], rhs=sq[:, 0, :], start=True, stop=False),
                     r=[("sq", 0), ("ones_bf",)], w=[("pp", pq)])
                S.op("pe", lambda: nc.tensor.matmul(pp[pq][:], lhsT=ones_bf[:], rhs=sq[:, 1, :], start=False, stop=True),
                     r=[("sq", 1), ("ones_bf",)], w=[("pp", pq)])
                rms_rstd((pp[pq][:], ("pp", pq)), (rq[:, 0, :], ("rq", 0)), QL)
                pk = nxt("pp", 7)
                S.op("pe", lambda: nc.tensor.matmul(pp[pk][:], lhsT=ones_bf[:], rhs=sq[:, 2, :], start=True, stop=True),
                     r=[("sq", 2), ("ones_bf",)], w=[("pp", pk)])
                rms_rstd((pp[pk][:], ("pp", pk)), (rq[:, 1, :], ("rq", 1)), KVL)
                for j in range(2):
                    S.op("dve", lambda j=j: nc.vector.scalar_tensor_tensor(
                        out=cqn[:, j, :], in0=craw[:, j, :], scalar=gq[:, 2 * l + j:2 * l + j + 1], in1=rq[:, 0, :],
                        op0=ALU.mult, op1=ALU.mult), r=[("craw", j), ("gq",), ("rq", 0)], w=[("cqn", j)])
                S.op("dve", lambda: nc.vector.scalar_tensor_tensor(
                    out=ckvn[:], in0=craw[:, 2, :], scalar=gkv[:, l:l + 1], in1=rq[:, 1, :],
                    op0=ALU.mult, op1=ALU.mult), r=[("craw", 2), ("gkv",), ("rq", 1)], w=[("ckvn",)])

                def rope_evict(pi, dst, dk, cos_rows, sin_rows, pbase=0):
                    r1, r2 = nxt("rt", 4), nxt("rt", 4)
                    S.op("dve", lambda: nc.vector.tensor_tensor(out=rt[r1][:], in0=pp[pi][pbase:pbase + 32, :],
                                                                in1=tab[cos_rows, :], op=ALU.mult),
                         r=[("pp", pi), tabk], w=[("rt", r1)])
                    S.op("dve", lambda: nc.vector.tensor_tensor(out=rt[r2][:], in0=pp[pi][pbase + 32:pbase + 64, :],
                                                                in1=tab[sin_rows, :], op=ALU.mult),
                         r=[("pp", pi), tabk], w=[("rt", r2)])
                    S.op("pool", lambda: nc.gpsimd.tensor_tensor(out=dst, in0=rt[r1][:], in1=rt[r2][:], op=ALU.add),
                         r=[("rt", r1), ("rt", r2)], w=[dk])

                stop_here("A%d%s" % (l, "x1"))
                pi = group(CA_KR, 64, hTi)
                si = nxt("stg", NST)
                rope_evict(pi, stg[si][0:32, :], ("stg", si), slice(0, 32), slice(64, 96))
                store_stage(si, 32, kropeT[b, :, tsl], dkey("kropeT", b))
                stop_here("A%d%s" % (l, "x2"))
                for h in range(NH):
                    pi = nxt("pp", 7)
                    for kc in range(2):
                        S.op("pe", lambda kc=kc, h=h: nc.tensor.matmul(
                            pp[pi][:], lhsT=WQ[:, kc, h * 128:(h + 1) * 128], rhs=cqn[:, kc, :],
                            start=(kc == 0), stop=(kc == 1)), r=[("WQ",), ("cqn", kc)], w=[("pp", pi)])
                    si = nxt("stg", NST)
                    S.op("act", lambda pi=pi, si=si: nc.scalar.mul(out=stg[si][0:64, :], in_=pp[pi][0:64, :],
                                                                   mul=MLA_SCALE),
                         r=[("pp", pi)], w=[("stg", si)])
                    rope_evict(pi, stg[si][64:96, :], ("stg", si), slice(32, 64), slice(96, 128), 64)
                    store_stage(si, 96, qmT[b, h, :, tsl], dkey("qmT", b, h))
                stop_here("A%d%s" % (l, "x3"))
                for g in range(4):
                    pi = nxt("pp", 7)
                    S.op("pe", lambda g=g: nc.tensor.matmul(pp[pi][:], lhsT=WKV[:, 0, g * 128:(g + 1) * 128],
                                                            rhs=ckvn[:], start=True, stop=True),
                         r=[("WKV",), ("ckvn",)], w=[("pp", pi)])
                    si = nxt("stg", NST)
                    S.op("dve", lambda pi=pi, si=si: nc.vector.tensor_copy(out=stg[si][:], in_=pp[pi][:]),
                         r=[("pp", pi)], w=[("stg", si)])
                    store_stage(si, 128, knopeT[b, g * 128:(g + 1) * 128, tsl], dkey("knopeT", b, g))
                stop_here("A%d%s" % (l, "x4"))
                for blk in range(4):
                    pi = nxt("pp", 7)
                    S.op("pe", lambda blk=blk: nc.tensor.matmul(pp[pi][:], lhsT=ckvn[:, blk * 128:(blk + 1) * 128],
                                                                rhs=WKV[:, 0, 512:1024], start=True, stop=True),
                         r=[("WKV",), ("ckvn",)], w=[("pp", pi)])
                    vi = nxt("vst", 4)
                    S.op("act", lambda pi=pi, vi=vi: nc.scalar.copy(
                        out=vst[vi][:, :, 0:64], in_=pp[pi][:].rearrange("p (h d) -> p h d", h=NH)),
                        r=[("pp", pi)], w=[("vst", vi)])
                    r0 = t0 + blk * 128
                    S.dma("sp", vmla[b, r0:r0 + 128, :], vst[vi][:].rearrange("p h d -> p (h d)"),
                          r=[("vst", vi)], w=[dkey("vmla", b)])
                stop_here("A%d%s" % (l, "x5"))
                for g in range(4):
                    pi = group(CA_FQ + g * 128, 128, hTi)
                    si = nxt("stg", NST)
                    S.op("act", lambda pi=pi, si=si: nc.scalar.mul(out=stg[si][:], in_=pp[pi][:], mul=FOX_SCALE),
                         r=[("pp", pi)], w=[("stg", si)])
                    store_stage(si, 128, fqT[b, g * 128:(g + 1) * 128, tsl], dkey("fqT", b, g))
                for g in range(4):
                    pi = group(CA_FK + g * 128, 128, hTi)
                    si = nxt("stg", NST)
                    S.op("dve", lambda pi=pi, si=si: nc.vector.tensor_copy(out=stg[si][:], in_=pp[pi][:]),
                         r=[("pp", pi)], w=[("stg", si)])
                    store_stage(si, 128, fkT[b, g * 128:(g + 1) * 128, tsl], dkey("fkT", b, g))
                stop_here("A%d%s" % (l, "x6"))
                for blk in range(4):
                    pi = nxt("pp", 7)
                    for kc in range(8):
                        S.op("pe", lambda kc=kc, blk=blk: nc.tensor.matmul(
                            pp[pi][:], lhsT=hT[hTi][:, kc, blk * 128:(blk + 1) * 128],
                            rhs=WA[:, kc, CA_FV:CA_FV + 512], start=(kc == 0), stop=(kc == 7)),
                            r=[("WA",), ("hT", hTi)], w=[("pp", pi)])
                    vi = nxt("vst", 4)
                    S.op("act", lambda pi=pi, vi=vi: nc.scalar.copy(
                        out=vst[vi][:, :, 0:64], in_=pp[pi][:].rearrange("p (h d) -> p h d", h=NH)),
                        r=[("pp", pi)], w=[("vst", vi)])
                    r0 = t0 + blk * 128
                    S.dma("sp", vfox[b, r0:r0 + 128, :], vst[vi][:].rearrange("p h d -> p (h d)"),
                          r=[("vst", vi)], w=[dkey("vfox", b)])
                stop_here("A%d%s" % (l, "x7"))
                pi = group(CA_FL, NH, hTi)
                S.op("act", lambda: nc.scalar.activation(out=fl[0][:], in_=pp[pi][0:NH, :], func=AF.Exp,
                                                         bias=nbf[:, 0:1], scale=-1.0),
                     r=[("pp", pi), ("nbf",)], w=[("fl", 0)])
                S.op("act", lambda: nc.scalar.activation(out=fl[1][:], in_=fl[0][:], func=AF.Ln, bias=1.0, scale=1.0),
                     r=[("fl", 0)], w=[("fl", 1)])
                li = tt % 2
                init = 0.0 if carry is None else carry
                rr = [("fl", 1), ("ones_f",)] + ([("Lt", 1 - li)] if carry is not None else [])
                S.op("dve", lambda: nc.vector.tensor_tensor_scan(out=Lt[li][:], data0=ones_f[0:NH, :], data1=fl[1][:],
                                                                 initial=init, op0=ALU.mult, op1=ALU.subtract),
                     r=rr, w=[("Lt", li)])
                S.op("dve", lambda: nc.vector.tensor_copy(out=l3[:, 0, :], in_=Lt[li][:]), r=[("Lt", li)], w=[("l3",)])
                S.op("dve", lambda: nc.vector.tensor_tensor(out=fl[2][:], in0=Lt[li][:], in1=l3[:, 0, :], op=ALU.subtract),
                     r=[("Lt", li), ("l3",)], w=[("fl", 2)])
                S.op("dve", lambda: nc.vector.tensor_copy(out=l3[:, 1, :], in_=fl[2][:]), r=[("fl", 2)], w=[("l3",)])
                S.op("dve", lambda: nc.vector.tensor_tensor(out=fl[0][:], in0=fl[2][:], in1=l3[:, 1, :], op=ALU.subtract),
                     r=[("fl", 2), ("l3",)], w=[("fl", 0)])
                S.op("dve", lambda: nc.vector.tensor_copy(out=l3[:, 2, :], in_=fl[0][:]), r=[("fl", 0)], w=[("l3",)])
                S.op("dve", lambda: nc.vector.tensor_scalar(out=l3[:, 3:6, :], in0=l3[:, 0:3, :], scalar1=-1.0,
                                                            scalar2=None, op0=ALU.mult), r=[("l3",)], w=[("l3",)])
                S.dma("sp", L3[b, :, :, tsl], l3[:, 0:3, :], r=[("l3",)], w=[dkey("L3", b)], key=("l3",))
                S.dma("sp", nL3[b, :, :, tsl], l3[:, 3:6, :], r=[("l3",)], w=[dkey("nL3", b)], key=("l3",))
                stop_here("A%d%s" % (l, "x8"))
                for g in range(16):
                    pi = group(CA_GA + g * 128, 128, hTi)
                    si = nxt("stg", NST)
                    S.op("act", lambda pi=pi, si=si: nc.scalar.activation(out=stg[si][:], in_=pp[pi][:], func=AF.Sigmoid),
                         r=[("pp", pi)], w=[("stg", si)])
                    store_stage(si, 128, sgT[b, g * 128:(g + 1) * 128, tsl], dkey("sgT", b, g))
                return Lt[li][:, 511:512]

            stop_here("A%dw" % l)
            for b in range(NSEQ):
                S.dma("sp", abc[:], modrows[l, 0, b, 0, :].partition_broadcast(128), r=[dkey("modrows", l, 0)], w=[("abc",)])
                S.dma("sp", bbc[:], modrows[l, 0, b, 1, :].partition_broadcast(128), r=[dkey("modrows", l, 0)], w=[("abc",)],
                      key=("abc",))
                carry = None
                stage1(b, 0, 0)
                stop_here("A%ds1" % l)
                for tt in range(NT):
                    if tt + 1 < NT:
                        stage1(b, tt + 1, (tt + 1) % 2)
                    carry = stage2(b, tt, tt % 2, carry)
            S.barrier()
        stop_here("A%d" % l)

        with contextlib.ExitStack() as st:
            QA = [st.enter_context(SB("QA%d" % i, [128, SEQ], BF16)) for i in range(2)]
            KA = [st.enter_context(SB("KA%d" % i, [128, SEQ], BF16)) for i in range(2)]
            VA = [st.enter_context(SB("VA%d" % i, [128, SEQ // 128, 128], BF16)) for i in range(2)]
            NPT = 4
            NPS = 5
            LA = 3
            PT = [st.enter_context(SB("PT%d" % i, [128, 512], BF16)) for i in range(NPT)]
            rcp = [st.enter_context(SB("rcp%d" % i, [128, 512], F32)) for i in range(2)]
            yst = [st.enter_context(SB("yst%d" % i, [64, 512], BF16)) for i in range(3)]
            pS = [st.enter_context(PS("pS%d" % i, [128, 512], F32)) for i in range(NPS)]
            pO = [st.enter_context(PS("pO%d" % i, [128, 512], F32)) for i in range(2)]
            cB = {"po": 0, "y": 0}
            heads = [(b, mixer, h) for b in range(NSEQ) for mixer in range(2) for h in range(NH)]

            def load_head(n):
                b, mixer, h = heads[n]
                bi = n % 2
                if mixer == 0:
                    S.dma("sp", QA[bi][0:96, :], qmT[b, h, :, :], r=[dkey("qmT", b, h)], w=[("QA", bi)])
                    S.dma("sp", KA[bi][64:96, :], kropeT[b, :, :], r=[dkey("kropeT", b)], w=[("KA", bi)])
                    S.dma("sp", KA[bi][0:64, :], knopeT[b, h * 64:(h + 1) * 64, :],
                          r=[dkey("knopeT", b, h // 2)], w=[("KA", bi)])
                    vsrc = vmla
                else:
                    S.op("pool", lambda: nc.gpsimd.memset(QA[bi][64:96, :], 1.0), w=[("QA", bi)])
                    S.op("pool", lambda: nc.gpsimd.memset(KA[bi][64:96, :], 1.0), w=[("KA", bi)])
                    S.dma("sp", QA[bi][0:64, :], fqT[b, h * 64:(h + 1) * 64, :], r=[dkey("fqT", b, h // 2)],
                          w=[("QA", bi)])
                    S.dma("sp", QA[bi][64:67, :], L3[b, h, :, :], r=[dkey("L3", b)], w=[("QA", bi)])
                    S.dma("sp", KA[bi][0:64, :], fkT[b, h * 64:(h + 1) * 64, :], r=[dkey("fkT", b, h // 2)],
                          w=[("KA", bi)])
                    S.dma("sp", KA[bi][67:70, :], nL3[b, h, :, :], r=[dkey("nL3", b)], w=[("KA", bi)])
                    vsrc = vfox
                S.dma("sp", VA[bi][:], vsrc[b, :, h * 128:(h + 1) * 128].rearrange("(k p) d -> p k d", p=128),
                      r=[dkey("v", b)], w=[("VA", bi)])

            load_head(0)
            per_head = (len(fills) + len(heads) - 1) // len(heads) if l == 0 else 0
            for n, (b, mixer, h) in enumerate(heads):
                if n + 1 < len(heads):
                    load_head(n + 1)
                for dst, src in fills[n * per_head:(n + 1) * per_head]:
                    S.dma("sp", dst, src, r=[("zfill_f",)], w=[dkey("fill")], key=("zfill_f",))
                bi = n % 2
                dq = 96 if mixer == 0 else 70
                tiles = []
                for qt in range(NT):
                    nkb = 4 * qt + 4
                    for kb in range(nkb):
                        tiles.append((qt, kb, nkb))

                def emit_qk(i):
                    qt, kb, nkb = tiles[i]
                    j = kb - 4 * qt
                    q0 = 0 if j < 0 else j * 128
                    n_ = 512 - q0
                    si = i % NPS
                    diag = j >= 0
                    S.op("pe", lambda: nc.tensor.matmul(
                        pS[si][:, 0:n_], lhsT=KA[bi][0:dq, kb * 128:(kb + 1) * 128],
                        rhs=QA[bi][0:dq, qt * 512 + q0:qt * 512 + 512],
                        start=True, stop=not diag), r=[("KA", bi), ("QA", bi)], w=[("pS", si)])
                    if diag:
                        S.op("pe", lambda: nc.tensor.matmul(
                            pS[si][:, 0:128], lhsT=ident[:], rhs=maskT[:], start=False, stop=True),
                            r=[("ident",), ("maskT",)], w=[("pS", si)])

                for i in range(min(LA, len(tiles))):
                    emit_qk(i)
                oi = None
                for i, (qt, kb, nkb) in enumerate(tiles):
                    if i + LA < len(tiles):
                        emit_qk(i + LA)
                    if kb == 0:
                        oi = cB["po"] % 2
                        cB["po"] += 1
                    j = kb - 4 * qt
                    q0 = 0 if j < 0 else j * 128
                    n_ = 512 - q0
                    si = i % NPS
                    pi = i % NPT
                    S.op("act", lambda: nc.scalar.activation(out=PT[pi][:, 0:n_], in_=pS[si][:, 0:n_], func=AF.Exp),
                         r=[("pS", si)], w=[("PT", pi)])
                    S.op("pe", lambda: nc.tensor.matmul(
                        pO[oi][:, q0:512], lhsT=VA[bi][:, kb, :], rhs=PT[pi][:, 0:n_],
                        start=(kb == 0), stop=(kb == nkb - 1)), r=[("VA", bi), ("PT", pi)], w=[("pO", oi)])
                    if kb == nkb - 1:
                        ri = oi
                        S.op("dve", lambda: nc.vector.reciprocal(out=rcp[ri][64:128, :], in_=pO[oi][64:128, :]),
                             r=[("pO", oi)], w=[("rcp", ri)])
                        yi = cB["y"] % 3
                        cB["y"] += 1
                        S.op("dve", lambda: nc.vector.tensor_tensor(out=yst[yi][:], in0=pO[oi][0:64, :],
                                                                    in1=rcp[ri][64:128, :], op=ALU.mult),
                             r=[("pO", oi), ("rcp", ri)], w=[("yst", yi)])
                        row0 = mixer * 512 + h * 64
                        S.dma("sp", yT[b, row0:row0 + 64, qt * 512:(qt + 1) * 512], yst[yi][:],
                              r=[("yst", yi)], w=[dkey("yT", b, row0 // 128)])
            S.barrier()
        stop_here("B%d" % l)

        with contextlib.ExitStack() as st:
            WBM = load_w(st, "WBM", wbm_in[l], 512, D)
            WBF = load_w(st, "WBF", wbf_in[l], 512, D)
            WO = load_w(st, "WO", wo_in[l], D, D)
            yt = [st.enter_context(SB("yt%d" % i, [128, 8, 512], BF16)) for i in range(2)]
            sgt = [st.enter_context(SB("sgt%d" % i, [128, 16, 512], BF16)) for i in range(2)]
            xt = [st.enter_context(SB("xt%d" % i, [128, 4, D], F32)) for i in range(2)]
            mT = [st.enter_context(SB("mT%d" % i, [128, 8, 512], BF16)) for i in range(2)]
            tA = [st.enter_context(SB("tA%d" % i, [128, 512], F32)) for i in range(3)]
            tB = [st.enter_context(SB("tB%d" % i, [128, 512], F32)) for i in range(3)]
            tO = [st.enter_context(SB("tO%d" % i, [128, 512], F32)) for i in range(3)]
            pC = [st.enter_context(PS("pC%d" % i, [128, 512], F32)) for i in range(8)]
            cC = {"p": 0, "t": 0, "o": 0}
            it = 0
            tilesC = [(b, tt) for b in range(NSEQ) for tt in range(NT)]

            def loadC(n):
                b, tt = tilesC[n]
                i2 = n % 2
                tsl = slice(tt * 512, tt * 512 + 512)
                r0 = b * SEQ + tt * 512
                S.dma("sp", yt[i2][:], yT[b, :, tsl].rearrange("(k p) t -> p k t", p=128),
                      r=[dkey("yT", b)], w=[("yt", i2)])
                S.dma("sp", sgt[i2][:], sgT[b, :, tsl].rearrange("(k p) t -> p k t", p=128),
                      r=[dkey("sgT", b)], w=[("sgt", i2)])
                S.dma("sp", xt[i2][:], xsrc[r0:r0 + 512, :].rearrange("(k p) d -> p k d", p=128),
                      r=[dkey(xsrc_key)], w=[("xt", i2)])

            gbcC = [st.enter_context(SB("gbcC%d" % i, [128, D], F32)) for i in range(NSEQ)]
            for b in range(NSEQ):
                S.dma("sp", gbcC[b][:], modrows[l, 0, b, 2, :].partition_broadcast(128), r=[dkey("modrows", l, 0)],
                      w=[("gbcC", b)])
            loadC(0)
            for b in range(NSEQ):
                for tt in range(NT):
                    i2 = it % 2
                    it += 1
                    if it < len(tilesC):
                        loadC(it)
                    tsl = slice(tt * 512, tt * 512 + 512)
                    r0 = b * SEQ + tt * 512
                    for dc in range(8):
                        pa = cC["p"] % 8
                        pb = (cC["p"] + 1) % 8
                        cC["p"] += 2
                        for kc in range(4):
                            S.op("pe", lambda kc=kc: nc.tensor.matmul(
                                pC[pa][:], lhsT=WBM[:, kc, dc * 128:(dc + 1) * 128], rhs=yt[i2][:, kc, :],
                                start=(kc == 0), stop=(kc == 3)), r=[("WBM",), ("yt", i2)], w=[("pC", pa)])
                        for kc in range(4):
                            S.op("pe", lambda kc=kc: nc.tensor.matmul(
                                pC[pb][:], lhsT=WBF[:, kc, dc * 128:(dc + 1) * 128], rhs=yt[i2][:, 4 + kc, :],
                                start=(kc == 0), stop=(kc == 3)), r=[("WBF",), ("yt", i2)], w=[("pC", pb)])
                        ti = cC["t"] % 3
                        cC["t"] += 1
                        S.op("dve", lambda: nc.vector.tensor_tensor(out=tA[ti][:], in0=pC[pa][:], in1=sgt[i2][:, dc, :],
                                                                    op=ALU.mult), r=[("pC", pa), ("sgt", i2)], w=[("tA", ti)])
                        S.op("dve", lambda: nc.vector.tensor_tensor(out=tB[ti][:], in0=pC[pb][:], in1=sgt[i2][:, 8 + dc, :],
                                                                    op=ALU.mult), r=[("pC", pb), ("sgt", i2)], w=[("tB", ti)])
                        S.op("pool", lambda: nc.gpsimd.tensor_tensor(out=mT[i2][:, dc, :], in0=tA[ti][:], in1=tB[ti][:],
                                                                     op=ALU.add), r=[("tA", ti), ("tB", ti)], w=[("mT", i2, dc)])
                    for blk in range(4):
                        for half in range(2):
                            po = cC["p"] % 8
                            cC["p"] += 1
                            for kc in range(8):
                                S.op("pe", lambda kc=kc: nc.tensor.matmul(
                                    pC[po][:], lhsT=mT[i2][:, kc, blk * 128:(blk + 1) * 128],
                                    rhs=WO[:, kc, half * 512:(half + 1) * 512], start=(kc == 0), stop=(kc == 7)),
                                    r=[("WO",), ("mT", i2, kc)], w=[("pC", po)])
                            oi = cC["o"] % 3
                            cC["o"] += 1
                            hs = slice(half * 512, half * 512 + 512)
                            S.op("dve", lambda: nc.vector.tensor_tensor(out=tO[oi][:], in0=pC[po][:], in1=gbcC[b][:, hs],
                                                                        op=ALU.mult), r=[("pC", po), ("gbcC", b)], w=[("tO", oi)])
                            S.op("pool", lambda: nc.gpsimd.tensor_tensor(out=xt[i2][:, blk, hs], in0=xt[i2][:, blk, hs],
                                                                         in1=tO[oi][:], op=ALU.add),
                                 r=[("tO", oi), ("xt", i2)], w=[("xt", i2)])
                    S.dma("sp", xs[r0:r0 + 512, :].rearrange("(k p) d -> p k d", p=128), xt[i2][:],
                          r=[("xt", i2)], w=[dkey("xs")])
            S.barrier()
        stop_here("C%d" % l)

        is_moe = (l % 2 == 1)
        li = l // 2
        with contextlib.ExitStack() as st:
            NXB = 4
            xbuf = [st.enter_context(SB("dxb%d" % i, [128, D], F32)) for i in range(NXB)]
            junk = st.enter_context(SB("djunk", [128, D], BF16))
            tmpf = [st.enter_context(SB("dtmpf%d" % i, [128, D], F32)) for i in range(2)]
            hbuf = [st.enter_context(SB("dhb%d" % i, [128, D], BF16)) for i in range(2)]
            hf = [st.enter_context(SB("dhf%d" % i, [128, D], F32)) for i in range(2)]
            ssb = [st.enter_context(SB("dss%d" % i, [128, 1], F32)) for i in range(4)]
            rsb = [st.enter_context(SB("drs%d" % i, [128, 1], F32)) for i in range(4)]
            hTs = [st.enter_context(SB("dhT%d" % i, [128, 8, 128], BF16)) for i in range(3)]
            abc = st.enter_context(SB("dabc", [128, D], F32))
            bbc = st.enter_context(SB("dbbc", [128, D], F32))
            pst = st.enter_context(PS("dpst", [128, D], BF16))
            if is_moe:
                wrb = st.enter_context(SB("wrb", [128, NEXP, D], F32))
                lg = [st.enter_context(SB("lg%d" % i, [128, NEXP], F32)) for i in range(2)]
                m8 = [st.enter_context(SB("m8%d" % i, [128, 8], F32)) for i in range(2)]
                w12 = [st.enter_context(SB("w12%d" % i, [128, 4], F32)) for i in range(2)]
                cmb = [st.enter_context(SB("cmb%d" % i, [128, 2, NEXP], F32)) for i in range(2)]
                junkf = st.enter_context(SB("junkf", [128, D], F32))
                for e in range(NEXP):
                    S.dma("sp", wrb[:, e, :], wr_in[li, e, :].partition_broadcast(128), r=[dkey("wr")], w=[("wrb",)])
                base8 = st.enter_context(SB("base8", [128, NEXP], F32))
                mk8 = [st.enter_context(SB("mk8%d" % i, [128, NEXP], F32)) for i in range(2)]
                fl8 = [st.enter_context(SB("fl8%d" % i, [128, 2, NEXP], F32)) for i in range(2)]
                gf2 = [st.enter_context(SB("gf2%d" % i, [128, 4], F32)) for i in range(2)]
                gi2 = [st.enter_context(SB("gi2%d" % i, [128, 2], I32)) for i in range(2)]
                gw2 = [st.enter_context(SB("gw2%d" % i, [128, 2], F32)) for i in range(2)]
                cnti = st.enter_context(SB("cnti", [1, NEXP], I32))
                prt = st.enter_context(PS("prt", [128, 2 * NEXP], F32))
                S.op("pool", lambda: nc.gpsimd.memset(base8[:], 0.0), w=[("base8",)])
            ci = 0
            for b in range(NSEQ):
                S.dma("sp", abc[:], modrows[l, 1, b, 0, :].partition_broadcast(128), r=[dkey("modrows", l, 1)], w=[("abc",)])
                S.dma("sp", bbc[:], modrows[l, 1, b, 1, :].partition_broadcast(128), r=[dkey("modrows", l, 1)], w=[("abc",)],
                      key=("abc",))
                for blk in range(SEQ // 128):
                    r0 = b * SEQ + blk * 128
                    xi = ci % NXB
                    si = ci % 4
                    hi = ci % 2
                    ti = ci % 3
                    ci += 1
                    S.dma("sp", xbuf[xi][:], xs[r0:r0 + 128, :], r=[dkey("xs")], w=[("xb", xi)])
                    S.op("act", lambda: nc.scalar.activation(out=junk[:], in_=xbuf[xi][:], func=AF.Square,
                                                             accum_out=ssb[si][:]), r=[("xb", xi)], w=[("junk",), ("ss", si)])
                    rms_rstd((ssb[si][:], ("ss", si)), (rsb[si][:], ("rs", si)), D)
                    S.op("dve", lambda: nc.vector.scalar_tensor_tensor(out=tmpf[hi][:], in0=xbuf[xi][:], scalar=rsb[si][:],
                                                                       in1=abc[:], op0=ALU.mult, op1=ALU.mult),
                         r=[("xb", xi), ("rs", si), ("abc",)], w=[("tmpf", hi)])
                    S.op("pool", lambda: nc.gpsimd.tensor_tensor(out=hf[hi][:], in0=tmpf[hi][:], in1=bbc[:], op=ALU.add),
                         r=[("tmpf", hi), ("abc",)], w=[("hf", hi)])
                    S.op("act", lambda: nc.scalar.copy(out=hbuf[hi][:], in_=hf[hi][:]), r=[("hf", hi)], w=[("hb", hi)])
                    for kc in range(8):
                        S.op("pe", lambda kc=kc: nc.tensor.transpose(
                            pst[:, kc * 128:(kc + 1) * 128], hbuf[hi][:, kc * 128:(kc + 1) * 128], ident[:]),
                            r=[("hb", hi), ("ident",)], w=[("pst",)])
                    S.op("act", lambda: nc.scalar.copy(out=hTs[ti][:], in_=pst[:].rearrange("p (k t) -> p k t", k=8)),
                         r=[("pst",)], w=[("hTs", ti)])
                    S.dma("sp", h2T[:, r0:r0 + 128].rearrange("(k p) t -> p k t", p=128), hTs[ti][:],
                          r=[("hTs", ti)], w=[dkey("h2T")])
                    if is_moe:
                        for e in range(NEXP):
                            S.op("dve", lambda e=e: nc.vector.scalar_tensor_tensor(
                                out=junkf[:], in0=hf[hi][:], scalar=1.0, in1=wrb[:, e, :], op0=ALU.mult, op1=ALU.mult,
                                accum_out=lg[hi][:, e:e + 1]),
                                r=[("hf", hi), ("wrb",)], w=[("junkf",), ("lg", hi)])
                        S.op("dve", lambda: nc.vector.max(out=m8[hi][:], in_=lg[hi][:]), r=[("lg", hi)], w=[("m8", hi)])
                        S.op("dve", lambda: nc.vector.tensor_tensor(out=w12[hi][:, 0:1], in0=m8[hi][:, 1:2], in1=m8[hi][:, 0:1],
                                                                    op=ALU.subtract), r=[("m8", hi)], w=[("w12", hi)])
                        S.op("act", lambda: nc.scalar.activation(out=w12[hi][:, 1:2], in_=w12[hi][:, 0:1], func=AF.Exp),
                             r=[("w12", hi)], w=[("w12", hi)])
                        S.op("dve", lambda: nc.vector.tensor_scalar(out=w12[hi][:, 1:2], in0=w12[hi][:, 1:2], scalar1=1.0,
                                                                    scalar2=None, op0=ALU.add), r=[("w12", hi)], w=[("w12", hi)])
                        S.op("dve", lambda: nc.vector.reciprocal(out=w12[hi][:, 2:3], in_=w12[hi][:, 1:2]),
                             r=[("w12", hi)], w=[("w12", hi)])
                        S.op("dve", lambda: nc.vector.tensor_scalar(out=w12[hi][:, 3:4], in0=w12[hi][:, 2:3], scalar1=-1.0,
                                                                    scalar2=1.0, op0=ALU.mult, op1=ALU.add),
                             r=[("w12", hi)], w=[("w12", hi)])
                        S.op("dve", lambda: nc.vector.tensor_scalar(out=cmb[hi][:, 0, :], in0=lg[hi][:], scalar1=m8[hi][:, 0:1],
                                                                    scalar2=w12[hi][:, 2:3], op0=ALU.is_equal, op1=ALU.mult),
                             r=[("lg", hi), ("m8", hi), ("w12", hi)], w=[("cmb", hi)])
                        S.op("dve", lambda: nc.vector.tensor_scalar(out=cmb[hi][:, 1, :], in0=lg[hi][:], scalar1=m8[hi][:, 1:2],
                                                                    scalar2=w12[hi][:, 3:4], op0=ALU.is_equal, op1=ALU.mult),
                             r=[("lg", hi), ("m8", hi), ("w12", hi)], w=[("cmb", hi)])
                        S.op("dve", lambda: nc.vector.tensor_tensor(out=cmb[hi][:, 0, :], in0=cmb[hi][:, 0, :],
                                                                    in1=cmb[hi][:, 1, :], op=ALU.add),
                             r=[("cmb", hi)], w=[("cmb", hi)])
                        S.dma("sp", comb[r0:r0 + 128, :], cmb[hi][:, 0, :], r=[("cmb", hi)], w=[dkey("comb")])
                        S.op("dve", lambda: nc.vector.tensor_scalar(out=mk8[hi][:], in0=cmb[hi][:, 0, :], scalar1=0.0,
                                                                    scalar2=None, op0=ALU.is_gt), r=[("cmb", hi)], w=[("mk8", hi)])
                        S.op("pe", lambda: nc.tensor.matmul(prt[:, 0:NEXP], lhsT=ustr[:], rhs=mk8[hi][:], start=True, stop=True),
                             r=[("ustr",), ("mk8", hi)], w=[("prt",)])
                        S.op("pe", lambda: nc.tensor.matmul(prt[:, NEXP:2 * NEXP], lhsT=onesq[:], rhs=mk8[hi][:], start=True,
                                                            stop=True), r=[("onesq",), ("mk8", hi)], w=[("prt",)])
                        S.op("dve", lambda: nc.vector.tensor_tensor(out=fl8[hi][:, 0, :], in0=prt[:, 0:NEXP], in1=base8[:],
                                                                    op=ALU.add), r=[("prt",), ("base8",)], w=[("fl8", hi)])
                        S.op("dve", lambda: nc.vector.tensor_tensor(out=fl8[hi][:, 0, :], in0=fl8[hi][:, 0, :], in1=eoff[:],
                                                                    op=ALU.add), r=[("fl8", hi), ("eoff",)], w=[("fl8", hi)])
                        S.op("dve", lambda: nc.vector.tensor_tensor(out=fl8[hi][:, 0, :], in0=fl8[hi][:, 0, :], in1=mk8[hi][:],
                                                                    op=ALU.mult), r=[("fl8", hi), ("mk8", hi)], w=[("fl8", hi)])
                        S.op("dve", lambda: nc.vector.tensor_tensor(out=base8[:], in0=prt[:, NEXP:2 * NEXP], in1=base8[:],
                                                                    op=ALU.add), r=[("prt",), ("base8",)], w=[("base8",)])
                        S.op("dve", lambda: nc.vector.tensor_reduce(out=gf2[hi][:, 1:2], in_=fl8[hi][:, 0, :],
                                                                    axis=mybir.AxisListType.X, op=ALU.max),
                             r=[("fl8", hi)], w=[("gf2", hi)])
                        S.op("dve", lambda: nc.vector.tensor_reduce(out=gf2[hi][:, 2:3], in_=fl8[hi][:, 0, :],
                                                                    axis=mybir.AxisListType.X, op=ALU.add),
                             r=[("fl8", hi)], w=[("gf2", hi)])
                        S.op("dve", lambda: nc.vector.tensor_tensor(out=gf2[hi][:, 0:1], in0=gf2[hi][:, 2:3], in1=gf2[hi][:, 1:2],
                                                                    op=ALU.subtract), r=[("gf2", hi)], w=[("gf2", hi)])
                        S.op("dve", lambda: nc.vector.tensor_copy(out=gi2[hi][:], in_=gf2[hi][:, 0:2]), r=[("gf2", hi)],
                             w=[("gi2", hi)])
                        S.op("dve", lambda: nc.vector.tensor_scalar(out=fl8[hi][:, 1, :], in0=fl8[hi][:, 0, :],
                                                                    scalar1=gf2[hi][:, 1:2], scalar2=None, op0=ALU.is_equal),
                             r=[("fl8", hi), ("gf2", hi)], w=[("fl8", hi)])
                        S.op("dve", lambda: nc.vector.tensor_tensor(out=fl8[hi][:, 1, :], in0=fl8[hi][:, 1, :],
                                                                    in1=cmb[hi][:, 0, :], op=ALU.mult),
                             r=[("fl8", hi), ("cmb", hi)], w=[("fl8", hi)])
                        S.op("dve", lambda: nc.vector.tensor_reduce(out=gw2[hi][:, 1:2], in_=fl8[hi][:, 1, :],
                                                                    axis=mybir.AxisListType.X, op=ALU.add),
                             r=[("fl8", hi)], w=[("gw2", hi)])
                        S.op("dve", lambda: nc.vector.tensor_scalar(out=gw2[hi][:, 0:1], in0=gw2[hi][:, 1:2], scalar1=-1.0,
                                                                    scalar2=1.0, op0=ALU.mult, op1=ALU.add),
                             r=[("gw2", hi)], w=[("gw2", hi)])
                        S.dma("sp", gidx[r0:r0 + 128, :], gi2[hi][:], r=[("gi2", hi)], w=[dkey("gidx")])
                        S.dma("sp", gwd[r0:r0 + 128, :], gw2[hi][:], r=[("gw2", hi)], w=[dkey("gwd")])
                        for k in range(2):
                            S.dma_indirect("pool", out=hg, out_offset=bass.IndirectOffsetOnAxis(ap=gi2[hi][:, k:k + 1], axis=0),
                                           in_=hbuf[hi][:], in_offset=None, r=[("hb", hi), ("gi2", hi)], w=[dkey("hg")],
                                           key=("hb", hi))
            if is_moe:
                S.op("dve", lambda: nc.vector.tensor_copy(out=cnt_sb[:], in_=base8[0:1, :]), r=[("base8",)], w=[("cnt_sb",)])
                S.dma("sp", cnts[:, :], cnt_sb[:], r=[("cnt_sb",)], w=[dkey("cnts")])
            S.barrier()
        stop_here("D%d" % l)

        if is_moe:
            FQ = DFF // 4
            NFC = FQ // 128
            passes = [(e, q) for e in range(NEXP) for q in range(4)]
            with contextlib.ExitStack() as st:
                WG = [st.enter_context(SB("rWG%d" % i, [128, 8, FQ], BF16)) for i in range(2)]
                WU = [st.enter_context(SB("rWU%d" % i, [128, 8, FQ], BF16)) for i in range(2)]
                WD = [st.enter_context(SB("rWD%d" % i, [128, NFC, D], BF16)) for i in range(2)]
                htm = [st.enter_context(SB("htm%d" % i, [128, 4, D], BF16)) for i in range(2)]
                h2 = [st.enter_context(SB("rh2%d" % i, [128, 8, 512], BF16)) for i in range(2)]
                AT = [st.enter_context(SB("rAT%d" % i, [128, NFC, 512], BF16)) for i in range(2)]
                sg = [st.enter_context(SB("rsg%d" % i, [128, 512], F32)) for i in range(3)]
                ot = [st.enter_context(SB("rot%d" % i, [128, 4, D], F32)) for i in range(2)]
                pE = [st.enter_context(PS("rpE%d" % i, [128, 512], F32)) for i in range(7)]
                pstE = st.enter_context(PS("rpst", [128, D], BF16))
                cE = {"p": 0, "s": 0}

                def load_set(pi):
                    e, q = passes[pi]
                    ws = pi % 2
                    gsrc, usrc, dsrc = mwg_in[li, e], mwu_in[li, e], mwd_in[li, e]
                    for k in range(8):
                        S.dma("pool", WG[ws][:, k, :], gsrc[k * 128:(k + 1) * 128, q * FQ:(q + 1) * FQ],
                              r=[dkey("wg")], w=[("WG", ws)])
                    for k in range(8):
                        S.dma("pool", WU[ws][:, k, :], usrc[k * 128:(k + 1) * 128, q * FQ:(q + 1) * FQ],
                              r=[dkey("wu")], w=[("WU", ws)])
                    for k in range(NFC):
                        S.dma("pool", WD[ws][:, k, :], dsrc[q * FQ + k * 128:q * FQ + (k + 1) * 128, :],
                              r=[dkey("wd")], w=[("WD", ws)])

                def load_slots(Sx, pi, it_, i2):
                    e, q = passes[pi]
                    row0 = e * CAPR + it_ * 512
                    if q == 0:
                        Sx.dma("sp", htm[i2][:], hg[row0:row0 + 512, :].rearrange("(k p) d -> p k d", p=128),
                               r=[dkey("hg")], w=[("htm", i2)])
                    else:
                        Sx.dma("sp", h2[i2][:], hgT[:, row0:row0 + 512].rearrange("(k p) t -> p k t", p=128),
                               r=[("hgTrow", e, it_)], w=[("h2", i2)], key=("h2", i2))
                    if q > 0:
                        Sx.dma("sp", ot[i2][:], og[row0:row0 + 512, :].rearrange("(k p) d -> p k d", p=128),
                               r=[("ogrow", e, it_)], w=[("ot", i2)], key=("ot", i2))

                def emit_tile(Sx, pi, it_, i2):
                    e, q = passes[pi]
                    ws = pi % 2
                    row0 = e * CAPR + it_ * 512
                    if q == 0:
                        for blk in range(4):
                            for kc in range(8):
                                Sx.op("pe", lambda kc=kc: nc.tensor.transpose(
                                    pstE[:, kc * 128:(kc + 1) * 128], htm[i2][:, blk, kc * 128:(kc + 1) * 128], ident[:]),
                                    r=[("htm", i2), ("ident",)], w=[("pstE",)])
                            Sx.op("act", lambda: nc.scalar.copy(out=h2[i2][:, :, blk * 128:(blk + 1) * 128],
                                                                in_=pstE[:].rearrange("p (k t) -> p k t", k=8)),
                                  r=[("pstE",)], w=[("h2", i2)])
                        Sx.dma("sp", hgT[:, row0:row0 + 512].rearrange("(k p) t -> p k t", p=128), h2[i2][:],
                               r=[("h2", i2)], w=[("hgTrow", e, it_)], key=("h2", i2))
                    for fc in range(NFC):
                        pg = cE["p"] % 7
                        pu = (cE["p"] + 1) % 7
                        cE["p"] += 2
                        for kc in range(8):
                            Sx.op("pe", lambda kc=kc: nc.tensor.matmul(
                                pE[pg][:], lhsT=WG[ws][:, kc, fc * 128:(fc + 1) * 128], rhs=h2[i2][:, kc, :],
                                start=(kc == 0), stop=(kc == 7)), r=[("WG", ws), ("h2", i2)], w=[("pE", pg)])
                        for kc in range(8):
                            Sx.op("pe", lambda kc=kc: nc.tensor.matmul(
                                pE[pu][:], lhsT=WU[ws][:, kc, fc * 128:(fc + 1) * 128], rhs=h2[i2][:, kc, :],
                                start=(kc == 0), stop=(kc == 7)), r=[("WU", ws), ("h2", i2)], w=[("pE", pu)])
                        si = cE["s"] % 3
                        cE["s"] += 1
                        Sx.op("act", lambda: nc.scalar.activation(out=sg[si][:], in_=pE[pg][:], func=AF.Silu),
                              r=[("pE", pg)], w=[("sg", si)])
                        Sx.op("dve", lambda: nc.vector.tensor_tensor(out=AT[i2][:, fc, :], in0=pE[pu][:], in1=sg[si][:],
                                                                     op=ALU.mult),
                              r=[("pE", pu), ("sg", si)], w=[("AT", i2, fc)])
                    for blk in range(4):
                        for half in range(2):
                            po = cE["p"] % 7
                            cE["p"] += 1
                            for fc in range(NFC):
                                Sx.op("pe", lambda fc=fc: nc.tensor.matmul(
                                    pE[po][:], lhsT=AT[i2][:, fc, blk * 128:(blk + 1) * 128],
                                    rhs=WD[ws][:, fc, half * 512:(half + 1) * 512], start=(fc == 0), stop=(fc == NFC - 1)),
                                    r=[("WD", ws), ("AT", i2, fc)], w=[("pE", po)])
                            hs = slice(half * 512, half * 512 + 512)
                            if q == 0:
                                Sx.op("dve", lambda: nc.vector.tensor_copy(out=ot[i2][:, blk, hs], in_=pE[po][:]),
                                      r=[("pE", po)], w=[("ot", i2)])
                            else:
                                Sx.op("dve", lambda: nc.vector.tensor_tensor(out=ot[i2][:, blk, hs], in0=pE[po][:],
                                                                             in1=ot[i2][:, blk, hs], op=ALU.add),
                                      r=[("pE", po), ("ot", i2)], w=[("ot", i2)])
                    Sx.dma("sp", og[row0:row0 + 512, :].rearrange("(k p) d -> p k d", p=128), ot[i2][:],
                           r=[("ot", i2)], w=[("ogrow", e, it_)], key=("ot", i2))

                load_set(0)
                for pi, (e, q) in enumerate(passes):
                    if pi + 1 < len(passes):
                        load_set(pi + 1)
                    load_slots(S, pi, 0, 0)
                    for it_ in range(KT):
                        if it_ + 1 < KT:
                            load_slots(S, pi, it_ + 1, (it_ + 1) % 2)
                        emit_tile(S, pi, it_, it_ % 2)
                    if KT < CAPT:
                        S.barrier()
                        for reg in cnt_reg:
                            nc.reg_load(reg, cnt_sb[0:1, e:e + 1])
                        dyn = [list(range(KT, CAPT))]
                        for grp in dyn:
                            with nc.If_cmp(cnt_reg, grp[0] * 512, "IS_GT"):
                                for it_ in grp:
                                    load_slots(S2, pi, it_, it_ % 2)
                                    emit_tile(S2, pi, it_, it_ % 2)
                                S2.finish_local_block()
                S.barrier()
            stop_here("E%d" % l)
            with contextlib.ExitStack() as st:
                xb = [st.enter_context(SB("gxb%d" % i, [128, D], F32)) for i in range(3)]
                o1 = [st.enter_context(SB("go1%d" % i, [128, D], F32)) for i in range(2)]
                o2 = [st.enter_context(SB("go2%d" % i, [128, D], F32)) for i in range(2)]
                tt_ = [st.enter_context(SB("gtt%d" % i, [128, D], F32)) for i in range(2)]
                gi = [st.enter_context(SB("ggi%d" % i, [128, 2], I32)) for i in range(2)]
                gw = [st.enter_context(SB("ggw%d" % i, [128, 2], F32)) for i in range(2)]
                gb = [st.enter_context(SB("ggb%d" % i, [128, D], F32)) for i in range(NSEQ)]
                for b in range(NSEQ):
                    S.dma("sp", gb[b][:], modrows[l, 1, b, 2, :].partition_broadcast(128), r=[dkey("modrows", l, 1)],
                          w=[("ggb", b)])
                for n in range(T // 128):
                    b = (n * 128) // SEQ
                    r0 = n * 128
                    i2, i3 = n % 2, n % 3
                    S.dma("sp", xb[i3][:], xs[r0:r0 + 128, :], r=[dkey("xs")], w=[("gxb", i3)])
                    S.dma("sp", gi[i2][:], gidx[r0:r0 + 128, :], r=[dkey("gidx")], w=[("ggi", i2)])
                    S.dma("sp", gw[i2][:], gwd[r0:r0 + 128, :], r=[dkey("gwd")], w=[("ggw", i2)])
                    S.dma_indirect("pool", out=o1[i2][:], out_offset=None, in_=og,
                                   in_offset=bass.IndirectOffsetOnAxis(ap=gi[i2][:, 0:1], axis=0),
                                   r=[("ggi", i2)], w=[("go1", i2)], key=("go1", i2))
                    S.dma_indirect("pool", out=o2[i2][:], out_offset=None, in_=og,
                                   in_offset=bass.IndirectOffsetOnAxis(ap=gi[i2][:, 1:2], axis=0),
                                   r=[("ggi", i2)], w=[("go2", i2)], key=("go2", i2))
                    S.op("dve", lambda: nc.vector.tensor_scalar(out=tt_[i2][:], in0=o1[i2][:], scalar1=gw[i2][:, 0:1],
                                                                scalar2=None, op0=ALU.mult),
                         r=[("go1", i2), ("ggw", i2)], w=[("gtt", i2)])
                    S.op("dve", lambda: nc.vector.scalar_tensor_tensor(out=tt_[i2][:], in0=o2[i2][:], scalar=gw[i2][:, 1:2],
                                                                       in1=tt_[i2][:], op0=ALU.mult, op1=ALU.add),
                         r=[("go2", i2), ("ggw", i2), ("gtt", i2)], w=[("gtt", i2)])
                    S.op("pool", lambda: nc.gpsimd.tensor_tensor(out=tt_[i2][:], in0=tt_[i2][:], in1=gb[b][:], op=ALU.mult),
                         r=[("gtt", i2), ("ggb", b)], w=[("gtt", i2)])
                    S.op("dve", lambda: nc.vector.tensor_tensor(out=xb[i3][:], in0=xb[i3][:], in1=tt_[i2][:], op=ALU.add),
                         r=[("gtt", i2), ("gxb", i3)], w=[("gxb", i3)])
                    S.dma("sp", xs[r0:r0 + 128, :], xb[i3][:], r=[("gxb", i3)], w=[dkey("xs")])
                S.barrier()
            continue

        FQ = DFF // 4
        NFC = FQ // 128
        n_exp = NEXP if is_moe else 1
        passes = [(e, q) for e in range(n_exp) for q in range(4)]
        with contextlib.ExitStack() as st:
            WG = [st.enter_context(SB("WG%d" % i, [128, 8, FQ], BF16)) for i in range(2)]
            WU = [st.enter_context(SB("WU%d" % i, [128, 8, FQ], BF16)) for i in range(2)]
            WD = [st.enter_context(SB("WD%d" % i, [128, NFC, D], BF16)) for i in range(2)]
            h2 = [st.enter_context(SB("h2%d" % i, [128, 8, 512], BF16)) for i in range(2)]
            AT = [st.enter_context(SB("AT%d" % i, [128, NFC, 512], BF16)) for i in range(2)]
            sg = [st.enter_context(SB("sg%d" % i, [128, 512], F32)) for i in range(3)]
            xt = [st.enter_context(SB("ext%d" % i, [128, 4, D], F32)) for i in range(2)]
            tO = [st.enter_context(SB("etO%d" % i, [128, 512], F32)) for i in range(3)]
            gb = [st.enter_context(SB("egb%d" % i, [128, D], F32)) for i in range(NSEQ)]
            cmt = [st.enter_context(SB("cmt%d" % i, [128, 4, NEXP], F32)) for i in range(2)]
            pE = [st.enter_context(PS("pE%d" % i, [128, 512], F32)) for i in range(8)]
            cE = {"p": 0, "s": 0, "o": 0}
            for b in range(NSEQ):
                S.dma("sp", gb[b][:], modrows[l, 1, b, 2, :].partition_broadcast(128), r=[dkey("modrows", l, 1)],
                      w=[("egb", b)])

            def load_set(pi):
                e, q = passes[pi]
                ws = pi % 2
                if is_moe:
                    gsrc, usrc, dsrc = mwg_in[li, e], mwu_in[li, e], mwd_in[li, e]
                else:
                    gsrc, usrc, dsrc = dwg_in[li], dwu_in[li], dwd_in[li]
                for k in range(8):
                    S.dma("pool", WG[ws][:, k, :], gsrc[k * 128:(k + 1) * 128, q * FQ:(q + 1) * FQ],
                          r=[dkey("wg")], w=[("WG", ws)])
                for k in range(8):
                    S.dma("pool", WU[ws][:, k, :], usrc[k * 128:(k + 1) * 128, q * FQ:(q + 1) * FQ],
                          r=[dkey("wu")], w=[("WU", ws)])
                for k in range(NFC):
                    S.dma("pool", WD[ws][:, k, :], dsrc[q * FQ + k * 128:q * FQ + (k + 1) * 128, :],
                          r=[dkey("wd")], w=[("WD", ws)])

            tiles = [(pi, b, tt) for pi in range(len(passes)) for b in range(NSEQ) for tt in range(NT)]

            def load_tile(n):
                pi, b, tt = tiles[n]
                i2 = n % 2
                r0 = b * SEQ + tt * 512
                S.dma("sp", h2[i2][:], h2T[:, r0:r0 + 512].rearrange("(k p) t -> p k t", p=128),
                      r=[dkey("h2T")], w=[("h2", i2)])
                S.dma("sp", xt[i2][:], xs[r0:r0 + 512, :].rearrange("(k p) d -> p k d", p=128),
                      r=[("xsrow", b, tt)], w=[("ext", i2)], key=("ext", i2))
                if is_moe:
                    with nc.allow_non_contiguous_dma(reason="per-token combine weights, 32B rows"):
                        S.dma("sp", cmt[i2][:], comb[r0:r0 + 512, :].rearrange("(k p) e -> p k e", p=128),
                              r=[dkey("comb")], w=[("cmt", i2)])

            load_set(0)
            load_tile(0)
            n_tiles_pass = NSEQ * NT
            for n, (pi, b, tt) in enumerate(tiles):
                e, q = passes[pi]
                ws = pi % 2
                i2 = n % 2
                r0 = b * SEQ + tt * 512
                if n % n_tiles_pass == 0 and pi + 1 < len(passes):
                    load_set(pi + 1)
                if n + 1 < len(tiles):
                    load_tile(n + 1)
                for fc in range(NFC):
                    pg = cE["p"] % 8
                    pu = (cE["p"] + 1) % 8
                    cE["p"] += 2
                    for kc in range(8):
                        S.op("pe", lambda kc=kc: nc.tensor.matmul(
                            pE[pg][:], lhsT=WG[ws][:, kc, fc * 128:(fc + 1) * 128], rhs=h2[i2][:, kc, :],
                            start=(kc == 0), stop=(kc == 7)), r=[("WG", ws), ("h2", i2)], w=[("pE", pg)])
                    for kc in range(8):
                        S.op("pe", lambda kc=kc: nc.tensor.matmul(
                            pE[pu][:], lhsT=WU[ws][:, kc, fc * 128:(fc + 1) * 128], rhs=h2[i2][:, kc, :],
                            start=(kc == 0), stop=(kc == 7)), r=[("WU", ws), ("h2", i2)], w=[("pE", pu)])
                    si = cE["s"] % 3
                    cE["s"] += 1
                    S.op("act", lambda: nc.scalar.activation(out=sg[si][:], in_=pE[pg][:], func=AF.Silu),
                         r=[("pE", pg)], w=[("sg", si)])
                    S.op("dve", lambda: nc.vector.tensor_tensor(out=AT[i2][:, fc, :], in0=pE[pu][:], in1=sg[si][:],
                                                                op=ALU.mult),
                         r=[("pE", pu), ("sg", si)], w=[("AT", i2, fc)])
                for blk in range(4):
                    for half in range(2):
                        po = cE["p"] % 8
                        cE["p"] += 1
                        for fc in range(NFC):
                            S.op("pe", lambda fc=fc: nc.tensor.matmul(
                                pE[po][:], lhsT=AT[i2][:, fc, blk * 128:(blk + 1) * 128],
                                rhs=WD[ws][:, fc, half * 512:(half + 1) * 512], start=(fc == 0), stop=(fc == NFC - 1)),
                                r=[("WD", ws), ("AT", i2, fc)], w=[("pE", po)])
                        oi = cE["o"] % 3
                        cE["o"] += 1
                        hs = slice(half * 512, half * 512 + 512)
                        if is_moe:
                            S.op("dve", lambda: nc.vector.scalar_tensor_tensor(
                                out=tO[oi][:], in0=pE[po][:], scalar=cmt[i2][:, blk, e:e + 1], in1=gb[b][:, hs],
                                op0=ALU.mult, op1=ALU.mult), r=[("pE", po), ("egb", b), ("cmt", i2)], w=[("etO", oi)])
                        else:
                            S.op("dve", lambda: nc.vector.tensor_tensor(out=tO[oi][:], in0=pE[po][:], in1=gb[b][:, hs],
                                                                        op=ALU.mult),
                                 r=[("pE", po), ("egb", b)], w=[("etO", oi)])
                        S.op("pool", lambda: nc.gpsimd.tensor_tensor(out=xt[i2][:, blk, hs], in0=xt[i2][:, blk, hs],
                                                                     in1=tO[oi][:], op=ALU.add),
                             r=[("etO", oi), ("ext", i2)], w=[("ext", i2)])
                S.dma("sp", xs[r0:r0 + 512, :].rearrange("(k p) d -> p k d", p=128), xt[i2][:],
                      r=[("ext", i2)], w=[("xsrow", b, tt)], key=("ext", i2))
            S.barrier()

    with contextlib.ExitStack() as st:
        NXB = 4
        xbuf = [st.enter_context(SB("fxb%d" % i, [128, D], F32)) for i in range(NXB)]
        junk = st.enter_context(SB("fjunk", [128, D], BF16))
        ob = [st.enter_context(SB("fob%d" % i, [128, D], F32)) for i in range(3)]
        ssb = [st.enter_context(SB("fss%d" % i, [128, 1], F32)) for i in range(4)]
        rsb = [st.enter_context(SB("frs%d" % i, [128, 1], F32)) for i in range(4)]
        gf = st.enter_context(SB("gf", [128, D], F32))
        S.dma("sp", gf[:], gfin_in[:].partition_broadcast(128), r=[dkey("gfin")], w=[("gf",)])
        for i in range(T // 128):
            r0 = i * 128
            xi, si, oi = i % NXB, i % 4, i % 3
            S.dma("sp", xbuf[xi][:], xs[r0:r0 + 128, :], r=[dkey("xs")], w=[("xb", xi)])
            S.op("act", lambda: nc.scalar.activation(out=junk[:], in_=xbuf[xi][:], func=AF.Square, accum_out=ssb[si][:]),
                 r=[("xb", xi)], w=[("junk",), ("ss", si)])
            rms_rstd((ssb[si][:], ("ss", si)), (rsb[si][:], ("rs", si)), D)
            S.op("dve", lambda: nc.vector.scalar_tensor_tensor(out=ob[oi][:], in0=xbuf[xi][:], scalar=rsb[si][:], in1=gf[:],
                                                               op0=ALU.mult, op1=ALU.mult),
                 r=[("xb", xi), ("rs", si), ("gf",)], w=[("ob", oi)])
            S.dma("sp", out_d[r0:r0 + 128, :], ob[oi][:], r=[("ob", oi)], w=[dkey("out")])
        S.barrier()
    return nc, S


def _consts():
    c = np.zeros((128, 8), np.float32)
    p = np.arange(128)
    j = p % 16
    c[:, 0] = (10000.0 ** (-(2.0 * j) / 32.0)).astype(np.float32)
    is_sin = p >= 64
    sgn = np.where((p % 32) < 16, -1.0, 1.0)
    c[:, 2] = np.where(is_sin, sgn, -1.0)
    c[:, 3] = np.where(is_sin, 0.0, np.pi / 2)
    c[:, 4] = np.where((p // 32) % 2 == 1, MLA_SCALE, 1.0)
    return c


def _prep_shared(inp, L):
    w_in = np.asarray(inp["w_in"])
    offs = np.cumsum([256, 128, 32, 512, 512, 512, 8, 1024, 1024])[:-1]
    cq, ckv, kr, fq, fk, fv, flg, ga, gb = np.split(w_in, offs, axis=-1)
    kr_perm = np.concatenate([kr[..., 16:], kr[..., :16]], axis=-1)
    wa = np.ascontiguousarray(np.concatenate([cq, ckv, kr, kr_perm, fq, fk, fv, ga, gb, flg], axis=-1))
    assert wa.shape[-1] == CA
    w_uq = np.asarray(inp["w_uq"]).reshape(L, QL, NH, 96)
    nope, rope = w_uq[..., :64], w_uq[..., 64:]
    rope_perm = np.concatenate([rope[..., 16:], rope[..., :16]], axis=-1)
    wq = np.ascontiguousarray(np.concatenate([nope, rope, rope_perm], axis=-1).reshape(L, QL, NH * 128))
    w_ukv = np.asarray(inp["w_ukv"]).reshape(L, KVL, NH, 128)
    wkv = np.ascontiguousarray(np.concatenate([w_ukv[..., :64].reshape(L, KVL, 512),
                                               w_ukv[..., 64:].reshape(L, KVL, 512)], axis=-1))
    gq = np.asarray(inp["q_norm_g"])
    gqT = np.ascontiguousarray(gq.reshape(L, 2, 128).transpose(2, 0, 1).reshape(128, L * 2))
    gkvT = np.ascontiguousarray(np.asarray(inp["kv_norm_g"]).T)
    shared = {
        "ada_w": np.asarray(inp["ada_w"]), "ada_b": np.asarray(inp["ada_b"]),
        "norm_mix_g": np.asarray(inp["norm_mix_g"]), "norm_ffn_g": np.asarray(inp["norm_ffn_g"]),
        "wa": wa, "b_forget": np.asarray(inp["b_forget"]), "q_norm_gT": gqT, "wq": wq, "kv_norm_gT": gkvT, "wkv": wkv,
        "w_branch_mla": np.asarray(inp["w_branch_mla"]), "w_branch_fox": np.asarray(inp["w_branch_fox"]),
        "w_out": np.asarray(inp["w_out"]),
        "dense_w_gate": np.asarray(inp["dense_w_gate"]), "dense_w_up": np.asarray(inp["dense_w_up"]),
        "dense_w_down": np.asarray(inp["dense_w_down"]),
        "final_norm_g": np.asarray(inp["final_norm_g"]), "consts": _consts(),
    }
    if L // 2 > 0:
        shared["moe_w_routerT"] = np.ascontiguousarray(np.asarray(inp["moe_w_router"]).transpose(0, 2, 1))
        shared["moe_w_gate"] = np.asarray(inp["moe_w_gate"])
        shared["moe_w_up"] = np.asarray(inp["moe_w_up"])
        shared["moe_w_down"] = np.asarray(inp["moe_w_down"])
    else:
        shared["moe_w_routerT"] = np.zeros((1, NEXP, D), np.float32)
    return shared


def run(inp, cfg, extra_out=()):
    L = cfg.depth
    shared = _prep_shared(inp, L)
    x = np.asarray(inp["x"])
    c = np.asarray(inp["c"])
    pos = np.asarray(inp["positions"]).astype(np.int32)
    in_maps = []
    for i in range(cfg.ncores):
        sl = slice(i * cfg.nseq, (i + 1) * cfg.nseq)
        m = dict(shared)
        m["x"] = np.ascontiguousarray(x[sl].reshape(cfg.T, D))
        m["cT"] = np.ascontiguousarray(c[sl].T)
        m["pos"] = np.ascontiguousarray(pos[sl])
        in_maps.append(m)
    nc, S = build_program(cfg)
    res = run_bass_kernel_spmd(nc, in_maps, core_ids=list(range(cfg.ncores)))
    out = np.concatenate([r["out"].reshape(cfg.nseq, cfg.seq, D) for r in res.results], axis=0)
    if extra_out:
        return out, [{k: r[k] for k in extra_out} for r in res.results]
    return out


def kernel(**inputs):
    B, SEQ, _ = inputs["x"].shape
    L = inputs["ada_w"].shape[0]
    cfg = Cfg(nseq=B // 8, seq=SEQ, depth=L, ncores=8)
    out = run(inputs, cfg)
    return out.astype(np.float32)
```
